# Optimizing a Trainium2 kernel written in Bass

```python
import math
import jax, jax.numpy as jnp
from jax import lax
import numpy as np

D_MODEL = 1024
BATCH = 2
SEQ = 8192
DEPTH = 2

ATT_HEADS = 8
ATT_KV_HEADS = 2
HEAD_DIM = 64
IDX_HEADS = 8
IDX_DIM = 64
TOPK_MAX = 256
Q_BLOCK = 128
ROPE_THETA = 10000.0
RWKV_HEADS = 4
RWKV_HEAD = 64
RWKV_DECAY_LORA = 32
RWKV_A_LORA = 32
RWKV_MV_LORA = 32
RWKV_GATE_LORA = 64
RWKV_GN_EPS = 64e-5
GDN_HEADS = 4
GDN_HEAD = 64
GDN_CONV = 4
GDN_CHUNK = 64
D_FF = 2816
NORM_EPS = 1e-6

ATT_W = ATT_HEADS * HEAD_DIM
KV_W = ATT_KV_HEADS * HEAD_DIM
RWKV_W = RWKV_HEADS * RWKV_HEAD
GDN_W = GDN_HEADS * GDN_HEAD
MIX_W = ATT_W + RWKV_W + GDN_W
ATT_SPLITS = (ATT_W, KV_W, KV_W, IDX_HEADS * IDX_DIM, IDX_DIM, IDX_HEADS)
RWKV_SPLITS = (RWKV_W, RWKV_W, RWKV_W, RWKV_DECAY_LORA, RWKV_A_LORA, RWKV_GATE_LORA)
GDN_SPLITS = (GDN_W, GDN_W, GDN_W, GDN_W, GDN_HEADS, GDN_HEADS)
RWKV_IN = sum(RWKV_SPLITS)
GROUP_SPLITS = (sum(ATT_SPLITS), RWKV_IN, sum(GDN_SPLITS))
IN_W = sum(GROUP_SPLITS)

kernel_name = 'hybrid_dsa_rwkv7_gdn_macaron'


def split_cols(p, sizes):
    offs = [int(o) for o in np.cumsum(sizes)[:-1]]
    return jnp.split(p, offs, axis=-1)


def rms_norm(x, g, eps=NORM_EPS):
    xf = x.astype(jnp.float32)
    y = xf * lax.rsqrt(jnp.mean(xf * xf, axis=-1, keepdims=True) + eps)
    return (y * g.astype(jnp.float32)).astype(x.dtype)


def l2_normalize(x, eps):
    xf = x.astype(jnp.float32)
    return xf * lax.rsqrt(jnp.sum(xf * xf, axis=-1, keepdims=True) + eps)


def rope(x, pos):
    half = x.shape[-1] // 2
    inv = ROPE_THETA ** (-jnp.arange(half, dtype=jnp.float32) / half)
    ang = pos.astype(jnp.float32)[:, None] * inv[None, :]
    cos, sin = jnp.cos(ang)[:, None, :], jnp.sin(ang)[:, None, :]
    xf = x.astype(jnp.float32)
    x1, x2 = xf[..., :half], xf[..., half:]
    return jnp.concatenate([x1 * cos - x2 * sin, x2 * cos + x1 * sin], axis=-1).astype(x.dtype)


def swiglu(h, w_gate, w_up, w_down):
    return (jax.nn.silu(h @ w_gate) * (h @ w_up)) @ w_down


def dsa_attention(pa, q_norm, k_norm):
    B, L, _ = pa.shape
    q, k, v, qi, ki, wi = split_cols(pa, ATT_SPLITS)
    pos = jnp.arange(L)
    q = rope(rms_norm(q.reshape(B, L, ATT_HEADS, HEAD_DIM), q_norm), pos)
    k = rope(rms_norm(k.reshape(B, L, ATT_KV_HEADS, HEAD_DIM), k_norm), pos)
    v = v.reshape(B, L, ATT_KV_HEADS, HEAD_DIM)
    qi = rope(qi.reshape(B, L, IDX_HEADS, IDX_DIM), pos).astype(jnp.float32)
    ki = rope(ki[:, :, None, :], pos)[:, :, 0].astype(jnp.float32)
    wi = wi.astype(jnp.float32) * (IDX_HEADS ** -0.5 * IDX_DIM ** -0.5)
    n_sel = min(TOPK_MAX, L // 4)
    n_blk = L // Q_BLOCK
    rep = ATT_HEADS // ATT_KV_HEADS
    scale = HEAD_DIM ** -0.5

    def to_blocks(a):
        return a.reshape(B, n_blk, Q_BLOCK, *a.shape[2:]).swapaxes(0, 1)

    def block(args):
        qb, qib, wib, tb = args
        logits = jnp.einsum('bqhd,bsd->bqhs', qib, ki)
        score = jnp.einsum('bqhs,bqh->bqs', jax.nn.relu(logits), wib)
        score = jnp.where(pos[None, None, :] <= tb[None, :, None], score, -jnp.inf)
        _, idx = lax.top_k(score, n_sel)
        valid = idx <= tb[None, :, None]
        ks = jax.vmap(lambda kb, ib: kb[ib])(k, idx).astype(jnp.float32)
        vs = jax.vmap(lambda vb, ib: vb[ib])(v, idx)
        qg = qb.reshape(B, Q_BLOCK, ATT_KV_HEADS, rep, HEAD_DIM).astype(jnp.float32)
        s = jnp.einsum('bqgrd,bqkgd->bqgrk', qg, ks) * scale
        s = jnp.where(valid[:, :, None, None, :], s, -jnp.inf)
        p = jax.nn.softmax(s, axis=-1).astype(vs.dtype)
        o = jnp.einsum('bqgrk,bqkgd->bqgrd', p, vs)
        return o.reshape(B, Q_BLOCK, ATT_W)

    out = lax.map(block, (to_blocks(q), to_blocks(qi), to_blocks(wi), pos.reshape(n_blk, Q_BLOCK)))
    return out.swapaxes(0, 1).reshape(B, L, ATT_W).astype(pa.dtype)


def rwkv7_scan(r, w, k, v, a, b):
    B, L, H, N = r.shape

    def step(S, inp):
        rt, wt, kt, vt, at, bt = inp
        sa = jnp.einsum('bhij,bhj->bhi', S, at)
        S = S * wt[:, :, None, :] + sa[..., None] * bt[:, :, None, :] + vt[..., None] * kt[:, :, None, :]
        return S, jnp.einsum('bhij,bhj->bhi', S, rt)

    xs = tuple(jnp.moveaxis(t, 1, 0) for t in (r, w, k, v, a, b))
    _, y = lax.scan(step, jnp.zeros((B, H, N, N), jnp.float32), xs)
    return jnp.moveaxis(y, 0, 1)


def rwkv7_time_mix(pb, mu, w0, w_up, a0, a_up, g_up, k_k, k_a, r_k, gn_w, gn_b, v_first, v0, v_down, v_up):
    dt = pb.dtype
    B, L, _ = pb.shape
    hb = pb.astype(jnp.float32)
    prev = jnp.pad(hb, ((0, 0), (1, 0), (0, 0)))[:, :-1]
    hb = hb + (prev - hb) * mu
    r, k, v, wd, ad, gd = split_cols(hb, RWKV_SPLITS)
    heads = lambda t: t.reshape(B, L, RWKV_HEADS, RWKV_HEAD)
    w_log = -jax.nn.softplus(-(w0 + jnp.tanh(wd) @ w_up)) - 0.5
    decay = jnp.exp(-jnp.exp(w_log))
    a = jax.nn.sigmoid(a0 + ad @ a_up)
    g = jax.nn.sigmoid(gd) @ g_up
    kk = l2_normalize(heads(k * k_k), 1e-12)
    k = k * (1.0 + (a - 1.0) * k_a)
    if v_first is None:
        v_first = v
    else:
        v = v + (v_first - v) * jax.nn.sigmoid(v0 + (v @ v_down) @ v_up)
    rh, kh, vh = heads(r), heads(k), heads(v)
    y = rwkv7_scan(rh, heads(decay), kh, vh, -kk, kk * heads(a))
    mean = jnp.mean(y, axis=-1, keepdims=True)
    var = jnp.mean(jnp.square(y - mean), axis=-1, keepdims=True)
    y = ((y - mean) * lax.rsqrt(var + RWKV_GN_EPS)).reshape(B, L, RWKV_W) * gn_w + gn_b
    bonus = jnp.sum(rh * kh * r_k, axis=-1, keepdims=True) * vh
    y = (y + bonus.reshape(B, L, RWKV_W)) * g
    return y.astype(dt), v_first


def causal_dwconv(x, w):
    C = x.shape[-1]
    return lax.conv_general_dilated(x, w[:, None, :], window_strides=(1,), padding=[(w.shape[0] - 1, 0)],
                                    dimension_numbers=('NWC', 'WIO', 'NWC'), feature_group_count=C)


def chunk_gated_delta_rule(q, k, v, g, beta):
    B, L, H, Dk = q.shape
    Dv = v.shape[-1]
    C = GDN_CHUNK
    n = L // C
    chunks = lambda t: t.reshape(B, n, C, H, t.shape[-1]).transpose(0, 3, 1, 2, 4)
    q, k, v = chunks(q), chunks(k), chunks(v)
    beta = beta.reshape(B, n, C, H).transpose(0, 3, 1, 2)
    g = jnp.cumsum(g.reshape(B, n, C, H).transpose(0, 3, 1, 2), axis=-1)
    incl = jnp.tril(jnp.ones((C, C), bool))
    strict = jnp.tril(jnp.ones((C, C), bool), -1)
    decay = jnp.exp(jnp.where(incl, g[..., :, None] - g[..., None, :], -jnp.inf))
    kb = k * beta[..., None]
    lower = jnp.where(strict, jnp.einsum('bhncd,bhnsd->bhncs', kb, k) * decay, 0.0)
    rhs = jnp.concatenate([v * beta[..., None], kb * jnp.exp(g)[..., None]], axis=-1)
    sol = lax.linalg.triangular_solve(jnp.eye(C, dtype=jnp.float32) + lower, rhs,
                                      left_side=True, lower=True, unit_diagonal=True)
    u, wk = sol[..., :Dv], sol[..., Dv:]
    intra = jnp.where(incl, jnp.einsum('bhncd,bhnsd->bhncs', q, k) * decay, 0.0)

    def step(S, inp):
        qc, kc, uc, wc, gc, ac = inp
        v_new = uc - jnp.einsum('bhck,bhkv->bhcv', wc, S)
        o = jnp.einsum('bhck,bhkv->bhcv', qc * jnp.exp(gc)[..., None], S) + jnp.einsum('bhcs,bhsv->bhcv', ac, v_new)
        g_last = gc[..., -1]
        S = S * jnp.exp(g_last)[..., None, None] + jnp.einsum(
            'bhck,bhcv->bhkv', kc * jnp.exp(g_last[..., None] - gc)[..., None], v_new)
        return S, o

    xs = tuple(jnp.moveaxis(t, 2, 0) for t in (q, k, u, wk, g, intra))
    _, o = lax.scan(step, jnp.zeros((B, H, Dk, Dv), jnp.float32), xs)
    return o.transpose(1, 0, 3, 2, 4).reshape(B, L, H, Dv)


def gated_deltanet(pc, conv_w, a_log, dt_bias, o_norm):
    dt = pc.dtype
    B, L, _ = pc.shape
    q, k, v, z, b_in, a_in = split_cols(pc.astype(jnp.float32), GDN_SPLITS)
    qkv = jax.nn.silu(causal_dwconv(jnp.concatenate([q, k, v], axis=-1), conv_w.astype(jnp.float32)))
    q, k, v = jnp.split(qkv, 3, axis=-1)
    heads = lambda t: t.reshape(B, L, GDN_HEADS, GDN_HEAD)
    q = l2_normalize(heads(q), 1e-6) * (GDN_HEAD ** -0.5)
    k = l2_normalize(heads(k), 1e-6)
    beta = jax.nn.sigmoid(b_in)
    g = -jnp.exp(a_log.astype(jnp.float32)) * jax.nn.softplus(a_in + dt_bias)
    o = chunk_gated_delta_rule(q, k, heads(v), g, beta)
    o = rms_norm(o, o_norm) * jax.nn.silu(heads(z))
    return o.reshape(B, L, GDN_W).astype(dt)


def setup_inputs(seed: int = 0) -> dict:
    key = jax.random.key(seed)
    ks = iter(jax.random.split(key, 40))
    nrm = lambda shape, s: jax.random.normal(next(ks), shape, jnp.float32) * s
    uni = lambda shape, lo, hi: jax.random.uniform(next(ks), shape, jnp.float32, lo, hi)
    D, F = D_MODEL, D_FF
    dt0 = jnp.exp(uni((DEPTH, GDN_HEADS), math.log(1e-3), math.log(1e-1)))
    return {
        'x': nrm((BATCH, SEQ, D), 1.0),
        'ffn_norm': 1.0 + nrm((DEPTH, 2, D), 0.02),
        'ffn_w_gate': nrm((DEPTH, 2, D, F), D ** -0.5),
        'ffn_w_up': nrm((DEPTH, 2, D, F), D ** -0.5),
        'ffn_w_down': nrm((DEPTH, 2, F, D), F ** -0.5),
        'mix_norm': 1.0 + nrm((DEPTH, D), 0.02),
        'w_in': nrm((DEPTH, D, IN_W), D ** -0.5),
        'w_out': nrm((DEPTH, MIX_W, D), MIX_W ** -0.5),
        'att_q_norm': 1.0 + nrm((DEPTH, HEAD_DIM), 0.02),
        'att_k_norm': 1.0 + nrm((DEPTH, HEAD_DIM), 0.02),
        'rwkv_mu': uni((DEPTH, RWKV_IN), 0.0, 1.0),
        'rwkv_w0': uni((DEPTH, RWKV_W), -6.5, -1.5),
        'rwkv_w_up': nrm((DEPTH, RWKV_DECAY_LORA, RWKV_W), RWKV_DECAY_LORA ** -0.5),
        'rwkv_a0': nrm((DEPTH, RWKV_W), 0.1),
        'rwkv_a_up': nrm((DEPTH, RWKV_A_LORA, RWKV_W), RWKV_A_LORA ** -0.5),
        'rwkv_g_up': nrm((DEPTH, RWKV_GATE_LORA, RWKV_W), RWKV_GATE_LORA ** -0.5),
        'rwkv_k_k': 0.85 + nrm((DEPTH, RWKV_W), 0.05),
        'rwkv_k_a': 1.0 + nrm((DEPTH, RWKV_W), 0.05),
        'rwkv_r_k': nrm((DEPTH, RWKV_HEADS, RWKV_HEAD), 0.1),
        'rwkv_gn_w': 1.0 + nrm((DEPTH, RWKV_W), 0.02),
        'rwkv_gn_b': nrm((DEPTH, RWKV_W), 0.02),
        'rwkv_v0': 1.0 + nrm((DEPTH - 1, RWKV_W), 0.05),
        'rwkv_v_down': nrm((DEPTH - 1, RWKV_W, RWKV_MV_LORA), RWKV_W ** -0.5),
        'rwkv_v_up': nrm((DEPTH - 1, RWKV_MV_LORA, RWKV_W), 0.1 * RWKV_MV_LORA ** -0.5),
        'gdn_conv': nrm((DEPTH, GDN_CONV, 3 * GDN_W), GDN_CONV ** -0.5),
        'gdn_a_log': jnp.log(uni((DEPTH, GDN_HEADS), 1.0, 16.0)),
        'gdn_dt_bias': dt0 + jnp.log(-jnp.expm1(-dt0)),
        'gdn_o_norm': 1.0 + nrm((DEPTH, GDN_HEAD), 0.02),
    }


def reference(x, ffn_norm, ffn_w_gate, ffn_w_up, ffn_w_down, mix_norm, w_in, w_out, att_q_norm, att_k_norm,
              rwkv_mu, rwkv_w0, rwkv_w_up, rwkv_a0, rwkv_a_up, rwkv_g_up, rwkv_k_k, rwkv_k_a, rwkv_r_k,
              rwkv_gn_w, rwkv_gn_b, rwkv_v0, rwkv_v_down, rwkv_v_up, gdn_conv, gdn_a_log, gdn_dt_bias, gdn_o_norm):
    v_first = None
    for l in range(DEPTH):
        x = x + 0.5 * swiglu(rms_norm(x, ffn_norm[l, 0]), ffn_w_gate[l, 0], ffn_w_up[l, 0], ffn_w_down[l, 0])
        p = rms_norm(x, mix_norm[l]) @ w_in[l]
        pa, pb, pc = split_cols(p, GROUP_SPLITS)
        ya = dsa_attention(pa, att_q_norm[l], att_k_norm[l])
        if l == 0:
            vres = (None, None, None)
        else:
            vres = (rwkv_v0[l - 1], rwkv_v_down[l - 1], rwkv_v_up[l - 1])
        yb, v_first = rwkv7_time_mix(pb, rwkv_mu[l], rwkv_w0[l], rwkv_w_up[l], rwkv_a0[l], rwkv_a_up[l],
                                     rwkv_g_up[l], rwkv_k_k[l], rwkv_k_a[l], rwkv_r_k[l], rwkv_gn_w[l],
                                     rwkv_gn_b[l], v_first, *vres)
        yc = gated_deltanet(pc, gdn_conv[l], gdn_a_log[l], gdn_dt_bias[l], gdn_o_norm[l])
        x = x + jnp.concatenate([ya, yb, yc], axis=-1) @ w_out[l]
        x = x + 0.5 * swiglu(rms_norm(x, ffn_norm[l, 1]), ffn_w_gate[l, 1], ffn_w_up[l, 1], ffn_w_down[l, 1])
    return x
```

```python
import numpy as np
L = 8192
import concourse.bass as bass
import concourse.mybir as mybir
from concourse.bass_utils import run_bass_kernel_spmd

F32 = mybir.dt.float32
BF16 = mybir.dt.bfloat16
ALU = mybir.AluOpType
AF = mybir.ActivationFunctionType
AX = mybir.AxisListType

ENGS = ("pe", "act", "dve", "pool", "sp")
NDMASEM = 8


class Prog:
    def __init__(self, name="k"):
        self.nc = bass.Bass("TRN2", target_bir_lowering=False)
        self.ops = []
        self.last_write = {}
        self.readers = {}
        self.n_sb = 0
        self.n_ps = 0

    def dram(self, name, shape, dtype=F32, kind="ExternalInput"):
        return self.nc.dram_tensor(name, list(shape), dtype, kind=kind).ap()

    def sb(self, shape, dtype=F32, name=None):
        self.n_sb += 1
        return self.nc.alloc_sbuf_tensor("s_" + (name or f"sb{self.n_sb}"), list(shape), dtype)

    def ps(self, shape, dtype=F32, name=None):
        self.n_ps += 1
        return self.nc.alloc_psum_tensor("p_" + (name or f"ps{self.n_ps}"), list(shape), dtype)

    def op(self, eng, fn, reads=(), writes=(), dma=False):
        import sys
        fr = sys._getframe(1)
        if fr.f_code.co_name in ("mm", "dma"):
            fr = fr.f_back
        self._line = fr.f_lineno
        oid = len(self.ops)
        deps = set()
        for k in reads:
            if k in self.last_write:
                deps.add(self.last_write[k])
        for k in writes:
            if k in self.last_write:
                deps.add(self.last_write[k])
            for r in self.readers.get(k, ()):
                deps.add(r)
        deps.discard(oid)
        self.ops.append(dict(eng=eng, fn=fn, deps=deps, dma=dma, users=0, line=self._line, id=oid))
        for k in reads:
            self.readers.setdefault(k, []).append(oid)
        for k in writes:
            self.last_write[k] = oid
            self.readers[k] = []
        return oid

    def mm(self, out, lhsT, rhs, start, stop, reads, writes):
        self.op("pe", lambda e: e.matmul(out, lhsT, rhs, start=start, stop=stop), reads, writes)

    def dma(self, eng, out, in_, reads, writes, **kw):
        self.op(eng, lambda e: e.dma_start(out=out, in_=in_, **kw), reads, writes, dma=True)

    def emit(self, final_wait_keys=()):
        nc = self.nc
        ops = self.ops
        fdeps = set()
        for k in final_wait_keys:
            if k in self.last_write:
                fdeps.add(self.last_write[k])
        for o in ops:
            if o["eng"] == "pe" and not o["dma"]:
                o["deps"] = {d for d in o["deps"] if not (ops[d]["eng"] == "pe" and not ops[d]["dma"])}
        for o in ops:
            for d in o["deps"]:
                ops[d]["users"] += 1
        for d in fdeps:
            ops[d]["users"] += 1
        from contextlib import ExitStack
        with ExitStack() as st:
            esem = {e: st.enter_context(nc.semaphore(f"s_{e}")) for e in ENGS}
            dsem = {e: [st.enter_context(nc.semaphore(f"d_{e}{i}")) for i in range(NDMASEM)] for e in ("sp", "act", "pool")}
            ecount = {e: 0 for e in ENGS}
            dcount = {e: [0] * NDMASEM for e in dsem}
            drr = {e: 0 for e in dsem}
            for o in ops:
                e = o["eng"]
                if o["dma"]:
                    i = drr[e]
                    drr[e] = (i + 1) % NDMASEM
                    o["prev_ticket"] = (dsem[e][i], dcount[e][i])
                    dcount[e][i] += 16
                    o["ticket"] = (dsem[e][i], dcount[e][i])
                elif o["users"] > 0:
                    ecount[e] += 1
                    o["ticket"] = (esem[e], ecount[e])
                else:
                    o["ticket"] = None
            per_eng = {e: [o for o in ops if o["eng"] == e] for e in ENGS}
            block = st.enter_context(nc.Block())

            self.trace = {e: [] for e in ENGS}
            semname = {id(v): k for k, v in esem.items()}
            for k, lst in dsem.items():
                for i, v in enumerate(lst):
                    semname[id(v)] = f"d_{k}{i}"

            def run_engine(e, eng):
                waited = {}
                tr = self.trace[e]

                def wait(sem, val):
                    key = id(sem)
                    if waited.get(key, 0) >= val:
                        return
                    waited[key] = val
                    tr.append(f"   wait {semname[key]} >= {val}")
                    eng.wait_ge(sem, val)

                for o in per_eng[e]:
                    for d in sorted(o["deps"]):
                        sem, val = ops[d]["ticket"]
                        wait(sem, val)
                    if o["dma"]:
                        psem, pval = o["prev_ticket"]
                        if pval > 0:
                            wait(psem, pval)
                    ins = o["fn"](eng)
                    tr.append(f"op{o['id']} L{o['line']} {'dma' if o['dma'] else ''} -> {semname[id(o['ticket'][0])] + '=' + str(o['ticket'][1]) if o['ticket'] else '-'}")
                    if o["ticket"] is not None:
                        sem, val = o["ticket"]
                        ins.then_inc(sem, 16 if o["dma"] else 1)
                if e == "sp":
                    for d in sorted(fdeps):
                        sem, val = ops[d]["ticket"]
                        wait(sem, val)

            @block.sync
            def _(eng):
                run_engine("sp", eng)

            @block.tensor
            def _(eng):
                run_engine("pe", eng)

            @block.scalar
            def _(eng):
                run_engine("act", eng)

            @block.vector
            def _(eng):
                run_engine("dve", eng)

            @block.gpsimd
            def _(eng):
                run_engine("pool", eng)
        return nc


def run(prog_nc, in_maps, n=8):
    res = run_bass_kernel_spmd(prog_nc, in_maps, core_ids=list(range(n)))
    return res.results


D = 1024
DFF = 2816
NT = 2048
TT = 512
NTT = NT // TT
KC = D // 128
FC = DFF // 128
FM_COLS = 2880
TOK_COLS = 400
EPS = 1e-6


def build_P(stages):
    P = Prog()
    nc = P.nc
    xT_d = P.dram("xT", [D, NT])
    xo_d = P.dram("xo", [D, NT], kind="ExternalOutput")
    xT = P.sb([128, KC, NT], F32, "xT")
    hT = P.sb([128, KC, NT], BF16, "hT")
    ones = P.sb([128, 128], BF16, "ones")
    rstd = [P.sb([128, TT], F32, f"rstd{i}") for i in range(2)]
    sq = [P.sb([128, TT], BF16, f"sq{i}") for i in range(2)]
    wA = [P.sb([128, KC, 512], BF16, f"wA{i}") for i in range(2)]
    wB = [P.sb([128, KC, 512], BF16, f"wB{i}") for i in range(2)]
    wD = [P.sb([128, 4, D], BF16, f"wD{i}") for i in range(2)]
    actT = [P.sb([128, 4, TT], BF16, f"actT{i}") for i in range(2)]
    sg = [P.sb([128, TT], F32, f"sg{i}") for i in range(2)]
    stg = [P.sb([128, TT], F32, f"stg{i}") for i in range(2)]
    ps_a = [P.ps([128, TT], F32, f"psa{i}") for i in range(2)]
    ps_b = [P.ps([128, TT], F32, f"psb{i}") for i in range(2)]
    ps_d = [P.ps([128, TT], F32, f"psd{i}") for i in range(4)]

    P.op("dve", lambda e: e.memset(ones[:], 1.0 / D), writes=["ones"])
    for kc in range(KC):
        P.dma("sp", xT[:, kc, :], xT_d[kc * 128:(kc + 1) * 128, :], reads=[], writes=[("xT", kc, t) for t in range(NTT)])

    finals = []

    def fkey():
        k = ("fin", len(finals))
        finals.append(k)
        return k

    cnt = dict(sq=0, rstd=0, w=0, wd=0, act=0, sg=0, stg=0, psa=0, psd=0)

    def norm_to_hT(gcol, tag):
        for t in range(NTT):
            ts = slice(t * TT, (t + 1) * TT)
            pi = cnt["psa"] % 2
            cnt["psa"] += 1
            for kc in range(KC):
                si = cnt["sq"] % 2
                cnt["sq"] += 1
                P.op("act", lambda e, kc=kc, si=si, ts=ts: e.activation(out=sq[si][:], in_=xT[:, kc, ts], func=AF.Square),
                     reads=[("xT", kc, t)], writes=[("sq", si)])
                P.mm(ps_a[pi][:], ones[:], sq[si][:], kc == 0, kc == KC - 1, reads=["ones", ("sq", si)], writes=[("psa", pi)])
            ri = cnt["rstd"] % 2
            cnt["rstd"] += 1
            P.op("act", lambda e, ri=ri, pi=pi: e.activation(out=rstd[ri][:], in_=ps_a[pi][:], func=AF.Sqrt, bias=EPS, scale=1.0),
                 reads=[("psa", pi)], writes=[("rstd", ri)])
            P.op("dve", lambda e, ri=ri: e.reciprocal(out=rstd[ri][:], in_=rstd[ri][:]),
                 reads=[("rstd", ri)], writes=[("rstd", ri)])
            for kc in range(KC):
                P.op("dve", lambda e, kc=kc, ri=ri, ts=ts: e.scalar_tensor_tensor(out=hT[:, kc, ts], in0=xT[:, kc, ts], scalar=gcol[:, kc:kc + 1],
                                                                              in1=rstd[ri][:], op0=ALU.mult, op1=ALU.mult),
                     reads=[("xT", kc, t), ("rstd", ri), ("g", tag)], writes=[("hT", kc, t)])

    def load_gcol(name, tag):
        g_d = P.dram(name, [128, KC])
        g = P.sb([128, KC], F32, "g_" + name)
        P.dma("sp", g[:], g_d[:, :], reads=[], writes=[("g", tag)])
        return g

    def ffn(tag):
        g = load_gcol(f"g_{tag}", tag)
        wg_d = P.dram(f"wg_{tag}", [D, DFF])
        wu_d = P.dram(f"wu_{tag}", [D, DFF])
        wd_d = P.dram(f"wd_{tag}", [DFF, D])
        wg_v = wg_d.rearrange("(kc p) f -> p kc f", p=128)
        wu_v = wu_d.rearrange("(kc p) f -> p kc f", p=128)
        wd_v = wd_d.rearrange("(fc p) d -> p fc d", p=128)
        norm_to_hT(g, tag)
        groups = []
        f0 = 0
        while f0 < FC:
            n = min(4, FC - f0)
            groups.append((f0, n))
            f0 += n

        def load_group(gi):
            f0, n = groups[gi]
            wi = cnt["w"] % 2
            cnt["w"] += 1
            P.dma("pool", wA[wi][:, :, :n * 128], wg_v[:, :, f0 * 128:(f0 + n) * 128], reads=[], writes=[("wA", wi)])
            P.dma("pool", wB[wi][:, :, :n * 128], wu_v[:, :, f0 * 128:(f0 + n) * 128], reads=[], writes=[("wB", wi)])
            P.dma("pool", wD[wi][:, :n, :], wd_v[:, f0:f0 + n, :], reads=[], writes=[("wD", wi)])
            return wi

        nxt = load_group(0)
        for gi, (f0, n) in enumerate(groups):
            wi = nxt
            if gi + 1 < len(groups):
                nxt = load_group(gi + 1)
            for t in range(NTT):
                ts = slice(t * TT, (t + 1) * TT)
                ai = cnt["act"] % 2
                cnt["act"] += 1
                for f in range(n):
                    pi = cnt["psa"] % 2
                    cnt["psa"] += 1
                    for kc in range(KC):
                        P.mm(ps_a[pi][:], wA[wi][:, kc, f * 128:(f + 1) * 128], hT[:, kc, ts], kc == 0, kc == KC - 1,
                             reads=[("wA", wi), ("hT", kc, t)], writes=[("psa", pi)])
                    for kc in range(KC):
                        P.mm(ps_b[pi][:], wB[wi][:, kc, f * 128:(f + 1) * 128], hT[:, kc, ts], kc == 0, kc == KC - 1,
                             reads=[("wB", wi), ("hT", kc, t)], writes=[("psb", pi)])
                    si = cnt["sg"] % 2
                    cnt["sg"] += 1
                    P.op("act", lambda e, si=si, pi=pi: e.activation(out=sg[si][:], in_=ps_a[pi][:], func=AF.Silu),
                         reads=[("psa", pi)], writes=[("sg", si)])
                    P.op("dve", lambda e, si=si, pi=pi, ai=ai, f=f: e.tensor_tensor(out=actT[ai][:, f, :], in0=sg[si][:], in1=ps_b[pi][:], op=ALU.mult),
                         reads=[("sg", si), ("psb", pi)], writes=[("actT", ai, f)])
                for dc in range(KC):
                    di = cnt["psd"] % 4
                    cnt["psd"] += 1
                    for f in range(n):
                        P.mm(ps_d[di][:], wD[wi][:, f, dc * 128:(dc + 1) * 128], actT[ai][:, f, :], f == 0, f == n - 1,
                             reads=[("wD", wi), ("actT", ai, f)], writes=[("psd", di)])
                    P.op("dve", lambda e, di=di, dc=dc, ts=ts: e.scalar_tensor_tensor(out=xT[:, dc, ts], in0=ps_d[di][:], scalar=0.5, in1=xT[:, dc, ts],
                                                                                  op0=ALU.mult, op1=ALU.add),
                         reads=[("psd", di), ("xT", dc, t)], writes=[("xT", dc, t)])

    def outproj(tag):
        yT_d = P.dram(f"yT_{tag}", [D, NT])
        wo_d = P.dram(f"wo_{tag}", [D, D])
        wo_v = wo_d.rearrange("(kc p) d -> p kc d", p=128)
        for h in range(2):
            wi = cnt["w"] % 2
            cnt["w"] += 1
            P.dma("pool", wA[wi][:, :, :], wo_v[:, :, h * 512:(h + 1) * 512], reads=[], writes=[("wA", wi)])
            if h == 0:
                for kc in range(KC):
                    P.dma("pool", hT[:, kc, :], yT_d[kc * 128:(kc + 1) * 128, :], reads=[], writes=[("hT", kc, t) for t in range(NTT)])
            for t in range(NTT):
                ts = slice(t * TT, (t + 1) * TT)
                for dd in range(4):
                    dc = h * 4 + dd
                    di = cnt["psd"] % 4
                    cnt["psd"] += 1
                    for kc in range(KC):
                        P.mm(ps_d[di][:], wA[wi][:, kc, dd * 128:(dd + 1) * 128], hT[:, kc, ts], kc == 0, kc == KC - 1,
                             reads=[("wA", wi), ("hT", kc, t)], writes=[("psd", di)])
                    P.op("dve", lambda e, di=di, dc=dc, ts=ts: e.tensor_tensor(out=xT[:, dc, ts], in0=ps_d[di][:], in1=xT[:, dc, ts], op=ALU.add),
                         reads=[("psd", di), ("xT", dc, t)], writes=[("xT", dc, t)])

    def inproj(tag):
        g = load_gcol(f"g_{tag}", tag)
        win_d = P.dram(f"win_{tag}", [D, FM_COLS + TOK_COLS])
        win_v = win_d.rearrange("(kc p) c -> p kc c", p=128)
        pT_d = P.dram(f"pT_{tag}", [FM_COLS, NT], kind="ExternalOutput")
        ptok_d = P.dram(f"ptok_{tag}", [NT, TOK_COLS], kind="ExternalOutput")
        norm_to_hT(g, tag)
        c0 = 0
        while c0 < FM_COLS:
            n = min(512, FM_COLS - c0)
            wi = cnt["w"] % 2
            cnt["w"] += 1
            P.dma("pool", wA[wi][:, :, :n], win_v[:, :, c0:c0 + n], reads=[], writes=[("wA", wi)])
            for t in range(NTT):
                ts = slice(t * TT, (t + 1) * TT)
                for m0 in range(0, n, 128):
                    m = min(128, n - m0)
                    di = cnt["psd"] % 4
                    cnt["psd"] += 1
                    for kc in range(KC):
                        P.mm(ps_d[di][:m, :], wA[wi][:, kc, m0:m0 + m], hT[:, kc, ts], kc == 0, kc == KC - 1,
                             reads=[("wA", wi), ("hT", kc, t)], writes=[("psd", di)])
                    si = cnt["stg"] % 2
                    cnt["stg"] += 1
                    P.op("act", lambda e, si=si, di=di, m=m: e.activation(out=stg[si][:m, :], in_=ps_d[di][:m, :], func=AF.Copy),
                         reads=[("psd", di)], writes=[("stg", si)])
                    P.dma("sp", pT_d[c0 + m0:c0 + m0 + m, ts], stg[si][:m, :], reads=[("stg", si)], writes=[fkey()])
            c0 += n
        wi = cnt["w"] % 2
        cnt["w"] += 1
        P.dma("pool", wA[wi][:, :, :TOK_COLS], win_v[:, :, FM_COLS:FM_COLS + TOK_COLS], reads=[], writes=[("wA", wi)])
        for b in range(NT // 128):
            t = b // 4
            di = cnt["psd"] % 4
            cnt["psd"] += 1
            for kc in range(KC):
                P.mm(ps_d[di][:, :TOK_COLS], hT[:, kc, b * 128:(b + 1) * 128], wA[wi][:, kc, :TOK_COLS], kc == 0, kc == KC - 1,
                     reads=[("wA", wi), ("hT", kc, t)], writes=[("psd", di)])
            si = cnt["stg"] % 2
            cnt["stg"] += 1
            P.op("act", lambda e, si=si, di=di: e.activation(out=stg[si][:, :TOK_COLS], in_=ps_d[di][:, :TOK_COLS], func=AF.Copy),
                 reads=[("psd", di)], writes=[("stg", si)])
            P.dma("sp", ptok_d[b * 128:(b + 1) * 128, :], stg[si][:, :TOK_COLS], reads=[("stg", si)], writes=[fkey()])

    for kind, tag in stages:
        if kind == "ffn":
            ffn(tag)
        elif kind == "out":
            outproj(tag)
        elif kind == "inproj":
            inproj(tag)
    for kc in range(KC):
        P.dma("sp", xo_d[kc * 128:(kc + 1) * 128, :], xT[:, kc, :], reads=[("xT", kc, t) for t in range(NTT)], writes=[("xo", kc)])
        finals.append(("xo", kc))
    return P.emit(final_wait_keys=finals)


NQB = 16
NIT = 26
NEG = -30000.0


def dsa_consts():
    Rm = np.zeros((128, 128), np.float32)
    for blk in (0, 64):
        for dp in range(32):
            Rm[blk + dp + 32, blk + dp] = -1.0
            Rm[blk + dp, blk + dp + 32] = 1.0
    tri = np.where(np.arange(128)[None, :] > np.arange(128)[:, None], -1e30, 0.0).astype(np.float32)
    I4 = np.tile(np.eye(128, dtype=np.float32), (1, 4))
    sel = np.zeros((65, 64), np.float32)
    sel[64, :] = 1.0
    bo = np.zeros((128, 128), np.float32)
    bo[:64, :64] = 1.0 / 64
    bo[64:, 64:] = 1.0 / 64
    pw = np.tile((2.0 ** -(np.arange(NIT) + 1.0))[None, :], (128, 1)).astype(np.float32)
    inv = 10000.0 ** (-np.arange(32, dtype=np.float32) / 32)
    ang = np.arange(L, dtype=np.float32)[None, :] * inv[:, None]
    cos = np.cos(ang).astype(np.float32)
    sin = np.sin(ang).astype(np.float32)
    cosT = np.tile(cos, (4, 1))
    sinT = np.tile(sin, (4, 1))
    return dict(Rm=Rm, tri=tri, I4=I4, sel=sel, bo=bo, pw=pw, cosT=cosT, sinT=sinT)


def build_dsa():
    P = Prog()
    kT_d = P.dram("kT", [128, L])
    kiT_d = P.dram("kiT", [64, L])
    v_d = P.dram("vtok", [L, 128])
    qT_d = P.dram("qT", [512, NQB * 128])
    qiT_d = P.dram("qiT", [512, NQB * 128])
    wi_d = P.dram("wi", [NQB * 128, 8])
    cos_d = P.dram("cosT", [128, L])
    sin_d = P.dram("sinT", [128, L])
    cosq_d = P.dram("cosq", [128, NQB * 128])
    sinq_d = P.dram("sinq", [128, NQB * 128])
    gq_d = P.dram("gq", [128, 1])
    gk_d = P.dram("gk", [128, 1])
    Rm_d = P.dram("Rm", [128, 128])
    tri_d = P.dram("tri4", [128, 512])
    I4_d = P.dram("I4", [128, 512])
    sel_d = P.dram("sel", [65, 64])
    bo_d = P.dram("bo", [128, 128])
    pw_d = P.dram("pw", [128, NIT])
    ya_d = P.dram("yaT", [512, NQB * 128], kind="ExternalOutput")

    Rm = P.sb([128, 128], BF16, "Rm")
    tri = P.sb([128, 512], F32, "tri")
    I4 = P.sb([128, 512], BF16, "I4")
    sel = P.sb([65, 64], F32, "sel")
    bo = P.sb([128, 128], BF16, "bo")
    pw = P.sb([128, NIT], F32, "pw")
    gq = P.sb([128, 1], F32, "gq")
    gk = P.sb([128, 1], F32, "gk")
    P.dma("pool", Rm[:], Rm_d[:, :], [], ["Rm"])
    P.dma("pool", I4[:], I4_d[:, :], [], ["I4"])
    P.dma("pool", bo[:], bo_d[:, :], [], ["bo"])
    P.dma("sp", tri[:], tri_d[:, :], [], ["tri"])
    P.dma("sp", sel[:], sel_d[:, :], [], ["sel"])
    P.dma("sp", pw[:], pw_d[:, :], [], ["pw"])
    P.dma("sp", gq[:], gq_d[:, :], [], ["gq"])
    P.dma("sp", gk[:], gk_d[:, :], [], ["gk"])

    K_fin = P.sb([128, L], BF16, "K_fin")
    KI_fin = P.sb([128, L], BF16, "KI_fin")
    V1 = P.sb([128, 64, 2, 65], BF16, "V1")
    acc = P.sb([128, L], F32, "acc")
    junk = P.sb([128, L], BF16, "junk")
    mb = P.sb([128, L], BF16, "mb")
    Q_fin = P.sb([128, 4, 128], BF16, "Q_fin")
    QI_fin = P.sb([128, 4, 128], BF16, "QI_fin")
    wi = P.sb([128, 8], F32, "wi")
    raw = [P.sb([128, 512], F32, f"raw{i}") for i in range(2)]
    cs = [P.sb([128, 512], F32, f"cs{i}") for i in range(2)]
    sn = [P.sb([128, 512], F32, f"sn{i}") for i in range(2)]
    sqb = P.sb([128, 512], BF16, "sqb")
    rs = P.sb([128, 512], F32, "rs")
    kn = P.sb([128, 512], F32, "kn")
    knb = P.sb([128, 512], BF16, "knb")
    t1 = P.sb([128, 512], F32, "t1")
    t2 = P.sb([128, 512], F32, "t2")
    rbuf = [P.sb([128, 512], F32, f"rbuf{i}") for i in range(2)]
    pexp = [P.sb([128, 512], BF16, f"pexp{i}") for i in range(2)]
    o_sb = [P.sb([65, 512], F32, f"o_sb{i}") for i in range(2)]
    rden = P.sb([64, 512], F32, "rden")
    ya_t = [P.sb([64, 512], F32, f"ya_t{i}") for i in range(2)]
    col = {n: P.sb([128, 1], F32, "c_" + n) for n in ("mx", "mn", "lo", "rng", "mid", "cnt", "cs_", "thr0")}
    steps = P.sb([128, NIT], F32, "steps")

    ps_prep = P.ps([128, 512], F32, "ps_prep")
    ps_i = [P.ps([128, 512], F32, f"ps_i{i}") for i in range(2)]
    ps_l = [P.ps([128, 512], F32, f"ps_l{i}") for i in range(2)]
    ps_o = [P.ps([128, 512], F32, f"ps_o{i}") for i in range(2)]
    ps_den = P.ps([64, 512], F32, "ps_den")

    cnt = dict(raw=0, r=0, pi=0, pl=0, po=0, pe=0, ya=0)
    finals = []

    P.op("dve", lambda e: e.memset(col["thr0"][:], -1e29), writes=["thr0"])

    def rope_pipeline(src_key, src, cos_ap, sin_ap, cs_keys, out_ap, out_keys, n, norm_g=None, gkey=None):
        if norm_g is not None:
            P.op("act", lambda e: e.activation(out=sqb[:, :n], in_=src, func=AF.Square), [src_key], ["sqb"])
            P.mm(ps_prep[:, :n], bo[:], sqb[:, :n], True, True, ["bo", "sqb"], ["ps_prep"])
            P.op("act", lambda e: e.activation(out=rs[:, :n], in_=ps_prep[:, :n], func=AF.Sqrt, bias=1e-6, scale=1.0), ["ps_prep"], ["rs"])
            P.op("dve", lambda e: e.reciprocal(out=rs[:, :n], in_=rs[:, :n]), ["rs"], ["rs"])
            P.op("dve", lambda e: e.scalar_tensor_tensor(out=kn[:, :n], in0=src, scalar=norm_g[:, 0:1], in1=rs[:, :n], op0=ALU.mult, op1=ALU.mult),
                 [src_key, "rs", gkey], ["kn"])
            cur, cur_key = kn[:, :n], "kn"
        else:
            cur, cur_key = src, src_key
        P.op("act", lambda e: e.activation(out=knb[:, :n], in_=cur, func=AF.Copy), [cur_key], ["knb"])
        P.mm(ps_prep[:, :n], Rm[:], knb[:, :n], True, True, ["Rm", "knb"], ["ps_prep"])
        P.op("dve", lambda e: e.tensor_tensor(out=t1[:, :n], in0=cur, in1=cos_ap, op=ALU.mult), [cur_key] + cs_keys, ["t1"])
        P.op("dve", lambda e: e.tensor_tensor(out=t2[:, :n], in0=ps_prep[:, :n], in1=sin_ap, op=ALU.mult), ["ps_prep"] + cs_keys, ["t2"])
        P.op("dve", lambda e: e.tensor_tensor(out=out_ap, in0=t1[:, :n], in1=t2[:, :n], op=ALU.add), ["t1", "t2"], out_keys)

    v_v = v_d.rearrange("(kt p) (g d) -> p kt g d", p=128, g=2)
    for q4 in range(4):
        P.dma("pool", V1[:, q4 * 16:(q4 + 1) * 16, 0, 0:64], v_v[:, q4 * 16:(q4 + 1) * 16, 0, :], [], [("V1", q4)])
        P.dma("pool", V1[:, q4 * 16:(q4 + 1) * 16, 1, 0:64], v_v[:, q4 * 16:(q4 + 1) * 16, 1, :], [], [("V1b", q4)])
    P.op("dve", lambda e: e.memset(V1[:, :, :, 64:65], 1.0), [], ["V1ones"])
    for kt in range(L // 512):
        ks = slice(kt * 512, (kt + 1) * 512)
        ri = cnt["raw"] % 2
        cnt["raw"] += 1
        P.dma("sp", cs[ri][:], cos_d[:, ks], [], [("cs", ri)])
        P.dma("sp", sn[ri][:], sin_d[:, ks], [], [("sn", ri)])
        P.dma("sp", raw[ri][:], kT_d[:, ks], [], [("raw", ri)])
        rope_pipeline(("raw", ri), raw[ri][:], cs[ri][:], sn[ri][:], [("cs", ri), ("sn", ri)], K_fin[:, ks], [("K", kt)], 512, norm_g=gk, gkey="gk")
        ri2 = cnt["raw"] % 2
        cnt["raw"] += 1
        P.dma("sp", raw[ri2][0:64, :], kiT_d[:, ks], [], [("raw", ri2)])
        P.dma("sp", raw[ri2][64:128, :], kiT_d[:, ks], [], [("raw", ri2, "b")])
        rope_pipeline(("raw", ri2), raw[ri2][:], cs[ri][:], sn[ri][:], [("cs", ri), ("sn", ri), ("raw", ri2, "b")], KI_fin[:, ks], [("KI", kt)], 512)

    qT_v = qT_d.rearrange("(j p) t -> p j t", p=128)
    qiT_v = qiT_d.rearrange("(j p) t -> p j t", p=128)
    ya_v = ya_d.rearrange("(h d) t -> d h t", d=64)

    for lb in range(NQB):
        S = 512 * (lb + 1)
        qs = slice(lb * 128, (lb + 1) * 128)
        nkt = S // 128
        ri = cnt["raw"] % 2
        cnt["raw"] += 1
        P.dma("sp", raw[ri][:].rearrange("p (j t) -> p j t", j=4), qT_v[:, :, qs], [], [("raw", ri)])
        for j in range(4):
            P.dma("sp", cs[ri][:, j * 128:(j + 1) * 128], cosq_d[:, qs], [], [("cs", ri)])
            P.dma("sp", sn[ri][:, j * 128:(j + 1) * 128], sinq_d[:, qs], [], [("sn", ri)])
        cskeys = [("cs", ri), ("sn", ri)]
        rope_pipeline(("raw", ri), raw[ri][:], cs[ri][:], sn[ri][:], cskeys, Q_fin[:].rearrange("p j t -> p (j t)"), ["Q"], 512, norm_g=gq, gkey="gq")
        ri2 = cnt["raw"] % 2
        cnt["raw"] += 1
        P.dma("sp", raw[ri2][:].rearrange("p (j t) -> p j t", j=4), qiT_v[:, :, qs], [], [("raw", ri2)])
        rope_pipeline(("raw", ri2), raw[ri2][:], cs[ri][:], sn[ri][:], cskeys, QI_fin[:].rearrange("p j t -> p (j t)"), ["QI"], 512)
        P.dma("sp", wi[:], wi_d[qs, :], [], ["wi"])

        nst = (S + 511) // 512
        for st in range(nst):
            w = min(512, S - st * 512)
            ss = slice(st * 512, st * 512 + w)
            kts = sorted(set([(st * 512) // 512]))
            for h in range(8):
                m, half = h // 2, h % 2
                pi = cnt["pi"] % 2
                cnt["pi"] += 1
                hp = slice(64 * half, 64 * half + 64)
                P.mm(ps_i[pi][:, :w], QI_fin[hp, m, :], KI_fin[hp, ss], True, True, ["QI", ("KI", st)], [("ps_i", pi)])
                r_i = cnt["r"] % 2
                cnt["r"] += 1
                P.op("act", lambda e, r_i=r_i, pi=pi, w=w: e.activation(out=rbuf[r_i][:, :w], in_=ps_i[pi][:, :w], func=AF.Relu),
                     [("ps_i", pi)], [("rbuf", r_i)])
                if h == 0:
                    P.op("dve", lambda e, r_i=r_i, w=w, ss=ss: e.tensor_scalar(out=acc[:, ss], in0=rbuf[r_i][:, :w], scalar1=wi[:, 0:1], scalar2=None, op0=ALU.mult),
                         [("rbuf", r_i), "wi"], [("acc", st)])
                else:
                    P.op("dve", lambda e, r_i=r_i, w=w, ss=ss, h=h: e.scalar_tensor_tensor(out=acc[:, ss], in0=rbuf[r_i][:, :w], scalar=wi[:, h:h + 1], in1=acc[:, ss],
                                                                                         op0=ALU.mult, op1=ALU.add),
                         [("rbuf", r_i), "wi", ("acc", st)], [("acc", st)])
        acc_keys = [("acc", st) for st in range(nst)]
        if lb == 0:
            P.op("dve", lambda e: e.tensor_tensor(out=t1[:], in0=acc[:, 0:512], in1=tri[:], op=ALU.subtract), [("acc", 0), "tri"], ["t1"])
            P.op("dve", lambda e: e.tensor_reduce(out=col["lo"][:], in_=t1[:], axis=AX.X, op=ALU.min), ["t1"], ["lo"])
        else:
            P.op("dve", lambda e, S=S: e.tensor_reduce(out=col["lo"][:], in_=acc[:, :S - 512], axis=AX.X, op=ALU.min), acc_keys, ["lo"])
        P.op("dve", lambda e, S=S: e.tensor_tensor(out=acc[:, S - 512:S], in0=acc[:, S - 512:S], in1=tri[:], op=ALU.add),
             [("acc", nst - 1), "tri"], [("acc", nst - 1)])
        if True:
            P.op("dve", lambda e, S=S: e.tensor_reduce(out=col["mx"][:], in_=acc[:, :S], axis=AX.X, op=ALU.max), acc_keys, ["mx"])
            P.op("dve", lambda e: e.tensor_tensor(out=col["rng"][:], in0=col["mx"][:], in1=col["lo"][:], op=ALU.subtract), ["mx", "lo"], ["rng"])
            P.op("dve", lambda e: e.tensor_scalar(out=steps[:], in0=pw[:], scalar1=col["rng"][:, 0:1], scalar2=None, op0=ALU.mult), ["pw", "rng"], ["steps"])
            for k in range(NIT):
                P.op("dve", lambda e, k=k: e.tensor_tensor(out=col["mid"][:], in0=col["lo"][:], in1=steps[:, k:k + 1], op=ALU.add), ["lo", "steps"], ["mid"])
                P.op("dve", lambda e, S=S: e.tensor_scalar(out=junk[:, :S], in0=acc[:, :S], scalar1=col["mid"][:, 0:1], scalar2=None, op0=ALU.is_ge,
                                                          op1=ALU.add, accum_out=col["cnt"][:]),
                     acc_keys + ["mid"], ["junk", "cnt"])
                P.op("dve", lambda e: e.memset(col["mn"][:], 0.0), [], ["mn"])
                P.op("dve", lambda e, k=k: e.scalar_tensor_tensor(out=col["cs_"][:], in0=col["cnt"][:], scalar=255.5, in1=steps[:, k:k + 1], op0=ALU.is_ge, op1=ALU.mult),
                     ["cnt", "steps"], ["cs_"])
                P.op("dve", lambda e: e.tensor_tensor(out=col["lo"][:], in0=col["lo"][:], in1=col["cs_"][:], op=ALU.add), ["lo", "cs_"], ["lo"])
            thr, thr_key = col["lo"], "lo"
        P.op("dve", lambda e, S=S, thr=thr: e.tensor_scalar(out=mb[:, :S], in0=acc[:, :S], scalar1=thr[:, 0:1], scalar2=NEG, op0=ALU.is_lt, op1=ALU.mult),
             acc_keys + [thr_key], ["mb"])
        for g in range(2):
            gp = slice(64 * g, 64 * g + 64)
            po = cnt["po"] % 2
            cnt["po"] += 1
            for kt in range(nkt):
                pl = cnt["pl"] % 2
                cnt["pl"] += 1
                P.mm(ps_l[pl][:], K_fin[gp, kt * 128:(kt + 1) * 128], Q_fin[gp, :, :].rearrange("p j t -> p (j t)"), True, False,
                     [("K", kt // 4), "Q"], [("ps_l", pl)])
                P.mm(ps_l[pl][:], mb[:, kt * 128:(kt + 1) * 128], I4[:], False, True, ["mb", "I4"], [("ps_l", pl)])
                pe = cnt["pe"] % 2
                cnt["pe"] += 1
                P.op("act", lambda e, pe=pe, pl=pl: e.activation(out=pexp[pe][:], in_=ps_l[pl][:], func=AF.Exp, scale=0.125),
                     [("ps_l", pl)], [("pexp", pe)])
                P.mm(ps_o[po][:65, :], V1[:, kt, g, :], pexp[pe][:], kt == 0, kt == nkt - 1,
                     [("V1", kt // 16), ("V1b", kt // 16), "V1ones", ("pexp", pe)], [("ps_o", po)])
            P.op("act", lambda e, po=po: e.activation(out=o_sb[po][:], in_=ps_o[po][:65, :], func=AF.Copy), [("ps_o", po)], [("o_sb", po)])
            P.mm(ps_den[:], sel[:], o_sb[po][:], True, True, ["sel", ("o_sb", po)], ["ps_den"])
            P.op("dve", lambda e: e.reciprocal(out=rden[:], in_=ps_den[:]), ["ps_den"], ["rden"])
            yi = cnt["ya"] % 2
            cnt["ya"] += 1
            P.op("dve", lambda e, yi=yi, po=po: e.tensor_tensor(out=ya_t[yi][:], in0=o_sb[po][0:64, :], in1=rden[:], op=ALU.mult),
                 [("o_sb", po), "rden"], [("ya_t", yi)])
            fk = ("fin", len(finals))
            finals.append(fk)
            P.dma("sp", ya_v[:, 4 * g:4 * g + 4, qs], ya_t[yi][:].rearrange("p (j t) -> p j t", j=4), [("ya_t", yi)], [fk])
    return P.emit(finals)


RTT = 256
RG_NBLK = RTT // 128
RG_NTT = L // RTT


def build_rg(layer, nsteps=L):
    P = Prog()
    f32 = F32
    rw_r_d = P.dram("rw_r", [64, L])
    rw_k_d = P.dram("rw_k", [64, L])
    rw_v_d = P.dram("rw_v", [64, L])
    rw_lora_d = P.dram("rw_lora", [128, L])
    rw_vall_d = P.dram("rw_vall", [256, L])
    rwc_d = P.dram("rw_cols", [64, 16])
    rw_mul_d = P.dram("rw_mu_lora", [128, 1])
    rw_muv_d = P.dram("rw_mu_vall", [128, 2])
    rw_wup_d = P.dram("rw_wup", [32, 64])
    rw_aup_d = P.dram("rw_aup", [32, 64])
    rw_gup_d = P.dram("rw_gup", [64, 64])
    rw_vdown_d = P.dram("rw_vdown", [128, 2, 32])
    rw_vup_d = P.dram("rw_vup", [32, 64])
    vfirst_in_d = P.dram("vfirst_in", [64, L])
    vfirst_out_d = P.dram("vfirst_out", [64, L], kind="ExternalOutput")
    gd_q_d = P.dram("gd_q", [64, L])
    gd_k_d = P.dram("gd_k", [64, L])
    gd_v_d = P.dram("gd_v", [64, L])
    gd_z_d = P.dram("gd_z", [64, L])
    gd_b_d = P.dram("gd_b", [1, L])
    gd_a_d = P.dram("gd_a", [1, L])
    gdc_d = P.dram("gd_cols", [64, 16])
    ident_d = P.dram("ident", [128, 128])
    yb_d = P.dram("ybT", [64, L], kind="ExternalOutput")
    yc_d = P.dram("ycT", [64, L], kind="ExternalOutput")

    ident = P.sb([128, 128], f32, "ident")
    ones64 = P.sb([64, 64], f32, "ones64")
    ones1 = P.sb([64, 64], f32, "ones1")
    rwc = P.sb([64, 16], f32, "rwc")
    gdc = P.sb([64, 16], f32, "gdc")
    mul = P.sb([128, 1], f32, "mul")
    muv = P.sb([128, 2], f32, "muv")
    wup = P.sb([32, 64], f32, "wup")
    aup = P.sb([64, 64], f32, "aup")
    gup = P.sb([128, 64], f32, "gup")
    vdown = P.sb([128, 2, 32], f32, "vdown")
    vup = P.sb([32, 64], f32, "vup")
    Acol = P.sb([64, 1], f32, "Acol")
    for sbt, dt_, key in ((ident, ident_d, "ident"), (rwc, rwc_d, "rwc"), (gdc, gdc_d, "gdc"), (mul, rw_mul_d, "mul"), (muv, rw_muv_d, "muv"),
                          (wup, rw_wup_d, "wup"), (vup, rw_vup_d, "vup")):
        P.dma("sp", sbt[:], dt_[:, :], [], [key])
    P.dma("sp", vdown[:], rw_vdown_d[:, :, :], [], ["vdown"])
    P.dma("sp", aup[32:64, :], rw_aup_d[:, :], [], ["aup"])
    P.dma("sp", gup[64:128, :], rw_gup_d[:, :], [], ["gup"])
    P.op("dve", lambda e: e.memset(ones64[:], 1.0 / 64), [], ["ones64"])
    P.op("dve", lambda e: e.memset(ones1[:], 1.0), [], ["ones1"])
    P.op("act", lambda e: e.activation(out=Acol[:], in_=gdc[:, 12:13], func=AF.Exp), ["gdc"], ["Acol"])
    nAcol = P.sb([64, 1], f32, "nAcol")
    P.op("dve", lambda e: e.tensor_scalar(out=nAcol[:], in0=Acol[:], scalar1=-1.0, scalar2=None, op0=ALU.mult), ["Acol"], ["nAcol"])

    def T2(shape, name, dt=f32):
        return [P.sb(shape, dt, f"{name}{i}") for i in range(2)]

    NB = 2
    raw_r = T2([64, RTT + 1], "raw_r"); raw_k = T2([64, RTT + 1], "raw_k"); raw_v = T2([64, RTT + 1], "raw_v")
    raw_l = T2([128, RTT + 1], "raw_l"); raw_va = [T2([128, RTT + 1], "raw_va0"), T2([128, RTT + 1], "raw_va1")]
    NTMP = 56
    tmp = [P.sb([128, RTT], f32, f"tmp{i}") for i in range(NTMP)]
    r_t = T2([64, RTT], "r_t"); w_t = T2([64, RTT], "w_t"); a_t = T2([64, RTT], "a_t"); b_t = T2([64, RTT], "b_t")
    g_t = T2([64, RTT], "g_t"); v_t = T2([64, RTT], "v_t"); km_t = T2([64, RTT], "km_t"); bon_t = T2([64, RTT], "bon_t")
    ktok_r = T2([128, RG_NBLK, 64], "ktok_r"); vtok_r = T2([128, RG_NBLK, 64], "vtok_r")
    graw_q = T2([64, RTT + 3], "graw_q"); graw_k = T2([64, RTT + 3], "graw_k"); graw_v = T2([64, RTT + 3], "graw_v")
    gz = T2([64, RTT], "gz"); gb = T2([64, RTT], "gb"); ga = T2([64, RTT], "ga")
    gq_t = T2([64, RTT], "gq_t"); gk_t = T2([64, RTT], "gk_t"); gal_t = T2([64, RTT], "gal_t"); gnb_t = T2([64, RTT], "gnb_t")
    gv_t = T2([64, RTT], "gv_t"); gkb_t = T2([64, RTT], "gkb_t")
    ktok_g = T2([128, RG_NBLK, 64], "ktok_g"); vtok_g = T2([128, RG_NBLK, 64], "vtok_g")
    ytile = T2([64, RTT], "ytile"); otile = T2([64, RTT], "otile")
    yout = T2([64, RTT], "yout"); oout = T2([64, RTT], "oout")
    vmask = {"r": [P.sb([128, 64], f32, f"vmask_r{i}") for i in range(3)], "g": [P.sb([128, 64], f32, f"vmask_g{i}") for i in range(3)]}
    arep = {"r": [P.sb([64, 64], f32, f"arep_r{i}") for i in range(3)], "g": [P.sb([64, 64], f32, f"arep_g{i}") for i in range(3)]}
    St = {"r": [P.sb([64, 64], f32, f"St_r{i}") for i in range(3)], "g": [P.sb([64, 64], f32, f"St_g{i}") for i in range(3)]}
    Stmp = {"r": [P.sb([64, 64], f32, f"Stmp_r{i}") for i in range(2)], "g": [P.sb([64, 64], f32, f"Stmp_g{i}") for i in range(2)]}

    ps_kv = {"r": P.ps([64, 512], f32, "ps_kv_r"), "g": P.ps([64, 512], f32, "ps_kv_g")}
    ps_sa = {"r": P.ps([64, 512], f32, "ps_sa_r"), "g": P.ps([64, 512], f32, "ps_sa_g")}
    ps_y1 = {"r": P.ps([64, 512], f32, "ps_yr"), "g": P.ps([64, 512], f32, "ps_yg")}
    ps_y = {"r": [ps_y1["r"], ps_y1["r"]], "g": [ps_y1["g"], ps_y1["g"]]}
    ps_p = [P.ps([128, 512], f32, f"ps_p{i}") for i in range(2)]
    cnt = dict(pp=0, tmp=0, kv=0, sa=0)
    finals = []

    def fin():
        k = ("fin", len(finals))
        finals.append(k)
        return k

    for kind in ("r", "g"):
        P.op("dve", lambda e, kind=kind: e.memset(St[kind][2][:], 0.0), [], [("St", kind, 2)])

    def newtmp():
        i = cnt["tmp"]
        cnt["tmp"] += 1
        assert i < NTMP
        return tmp[i], ("tmp", i)

    def newps():
        i = cnt["pp"] % 2
        cnt["pp"] += 1
        return ps_p[i][:, 0:RTT], ("ps_p", i)

    def bcast_sum(dst, dkey, src, skey, lhs, lkey, rows=64):
        P.mm(dst, lhs, src, True, True, [skey, lkey], [dkey])

    def rsqrt_from_ps(out_ap, okey, ps_ap, pkey, eps):
        P.op("act", lambda e: e.activation(out=out_ap, in_=ps_ap, func=AF.Sqrt, bias=eps, scale=1.0), [pkey], [okey])
        P.op("dve", lambda e: e.reciprocal(out=out_ap, in_=out_ap), [okey], [okey])

    def transpose_to_tok(dst, dkey, srcT, skey):
        ps_t, ptk = newps()
        for bq in range(RG_NBLK):
            P.mm(ps_t[:, bq * 64:(bq + 1) * 64], srcT[:, bq * 128:(bq + 1) * 128], ident[0:64, 0:64], True, True, [skey, "ident"], [ptk])
        P.op("act", lambda e: e.activation(out=dst[:].rearrange("p a b -> p (a b)"), in_=ps_t[:, 0:RG_NBLK * 64], func=AF.Copy), [ptk], [dkey])

    def shift_mix(raw, rkey, mu_ap, mukey, out_ap, okey, rows):
        t, tk = newtmp()
        P.op("dve", lambda e: e.tensor_tensor(out=t[:rows, :], in0=raw[:rows, 0:RTT], in1=raw[:rows, 1:RTT + 1], op=ALU.subtract), [rkey], [tk])
        P.op("dve", lambda e: e.scalar_tensor_tensor(out=out_ap, in0=t[:rows, :], scalar=mu_ap, in1=raw[:rows, 1:RTT + 1], op0=ALU.mult, op1=ALU.add),
             [tk, rkey, mukey], [okey])

    def load_shift(raw, key, src_d, t0, rows, npre):
        if t0 == 0:
            P.op("dve", lambda e: e.memset(raw[:rows, 0:npre], 0.0), [], [key + ("z",)])
            P.dma("sp", raw[:rows, npre:npre + RTT], src_d[:, 0:RTT], [key + ("z",)], [key])
        else:
            P.dma("sp", raw[:rows, :], src_d[:, t0 - npre:t0 + RTT], [], [key])

    def prep_rwkv(n):
        bi = n % 2
        t0 = n * RTT
        ts = slice(t0, t0 + RTT)
        K = lambda nm: ("rw", nm, bi)
        load_shift(raw_r[bi], K("raw_r"), rw_r_d, t0, 64, 1)
        load_shift(raw_k[bi], K("raw_k"), rw_k_d, t0, 64, 1)
        load_shift(raw_v[bi], K("raw_v"), rw_v_d, t0, 64, 1)
        load_shift(raw_l[bi], K("raw_l"), rw_lora_d, t0, 128, 1)
        shift_mix(raw_r[bi], K("raw_r"), rwc[:, 0:1], "rwc", r_t[bi][:], K("r"), 64)
        kx, kxk = newtmp()
        shift_mix(raw_k[bi], K("raw_k"), rwc[:, 1:2], "rwc", kx[:64, :], kxk, 64)
        shift_mix(raw_v[bi], K("raw_v"), rwc[:, 2:3], "rwc", v_t[bi][:], K("v"), 64)
        lo, lok = newtmp()
        shift_mix(raw_l[bi], K("raw_l"), mul[:, 0:1], "mul", lo[:, :], lok, 128)
        th, thk = newtmp()
        P.op("act", lambda e: e.activation(out=th[0:32, :], in_=lo[0:32, :], func=AF.Tanh), [lok], [thk])
        ps, psk = newps()
        P.mm(ps[0:64, :], wup[:], th[0:32, :], True, True, ["wup", thk], [psk])
        e1, e1k = newtmp()
        nw0, nw0k = newtmp()
        P.op("dve", lambda e: e.tensor_scalar(out=nw0[0:64, 0:1], in0=rwc[:, 3:4], scalar1=-1.0, scalar2=None, op0=ALU.mult), ["rwc"], [nw0k])
        P.op("act", lambda e: e.activation(out=e1[0:64, :], in_=ps[0:64, :], func=AF.Exp, bias=nw0[0:64, 0:1], scale=-1.0), [psk, nw0k], [e1k])
        P.op("act", lambda e: e.activation(out=e1[0:64, :], in_=e1[0:64, :], func=AF.Ln, bias=1.0, scale=1.0), [e1k], [e1k])
        P.op("act", lambda e: e.activation(out=e1[0:64, :], in_=e1[0:64, :], func=AF.Exp, bias=-0.5, scale=-1.0), [e1k], [e1k])
        P.op("act", lambda e: e.activation(out=w_t[bi][:], in_=e1[0:64, :], func=AF.Exp, scale=-1.0), [e1k], [K("w")])
        ps2, ps2k = newps()
        P.mm(ps2[0:64, :], aup[32:64, :], lo[32:64, :], True, True, ["aup", lok], [ps2k])
        asg, asgk = newtmp()
        P.op("act", lambda e: e.activation(out=asg[0:64, :], in_=ps2[0:64, :], func=AF.Sigmoid, bias=rwc[:, 4:5], scale=1.0), [ps2k, "rwc"], [asgk])
        sg_, sgk = newtmp()
        P.op("act", lambda e: e.activation(out=sg_[64:128, :], in_=lo[64:128, :], func=AF.Sigmoid), [lok], [sgk])
        ps3, ps3k = newps()
        P.mm(ps3[0:64, :], gup[64:128, :], sg_[64:128, :], True, True, ["gup", sgk], [ps3k])
        P.op("act", lambda e: e.activation(out=g_t[bi][:], in_=ps3[0:64, :], func=AF.Copy), [ps3k], [K("g")])
        return dict(bi=bi, ts=ts, kx=kx, kxk=kxk, asg=asg, asgk=asgk, lo=lo, lok=lok)

    def prep_rwkv2(n, c):
        bi = c["bi"]
        t0 = n * RTT
        ts = c["ts"]
        K = lambda nm: ("rw", nm, bi)
        kx, kxk, asg, asgk = c["kx"], c["kxk"], c["asg"], c["asgk"]
        kk, kkk = newtmp()
        P.op("dve", lambda e: e.tensor_scalar(out=kk[0:64, :], in0=kx[0:64, :], scalar1=rwc[:, 5:6], scalar2=None, op0=ALU.mult), [kxk, "rwc"], [kkk])
        sq, sqk = newtmp()
        P.op("act", lambda e: e.activation(out=sq[0:64, :], in_=kk[0:64, :], func=AF.Square), [kkk], [sqk])
        ps, psk = newps()
        P.mm(ps[0:64, :], ones1[:], sq[0:64, :], True, True, ["ones1", sqk], [psk])
        rsq, rsqk = sq, sqk
        rsqrt_from_ps(rsq[0:64, :], rsqk, ps[0:64, :], psk, 1e-12)
        P.op("dve", lambda e: e.tensor_tensor(out=kk[0:64, :], in0=kk[0:64, :], in1=rsq[0:64, :], op=ALU.mult), [kkk, rsqk], [kkk])
        P.op("dve", lambda e: e.tensor_scalar(out=a_t[bi][:], in0=kk[0:64, :], scalar1=-1.0, scalar2=None, op0=ALU.mult), [kkk], [K("a")])
        P.op("dve", lambda e: e.tensor_tensor(out=b_t[bi][:], in0=kk[0:64, :], in1=asg[0:64, :], op=ALU.mult), [kkk, asgk], [K("b")])
        f, fk = newtmp()
        P.op("dve", lambda e: e.tensor_scalar(out=f[0:64, :], in0=asg[0:64, :], scalar1=-1.0, scalar2=rwc[:, 6:7], op0=ALU.add, op1=ALU.mult), [asgk, "rwc"], [fk])
        P.op("dve", lambda e: e.scalar_tensor_tensor(out=km_t[bi][:], in0=f[0:64, :], scalar=1.0, in1=kx[0:64, :], op0=ALU.add, op1=ALU.mult), [fk, kxk], [K("km")])
        if layer == 0:
            fk2 = fin()
            P.dma("sp", vfirst_out_d[:, ts], v_t[bi][:], [K("v")], [fk2])
        else:
            vas = []
            for h2 in range(2):
                key = K(f"raw_va{h2}")
                src = rw_vall_d[h2 * 128:(h2 + 1) * 128, :]
                if t0 == 0:
                    P.op("dve", lambda e, h2=h2: e.memset(raw_va[h2][bi][:, 0:1], 0.0), [], [key + ("z",)])
                    P.dma("sp", raw_va[h2][bi][:, 1:1 + RTT], src[:, 0:RTT], [key + ("z",)], [key])
                else:
                    P.dma("sp", raw_va[h2][bi][:, :], src[:, t0 - 1:t0 + RTT], [], [key])
                va, vak = newtmp()
                shift_mix(raw_va[h2][bi], key, muv[:, h2:h2 + 1], "muv", va[:, :], vak, 128)
                vas.append((va, vak))
            ps4, ps4k = newps()
            for h2 in range(2):
                P.mm(ps4[0:32, :], vdown[:, h2, :], vas[h2][0][:, :], h2 == 0, h2 == 1, ["vdown", vas[h2][1]], [ps4k])
            t32, t32k = newtmp()
            P.op("act", lambda e: e.activation(out=t32[0:32, :], in_=ps4[0:32, :], func=AF.Copy), [ps4k], [t32k])
            ps5, ps5k = newps()
            P.mm(ps5[0:64, :], vup[:], t32[0:32, :], True, True, ["vup", t32k], [ps5k])
            sgv, sgvk = newtmp()
            P.op("act", lambda e: e.activation(out=sgv[0:64, :], in_=ps5[0:64, :], func=AF.Sigmoid, bias=rwc[:, 10:11], scale=1.0), [ps5k, "rwc"], [sgvk])
            vf, vfk = newtmp()
            P.dma("sp", vf[0:64, :], vfirst_in_d[:, ts], [], [vfk])
            P.op("dve", lambda e: e.tensor_tensor(out=vf[0:64, :], in0=vf[0:64, :], in1=v_t[bi][:], op=ALU.subtract), [vfk, K("v")], [vfk])
            P.op("dve", lambda e: e.tensor_tensor(out=vf[0:64, :], in0=vf[0:64, :], in1=sgv[0:64, :], op=ALU.mult), [vfk, sgvk], [vfk])
            P.op("dve", lambda e: e.tensor_tensor(out=v_t[bi][:], in0=v_t[bi][:], in1=vf[0:64, :], op=ALU.add), [vfk, K("v")], [K("v")])
            fk2 = fin()
            P.dma("sp", vfirst_out_d[:, ts], v_t[bi][:], [K("v")], [fk2])
        rk, rkk = newtmp()
        P.op("dve", lambda e: e.scalar_tensor_tensor(out=rk[0:64, :], in0=r_t[bi][:], scalar=rwc[:, 7:8], in1=km_t[bi][:], op0=ALU.mult, op1=ALU.mult),
             [K("r"), K("km"), "rwc"], [rkk])
        ps6, ps6k = newps()
        P.mm(ps6[0:64, :], ones1[:], rk[0:64, :], True, True, ["ones1", rkk], [ps6k])
        P.op("dve", lambda e: e.tensor_tensor(out=bon_t[bi][:], in0=ps6[0:64, :], in1=v_t[bi][:], op=ALU.mult), [ps6k, K("v")], [K("bon")])
        transpose_to_tok(ktok_r[bi], K("ktok"), km_t[bi], K("km"))
        transpose_to_tok(vtok_r[bi], K("vtok"), v_t[bi], K("v"))

    def prep_gdn(n):
        bi = n % 2
        t0 = n * RTT
        ts = slice(t0, t0 + RTT)
        K = lambda nm: ("gd", nm, bi)
        load_shift(graw_q[bi], K("raw_q"), gd_q_d, t0, 64, 3)
        load_shift(graw_k[bi], K("raw_k"), gd_k_d, t0, 64, 3)
        load_shift(graw_v[bi], K("raw_v"), gd_v_d, t0, 64, 3)
        P.dma("sp", gz[bi][:], gd_z_d[:, ts], [], [K("z")])
        P.dma("sp", gb[bi][:], gd_b_d[:, ts].partition_broadcast(64), [], [K("b")])
        P.dma("sp", ga[bi][:], gd_a_d[:, ts].partition_broadcast(64), [], [K("a")])
        outs = {}
        for gi_, (raw, rk_) in enumerate(((graw_q[bi], K("raw_q")), (graw_k[bi], K("raw_k")), (graw_v[bi], K("raw_v")))):
            cv, cvk = newtmp()
            P.op("dve", lambda e, raw=raw, cv=cv, gi_=gi_: e.tensor_scalar(out=cv[0:64, :], in0=raw[:, 0:RTT], scalar1=gdc[:, 4 * gi_:4 * gi_ + 1], scalar2=None, op0=ALU.mult),
                 [rk_, "gdc"], [cvk])
            for i in range(1, 4):
                P.op("dve", lambda e, raw=raw, cv=cv, gi_=gi_, i=i: e.scalar_tensor_tensor(out=cv[0:64, :], in0=raw[:, i:i + RTT], scalar=gdc[:, 4 * gi_ + i:4 * gi_ + i + 1],
                                                                                      in1=cv[0:64, :], op0=ALU.mult, op1=ALU.add),
                     [rk_, "gdc", cvk], [cvk])
            P.op("act", lambda e, cv=cv: e.activation(out=cv[0:64, :], in_=cv[0:64, :], func=AF.Silu), [cvk], [cvk])
            outs[gi_] = (cv, cvk)
        for gi_, scale_, dst, dk in ((0, 0.125, gq_t[bi], K("q")), (1, 1.0, gk_t[bi], K("k"))):
            cv, cvk = outs[gi_]
            sq, sqk = newtmp()
            P.op("act", lambda e, cv=cv, sq=sq: e.activation(out=sq[0:64, :], in_=cv[0:64, :], func=AF.Square), [cvk], [sqk])
            ps, psk = newps()
            P.mm(ps[0:64, :], ones1[:], sq[0:64, :], True, True, ["ones1", sqk], [psk])
            rsqrt_from_ps(sq[0:64, :], sqk, ps[0:64, :], psk, 1e-6)
            P.op("dve", lambda e, cv=cv, sq=sq, dst=dst, scale_=scale_: e.scalar_tensor_tensor(out=dst[:], in0=cv[0:64, :], scalar=scale_, in1=sq[0:64, :], op0=ALU.mult, op1=ALU.mult),
                 [cvk, sqk], [dk])
        cvv, cvvk = outs[2]
        P.op("act", lambda e: e.activation(out=gv_t[bi][:], in_=cvv[0:64, :], func=AF.Copy), [cvvk], [K("v")])
        P.op("act", lambda e: e.activation(out=gb[bi][:], in_=gb[bi][:], func=AF.Sigmoid), [K("b")], [K("b")])
        P.op("act", lambda e: e.activation(out=ga[bi][:], in_=ga[bi][:], func=AF.Exp, bias=gdc[:, 13:14], scale=1.0), [K("a"), "gdc"], [K("a")])
        P.op("act", lambda e: e.activation(out=ga[bi][:], in_=ga[bi][:], func=AF.Ln, bias=1.0, scale=1.0), [K("a")], [K("a")])
        P.op("act", lambda e: e.activation(out=gal_t[bi][:], in_=ga[bi][:], func=AF.Exp, scale=nAcol[:, 0:1]), [K("a"), "nAcol"], [K("al")])
        P.op("dve", lambda e: e.tensor_tensor(out=gkb_t[bi][:], in0=gk_t[bi][:], in1=gb[bi][:], op=ALU.mult), [K("k"), K("b")], [K("kb")])
        P.op("dve", lambda e: e.scalar_tensor_tensor(out=gnb_t[bi][:], in0=gkb_t[bi][:], scalar=-1.0, in1=gal_t[bi][:], op0=ALU.mult, op1=ALU.mult),
             [K("kb"), K("al")], [K("nb")])
        transpose_to_tok(ktok_g[bi], K("ktok"), gkb_t[bi], K("kb"))
        transpose_to_tok(vtok_g[bi], K("vtok"), gv_t[bi], K("v"))
        P.op("act", lambda e: e.activation(out=gz[bi][:], in_=gz[bi][:], func=AF.Silu), [K("z")], [K("z")])

    def scan_step(kind, t, aT, akey, wT, wkey, bT, bkey, ktok, kkey, vtok, vkey, rT, rkey, psy, pykey):
        tl = t % RTT
        blk, tb = tl // 128, tl % 128
        prev = (t - 1) % 3
        cur = t % 3
        vm = vmask[kind][t % 3]
        vmk = ("vmask", kind, t % 3)
        ON = "vks12y"
        if "v" in ON:
            P.op("act", lambda e: e.activation(out=vm[:], in_=vtok[:, blk, :], func=AF.Copy, scale=ident[:, tb:tb + 1]), [vkey, "ident"], [vmk])
        kvs = "kv" + kind
        sas = "sa" + kind
        kv_ps = ps_kv[kind][:, 0:64]
        sa_ps = ps_sa[:, 0:64] if False else ps_sa[kind][:, 0:64]
        if "k" in ON:
            P.mm(kv_ps, ktok[:, blk, :], vm[:], True, True, [kkey, vmk], [("ps_s", kvs)])
        if "s" in ON:
            ar = arep[kind][t % 3]
            ark = ("arep", kind, t % 3)
            P.op("act", lambda e: e.activation(out=ar[:], in_=ones1[:], func=AF.Copy, scale=aT[:, tl:tl + 1]), [akey, "ones1"], [ark])
            P.mm(sa_ps, ar[:], St[kind][prev][:], True, True, [ark, ("St", kind, prev)], [("ps_s", sas)])
        stm = Stmp[kind][t % 2]
        stk = ("Stmp", kind, t % 2)
        if "1" in ON:
            P.op("dve", lambda e: e.scalar_tensor_tensor(out=stm[:], in0=St[kind][prev][:], scalar=wT[:, tl:tl + 1], in1=kv_ps, op0=ALU.mult, op1=ALU.add),
                 [("St", kind, prev), wkey, ("ps_s", kvs)], [stk])
        if "2" in ON:
            P.op("dve", lambda e: e.scalar_tensor_tensor(out=St[kind][cur][:], in0=sa_ps, scalar=bT[:, tl:tl + 1], in1=stm[:], op0=ALU.mult, op1=ALU.add),
                 [("ps_s", sas), bkey, stk], [("St", kind, cur)])
        if "y" in ON:
            P.mm(psy[:, tl:tl + 1], St[kind][cur][:], rT[:, tl:tl + 1], True, True, [("St", kind, cur), rkey], [pykey])

    def post_rwkv(n):
        bi = n % 2
        ts = slice(n * RTT, (n + 1) * RTT)
        K = lambda nm: ("rw", nm, bi)
        y = ytile[bi]
        P.op("act", lambda e: e.activation(out=y[:], in_=ps_y["r"][bi][:, 0:RTT], func=AF.Copy), [("ps_y", "r", 0)], [K("y")])
        ps, psk = newps()
        P.mm(ps[0:64, :], ones64[:], y[:], True, True, ["ones64", K("y")], [psk])
        P.op("dve", lambda e: e.tensor_tensor(out=y[:], in0=y[:], in1=ps[0:64, :], op=ALU.subtract), [K("y"), psk], [K("y")])
        sq, sqk = newtmp()
        P.op("act", lambda e: e.activation(out=sq[0:64, :], in_=y[:], func=AF.Square), [K("y")], [sqk])
        ps2, ps2k = newps()
        P.mm(ps2[0:64, :], ones64[:], sq[0:64, :], True, True, ["ones64", sqk], [ps2k])
        rsqrt_from_ps(sq[0:64, :], sqk, ps2[0:64, :], ps2k, 64e-5)
        P.op("dve", lambda e: e.tensor_tensor(out=y[:], in0=y[:], in1=sq[0:64, :], op=ALU.mult), [K("y"), sqk], [K("y")])
        P.op("dve", lambda e: e.tensor_scalar(out=y[:], in0=y[:], scalar1=rwc[:, 8:9], scalar2=rwc[:, 9:10], op0=ALU.mult, op1=ALU.add), [K("y"), "rwc"], [K("y")])
        P.op("dve", lambda e: e.tensor_tensor(out=y[:], in0=y[:], in1=bon_t[bi][:], op=ALU.add), [K("y"), K("bon")], [K("y")])
        P.op("dve", lambda e: e.tensor_tensor(out=yout[bi][:], in0=y[:], in1=g_t[bi][:], op=ALU.mult), [K("y"), K("g")], [K("yout")])
        P.dma("sp", yb_d[:, ts], yout[bi][:], [K("yout")], [fin()])

    def post_gdn(n):
        bi = n % 2
        ts = slice(n * RTT, (n + 1) * RTT)
        K = lambda nm: ("gd", nm, bi)
        o = otile[bi]
        P.op("act", lambda e: e.activation(out=o[:], in_=ps_y["g"][bi][:, 0:RTT], func=AF.Copy), [("ps_y", "g", 0)], [K("o")])
        sq, sqk = newtmp()
        P.op("act", lambda e: e.activation(out=sq[0:64, :], in_=o[:], func=AF.Square), [K("o")], [sqk])
        ps, psk = newps()
        P.mm(ps[0:64, :], ones64[:], sq[0:64, :], True, True, ["ones64", sqk], [psk])
        rsqrt_from_ps(sq[0:64, :], sqk, ps[0:64, :], psk, 1e-6)
        P.op("dve", lambda e: e.scalar_tensor_tensor(out=o[:], in0=o[:], scalar=gdc[:, 14:15], in1=sq[0:64, :], op0=ALU.mult, op1=ALU.mult), [K("o"), sqk, "gdc"], [K("o")])
        P.op("dve", lambda e: e.tensor_tensor(out=oout[bi][:], in0=o[:], in1=gz[bi][:], op=ALU.mult), [K("o"), K("z")], [K("oout")])
        P.dma("sp", yc_d[:, ts], oout[bi][:], [K("oout")], [fin()])

    ntiles = (nsteps + RTT - 1) // RTT
    for n in range(ntiles):
        cnt["tmp"] = 0
        c = prep_rwkv(n)
        prep_rwkv2(n, c)
        prep_gdn(n)
        bi = n % 2
        Kr = lambda nm: ("rw", nm, bi)
        Kg = lambda nm: ("gd", nm, bi)
        for tl in range(min(RTT, nsteps - n * RTT)):
            t = n * RTT + tl
            scan_step("r", t, a_t[bi], Kr("a"), w_t[bi], Kr("w"), b_t[bi], Kr("b"), ktok_r[bi], Kr("ktok"), vtok_r[bi], Kr("vtok"), r_t[bi], Kr("r"),
                      ps_y["r"][bi], ("ps_y", "r", 0))
            scan_step("g", t, gk_t[bi], Kg("k"), gal_t[bi], Kg("al"), gnb_t[bi], Kg("nb"), ktok_g[bi], Kg("ktok"), vtok_g[bi], Kg("vtok"), gq_t[bi], Kg("q"),
                      ps_y["g"][bi], ("ps_y", "g", 0))
        post_rwkv(n)
        post_gdn(n)
    return P.emit(finals)


NB = 64

def core_blocks(c):
    j = c % 4
    out = []
    for k in range(8):
        out += [8 * k + j, 8 * k + 7 - j]
    return out

def core_tokens(c):
    b = c // 4
    pos = np.concatenate([np.arange(g * 128, (g + 1) * 128) for g in core_blocks(c)])
    return b, pos

def fm_cols():
    cols = []
    for j in range(4):
        cols += list(range(64 * j, 64 * j + 64)) + list(range(64 * (j + 4), 64 * (j + 4) + 64))
    cols += list(range(512, 640))
    cols += list(range(768, 1280))
    cols += list(range(1280, 1344))
    cols += list(range(1352, 2248))
    g0 = 2248
    for h in range(4):
        cols += list(range(g0 + 64 * h, g0 + 64 * h + 64)) + list(range(g0 + 256 + 64 * h, g0 + 256 + 64 * h + 64))
    cols += list(range(g0 + 512, g0 + 768))
    assert len(cols) == 2880
    return cols

def tok_cols():
    cols = list(range(640, 768)) + list(range(1344, 1352)) + list(range(3272, 3276)) + list(range(3276, 3280)) + list(range(3016, 3272))
    assert len(cols) == 400
    return cols

def gcol(v):
    return np.ascontiguousarray(v.reshape(8, 128).T)

def core_blocks(c):
    j = c % 4
    return [4 * m + j for m in range(16)]

def tri4_for(c):
    j = c % 4
    q = np.arange(128)[:, None]
    col = np.arange(512)[None, :]
    blk, w = col // 128, col % 128
    valid = (blk < j) | ((blk == j) & (w <= q))
    return np.where(valid, 0.0, -1e30).astype(np.float32)

def q_pair_rows():
    rows = []
    for j in range(4):
        rows += list(range(64 * j, 64 * j + 64)) + list(range(64 * (j + 4), 64 * (j + 4) + 64))
    return rows

def rg_inputs(d, p_b, c, l, vfirst=None):
    j = c % 4
    hs = slice(64 * j, 64 * j + 64)
    pb = p_b[:, 1352:2248]
    pc = p_b[:, 2248:3280]
    T = lambda a: np.ascontiguousarray(a.T.astype(np.float32))
    mu = d["rwkv_mu"][l]
    rwc = np.zeros((64, 16), np.float32)
    rwc[:, 0] = mu[0:256][hs]; rwc[:, 1] = mu[256:512][hs]; rwc[:, 2] = mu[512:768][hs]
    rwc[:, 3] = d["rwkv_w0"][l][hs]; rwc[:, 4] = d["rwkv_a0"][l][hs]; rwc[:, 5] = d["rwkv_k_k"][l][hs]; rwc[:, 6] = d["rwkv_k_a"][l][hs]
    rwc[:, 7] = d["rwkv_r_k"][l][j]; rwc[:, 8] = d["rwkv_gn_w"][l][hs]; rwc[:, 9] = d["rwkv_gn_b"][l][hs]
    if l >= 1:
        rwc[:, 10] = d["rwkv_v0"][l - 1][hs]
        vdown = np.ascontiguousarray(d["rwkv_v_down"][l - 1].reshape(2, 128, 32).transpose(1, 0, 2))
        vup = np.ascontiguousarray(d["rwkv_v_up"][l - 1][:, hs])
    else:
        vdown = np.zeros((128, 2, 32), np.float32)
        vup = np.zeros((32, 64), np.float32)
    gdc = np.zeros((64, 16), np.float32)
    conv = d["gdn_conv"][l]
    for gi in range(3):
        for i in range(4):
            gdc[:, 4 * gi + i] = conv[i, gi * 256 + 64 * j: gi * 256 + 64 * j + 64]
    gdc[:, 12] = d["gdn_a_log"][l][j]; gdc[:, 13] = d["gdn_dt_bias"][l][j]; gdc[:, 14] = d["gdn_o_norm"][l]
    L = p_b.shape[0]
    return dict(
        rw_r=T(pb[:, 0:256][:, hs]), rw_k=T(pb[:, 256:512][:, hs]), rw_v=T(pb[:, 512:768][:, hs]), rw_lora=T(pb[:, 768:896]), rw_vall=T(pb[:, 512:768]),
        rw_cols=rwc, rw_mu_lora=mu[768:896][:, None].copy(), rw_mu_vall=np.ascontiguousarray(mu[512:768].reshape(2, 128).T),
        rw_wup=np.ascontiguousarray(d["rwkv_w_up"][l][:, hs]), rw_aup=np.ascontiguousarray(d["rwkv_a_up"][l][:, hs]), rw_gup=np.ascontiguousarray(d["rwkv_g_up"][l][:, hs]),
        rw_vdown=vdown, rw_vup=vup, vfirst_in=(vfirst if vfirst is not None else np.zeros((64, L), np.float32)),
        gd_q=T(pc[:, 0:256][:, hs]), gd_k=T(pc[:, 256:512][:, hs]), gd_v=T(pc[:, 512:768][:, hs]), gd_z=T(pc[:, 768:1024][:, hs]),
        gd_b=np.ascontiguousarray(pc[:, 1024 + j][None, :]), gd_a=np.ascontiguousarray(pc[:, 1028 + j][None, :]), gd_cols=gdc,
        ident=np.eye(128, dtype=np.float32))


_PROGS = {}


def _prog(name, builder):
    if name not in _PROGS:
        _PROGS[name] = builder()
    return _PROGS[name]


def _mixers(d, l, pT_list, ptok_list, vfirst_list):
    fm, tk = fm_cols(), tok_cols()
    p_full = [np.zeros((L, 3280), np.float32) for _ in range(2)]
    for c in range(8):
        b, pos = core_tokens(c)
        tmp = np.zeros((2048, 3280), np.float32)
        tmp[:, fm] = pT_list[c].T
        tmp[:, tk] = ptok_list[c]
        p_full[b][pos] = tmp
    C = dsa_consts()
    maps = []
    for c in range(8):
        b, pos = core_tokens(c)
        pa = p_full[b]
        maps.append(dict(kT=np.ascontiguousarray(pa[:, 512:640].T), kiT=np.ascontiguousarray(pa[:, 1280:1344].T), vtok=np.ascontiguousarray(pa[:, 640:768]),
                         qT=np.ascontiguousarray(pa[pos][:, q_pair_rows()].T), qiT=np.ascontiguousarray(pa[pos][:, 768:1280].T),
                         wi=np.ascontiguousarray(pa[pos][:, 1344:1352]),
                         cosT=C["cosT"], sinT=C["sinT"], cosq=np.ascontiguousarray(C["cosT"][:, pos]), sinq=np.ascontiguousarray(C["sinT"][:, pos]),
                         gq=np.tile(d["att_q_norm"][l], 2)[:, None].copy(), gk=np.tile(d["att_k_norm"][l], 2)[:, None].copy(),
                         Rm=C["Rm"], tri4=tri4_for(c), I4=C["I4"], sel=C["sel"], bo=C["bo"], pw=C["pw"]))
    res_a = run(_prog("dsa", build_dsa), maps)
    maps = [rg_inputs(d, p_full[c // 4], c, l, vfirst_list[c] if vfirst_list is not None else None) for c in range(8)]
    res_r = run(_prog(f"rg{min(l, 1)}", lambda: build_rg(min(l, 1))), maps)
    yT = []
    for c in range(8):
        b, pos = core_tokens(c)
        y = np.zeros((1024, 2048), np.float32)
        y[0:512] = res_a[c]["yaT"]
        for jj in range(4):
            y[512 + 64 * jj:512 + 64 * jj + 64] = res_r[4 * b + jj]["ybT"][:, pos]
            y[768 + 64 * jj:768 + 64 * jj + 64] = res_r[4 * b + jj]["ycT"][:, pos]
        yT.append(y)
    return yT, [res_r[c]["vfirst_out"] for c in range(8)]


def kernel(**inputs):
    d = {k: np.asarray(v) for k, v in inputs.items()}
    perm = fm_cols() + tok_cols()
    x = d["x"]

    def ffn_w(l, i, tag):
        return {f"g_{tag}": gcol(d["ffn_norm"][l, i]), f"wg_{tag}": np.ascontiguousarray(d["ffn_w_gate"][l, i]),
                f"wu_{tag}": np.ascontiguousarray(d["ffn_w_up"][l, i]), f"wd_{tag}": np.ascontiguousarray(d["ffn_w_down"][l, i])}

    def inp_w(l, tag):
        return {f"g_{tag}": gcol(d["mix_norm"][l]), f"win_{tag}": np.ascontiguousarray(d["w_in"][l][:, perm])}

    w0 = {**ffn_w(0, 0, "f00"), **inp_w(0, "i0")}
    maps = []
    for c in range(8):
        b, pos = core_tokens(c)
        maps.append({"xT": np.ascontiguousarray(x[b, pos, :].T), **w0})
    r0 = run(_prog("P0", lambda: build_P([("ffn", "f00"), ("inproj", "i0")])), maps)
    yT, vfirst = _mixers(d, 0, [r0[c]["pT_i0"] for c in range(8)], [r0[c]["ptok_i0"] for c in range(8)], None)
    w1 = {"wo_o0": np.ascontiguousarray(d["w_out"][0]), **ffn_w(0, 1, "f01"), **ffn_w(1, 0, "f10"), **inp_w(1, "i1")}
    maps = [{"xT": r0[c]["xo"], "yT_o0": yT[c], **w1} for c in range(8)]
    r1 = run(_prog("P1", lambda: build_P([("out", "o0"), ("ffn", "f01"), ("ffn", "f10"), ("inproj", "i1")])), maps)
    yT, _ = _mixers(d, 1, [r1[c]["pT_i1"] for c in range(8)], [r1[c]["ptok_i1"] for c in range(8)], vfirst)
    w2 = {"wo_o1": np.ascontiguousarray(d["w_out"][1]), **ffn_w(1, 1, "f11")}
    maps = [{"xT": r1[c]["xo"], "yT_o1": yT[c], **w2} for c in range(8)]
    r2 = run(_prog("P2", lambda: build_P([("out", "o1"), ("ffn", "f11")])), maps)
    out = np.zeros((2, L, 1024), np.float32)
    for c in range(8):
        b, pos = core_tokens(c)
        out[b, pos, :] = r2[c]["xo"].T
    return out
```

```python
import numpy as np
L = 8192
import concourse.bass as bass
import concourse.mybir as mybir
from concourse.bass_utils import run_bass_kernel_spmd

F32 = mybir.dt.float32
BF16 = mybir.dt.bfloat16
ALU = mybir.AluOpType
AF = mybir.ActivationFunctionType
AX = mybir.AxisListType

ENGS = ("pe", "act", "dve", "pool", "sp")
NDMASEM = 8


class Prog:
    def __init__(self, name="k"):
        self.nc = bass.Bass("TRN2", target_bir_lowering=False)
        self.ops = []
        self.last_write = {}
        self.readers = {}
        self.n_sb = 0
        self.n_ps = 0

    def dram(self, name, shape, dtype=F32, kind="ExternalInput"):
        return self.nc.dram_tensor(name, list(shape), dtype, kind=kind).ap()

    def sb(self, shape, dtype=F32, name=None):
        self.n_sb += 1
        return self.nc.alloc_sbuf_tensor("s_" + (name or f"sb{self.n_sb}"), list(shape), dtype)

    def ps(self, shape, dtype=F32, name=None):
        self.n_ps += 1
        return self.nc.alloc_psum_tensor("p_" + (name or f"ps{self.n_ps}"), list(shape), dtype)

    def op(self, eng, fn, reads=(), writes=(), dma=False):
        import sys, threading
        st = getattr(self, "_stepper", None)
        if st is not None and threading.current_thread() is st.thread:
            st.go.acquire()
            try:
                return self._op(eng, fn, reads, writes, dma, sys._getframe(1))
            finally:
                st.done.release()
        return self._op(eng, fn, reads, writes, dma, sys._getframe(1))

    def _op(self, eng, fn, reads, writes, dma, fr):
        import sys
        if fr.f_code.co_name in ("mm", "dma"):
            fr = fr.f_back
        self._line = fr.f_lineno
        oid = len(self.ops)
        deps = set()
        for k in reads:
            if k in self.last_write:
                deps.add(self.last_write[k])
        for k in writes:
            if k in self.last_write:
                deps.add(self.last_write[k])
            for r in self.readers.get(k, ()):
                deps.add(r)
        deps.discard(oid)
        self.ops.append(dict(eng=eng, fn=fn, deps=deps, dma=dma, users=0, line=self._line, id=oid))
        for k in reads:
            self.readers.setdefault(k, []).append(oid)
        for k in writes:
            self.last_write[k] = oid
            self.readers[k] = []
        return oid

    def mm(self, out, lhsT, rhs, start, stop, reads, writes):
        self.op("pe", lambda e: e.matmul(out, lhsT, rhs, start=start, stop=stop), reads, writes)

    def dma(self, eng, out, in_, reads, writes, **kw):
        self.op(eng, lambda e: e.dma_start(out=out, in_=in_, **kw), reads, writes, dma=True)

    def emit(self, final_wait_keys=()):
        nc = self.nc
        ops = self.ops
        fdeps = set()
        for k in final_wait_keys:
            if k in self.last_write:
                fdeps.add(self.last_write[k])
        for o in ops:
            if o["eng"] == "pe" and not o["dma"]:
                o["deps"] = {d for d in o["deps"] if not (ops[d]["eng"] == "pe" and not ops[d]["dma"])}
        for o in ops:
            for d in o["deps"]:
                ops[d]["users"] += 1
        for d in fdeps:
            ops[d]["users"] += 1
        from contextlib import ExitStack
        with ExitStack() as st:
            esem = {e: st.enter_context(nc.semaphore(f"s_{e}")) for e in ENGS}
            dsem = {e: [st.enter_context(nc.semaphore(f"d_{e}{i}")) for i in range(NDMASEM)] for e in ("sp", "act", "pool")}
            ecount = {e: 0 for e in ENGS}
            dcount = {e: [0] * NDMASEM for e in dsem}
            drr = {e: 0 for e in dsem}
            for o in ops:
                e = o["eng"]
                if o["dma"]:
                    i = drr[e]
                    drr[e] = (i + 1) % NDMASEM
                    o["prev_ticket"] = (dsem[e][i], dcount[e][i])
                    dcount[e][i] += 16
                    o["ticket"] = (dsem[e][i], dcount[e][i])
                elif o["users"] > 0:
                    ecount[e] += 1
                    o["ticket"] = (esem[e], ecount[e])
                else:
                    o["ticket"] = None
            per_eng = {e: [o for o in ops if o["eng"] == e] for e in ENGS}
            block = st.enter_context(nc.Block())

            self.trace = {e: [] for e in ENGS}
            semname = {id(v): k for k, v in esem.items()}
            for k, lst in dsem.items():
                for i, v in enumerate(lst):
                    semname[id(v)] = f"d_{k}{i}"

            def run_engine(e, eng):
                waited = {}
                tr = self.trace[e]

                def wait(sem, val):
                    key = id(sem)
                    if waited.get(key, 0) >= val:
                        return
                    waited[key] = val
                    tr.append(f"   wait {semname[key]} >= {val}")
                    eng.wait_ge(sem, val)

                for o in per_eng[e]:
                    for d in sorted(o["deps"]):
                        sem, val = ops[d]["ticket"]
                        wait(sem, val)
                    if o["dma"]:
                        psem, pval = o["prev_ticket"]
                        if pval > 0:
                            wait(psem, pval)
                    ins = o["fn"](eng)
                    tr.append(f"op{o['id']} L{o['line']} {'dma' if o['dma'] else ''} -> {semname[id(o['ticket'][0])] + '=' + str(o['ticket'][1]) if o['ticket'] else '-'}")
                    if o["ticket"] is not None:
                        sem, val = o["ticket"]
                        ins.then_inc(sem, 16 if o["dma"] else 1)
                if e == "sp":
                    for d in sorted(fdeps):
                        sem, val = ops[d]["ticket"]
                        wait(sem, val)

            @block.sync
            def _(eng):
                run_engine("sp", eng)

            @block.tensor
            def _(eng):
                run_engine("pe", eng)

            @block.scalar
            def _(eng):
                run_engine("act", eng)

            @block.vector
            def _(eng):
                run_engine("dve", eng)

            @block.gpsimd
            def _(eng):
                run_engine("pool", eng)
        return nc


def run(prog_nc, in_maps, n=8):
    res = run_bass_kernel_spmd(prog_nc, in_maps, core_ids=list(range(n)))
    return res.results


class Stepper:
    def __init__(self, P, fn):
        import threading
        self.P = P
        self.go = threading.Semaphore(0)
        self.done = threading.Semaphore(0)
        self.finished = False
        self.exc = None

        def body():
            try:
                fn()
            except BaseException as e:
                self.exc = e
            self.finished = True
            self.done.release()

        self.thread = threading.Thread(target=body, daemon=True)
        P._stepper = self
        self.thread.start()

    def step(self, k=1):
        for _ in range(k):
            if self.finished:
                return
            self.go.release()
            self.done.acquire()
            if self.finished:
                return

    def finish(self):
        while not self.finished:
            self.go.release()
            self.done.acquire()
        self.P._stepper = None
        if self.exc is not None:
            raise self.exc


D = 1024
DFF = 2816
NT = 2048
TT = 512
NTT = NT // TT
KC = D // 128
FC = DFF // 128
FM_COLS = 2880
TOK_COLS = 400
EPS = 1e-6


def build_P(stages):
    P = Prog()
    nc = P.nc
    xT_d = P.dram("xT", [D, NT])
    xo_d = P.dram("xo", [D, NT], kind="ExternalOutput")
    xT = P.sb([128, KC, NT], F32, "xT")
    hT = P.sb([128, KC, NT], BF16, "hT")
    ones = P.sb([128, 128], BF16, "ones")
    rstd = [P.sb([128, TT], F32, f"rstd{i}") for i in range(2)]
    sq = [P.sb([128, TT], BF16, f"sq{i}") for i in range(2)]
    wA = [P.sb([128, KC, 512], BF16, f"wA{i}") for i in range(2)]
    wB = [P.sb([128, KC, 512], BF16, f"wB{i}") for i in range(2)]
    wD = [P.sb([128, 4, D], BF16, f"wD{i}") for i in range(2)]
    actT = [P.sb([128, 4, TT], BF16, f"actT{i}") for i in range(2)]
    sg = [P.sb([128, TT], F32, f"sg{i}") for i in range(2)]
    stg = [P.sb([128, TT], F32, f"stg{i}") for i in range(2)]
    ps_a = [P.ps([128, TT], F32, f"psa{i}") for i in range(2)]
    ps_b = [P.ps([128, TT], F32, f"psb{i}") for i in range(2)]
    ps_d = [P.ps([128, TT], F32, f"psd{i}") for i in range(4)]

    P.op("dve", lambda e: e.memset(ones[:], 1.0 / D), writes=["ones"])
    for kc in range(KC):
        P.dma("sp", xT[:, kc, :], xT_d[kc * 128:(kc + 1) * 128, :], reads=[], writes=[("xT", kc, t) for t in range(NTT)])

    finals = []

    def fkey():
        k = ("fin", len(finals))
        finals.append(k)
        return k

    cnt = dict(sq=0, rstd=0, w=0, wd=0, act=0, sg=0, stg=0, psa=0, psd=0)

    def norm_to_hT(gcol, tag):
        for t in range(NTT):
            ts = slice(t * TT, (t + 1) * TT)
            pi = cnt["psa"] % 2
            cnt["psa"] += 1
            for kc in range(KC):
                si = cnt["sq"] % 2
                cnt["sq"] += 1
                P.op("act", lambda e, kc=kc, si=si, ts=ts: e.activation(out=sq[si][:], in_=xT[:, kc, ts], func=AF.Square),
                     reads=[("xT", kc, t)], writes=[("sq", si)])
                P.mm(ps_a[pi][:], ones[:], sq[si][:], kc == 0, kc == KC - 1, reads=["ones", ("sq", si)], writes=[("psa", pi)])
            ri = cnt["rstd"] % 2
            cnt["rstd"] += 1
            P.op("act", lambda e, ri=ri, pi=pi: e.activation(out=rstd[ri][:], in_=ps_a[pi][:], func=AF.Sqrt, bias=EPS, scale=1.0),
                 reads=[("psa", pi)], writes=[("rstd", ri)])
            P.op("dve", lambda e, ri=ri: e.reciprocal(out=rstd[ri][:], in_=rstd[ri][:]),
                 reads=[("rstd", ri)], writes=[("rstd", ri)])
            for kc in range(KC):
                P.op("dve", lambda e, kc=kc, ri=ri, ts=ts: e.scalar_tensor_tensor(out=hT[:, kc, ts], in0=xT[:, kc, ts], scalar=gcol[:, kc:kc + 1],
                                                                              in1=rstd[ri][:], op0=ALU.mult, op1=ALU.mult),
                     reads=[("xT", kc, t), ("rstd", ri), ("g", tag)], writes=[("hT", kc, t)])

    def load_gcol(name, tag):
        g_d = P.dram(name, [128, KC])
        g = P.sb([128, KC], F32, "g_" + name)
        P.dma("sp", g[:], g_d[:, :], reads=[], writes=[("g", tag)])
        return g

    def ffn(tag):
        g = load_gcol(f"g_{tag}", tag)
        wg_d = P.dram(f"wg_{tag}", [D, DFF])
        wu_d = P.dram(f"wu_{tag}", [D, DFF])
        wd_d = P.dram(f"wd_{tag}", [DFF, D])
        wg_v = wg_d.rearrange("(kc p) f -> p kc f", p=128)
        wu_v = wu_d.rearrange("(kc p) f -> p kc f", p=128)
        wd_v = wd_d.rearrange("(fc p) d -> p fc d", p=128)
        norm_to_hT(g, tag)
        groups = []
        f0 = 0
        while f0 < FC:
            n = min(4, FC - f0)
            groups.append((f0, n))
            f0 += n

        def load_group(gi):
            f0, n = groups[gi]
            wi = cnt["w"] % 2
            cnt["w"] += 1
            P.dma("pool", wA[wi][:, :, :n * 128], wg_v[:, :, f0 * 128:(f0 + n) * 128], reads=[], writes=[("wA", wi)])
            P.dma("pool", wB[wi][:, :, :n * 128], wu_v[:, :, f0 * 128:(f0 + n) * 128], reads=[], writes=[("wB", wi)])
            P.dma("pool", wD[wi][:, :n, :], wd_v[:, f0:f0 + n, :], reads=[], writes=[("wD", wi)])
            return wi

        nxt = load_group(0)
        for gi, (f0, n) in enumerate(groups):
            wi = nxt
            if gi + 1 < len(groups):
                nxt = load_group(gi + 1)
            for t in range(NTT):
                ts = slice(t * TT, (t + 1) * TT)
                ai = cnt["act"] % 2
                cnt["act"] += 1
                for f in range(n):
                    pi = cnt["psa"] % 2
                    cnt["psa"] += 1
                    for kc in range(KC):
                        P.mm(ps_a[pi][:], wA[wi][:, kc, f * 128:(f + 1) * 128], hT[:, kc, ts], kc == 0, kc == KC - 1,
                             reads=[("wA", wi), ("hT", kc, t)], writes=[("psa", pi)])
                    for kc in range(KC):
                        P.mm(ps_b[pi][:], wB[wi][:, kc, f * 128:(f + 1) * 128], hT[:, kc, ts], kc == 0, kc == KC - 1,
                             reads=[("wB", wi), ("hT", kc, t)], writes=[("psb", pi)])
                    si = cnt["sg"] % 2
                    cnt["sg"] += 1
                    P.op("act", lambda e, si=si, pi=pi: e.activation(out=sg[si][:], in_=ps_a[pi][:], func=AF.Silu),
                         reads=[("psa", pi)], writes=[("sg", si)])
                    P.op("dve", lambda e, si=si, pi=pi, ai=ai, f=f: e.tensor_tensor(out=actT[ai][:, f, :], in0=sg[si][:], in1=ps_b[pi][:], op=ALU.mult),
                         reads=[("sg", si), ("psb", pi)], writes=[("actT", ai, f)])
                for dc in range(KC):
                    di = cnt["psd"] % 4
                    cnt["psd"] += 1
                    for f in range(n):
                        P.mm(ps_d[di][:], wD[wi][:, f, dc * 128:(dc + 1) * 128], actT[ai][:, f, :], f == 0, f == n - 1,
                             reads=[("wD", wi), ("actT", ai, f)], writes=[("psd", di)])
                    P.op("dve", lambda e, di=di, dc=dc, ts=ts: e.scalar_tensor_tensor(out=xT[:, dc, ts], in0=ps_d[di][:], scalar=0.5, in1=xT[:, dc, ts],
                                                                                  op0=ALU.mult, op1=ALU.add),
                         reads=[("psd", di), ("xT", dc, t)], writes=[("xT", dc, t)])

    def outproj(tag):
        yT_d = P.dram(f"yT_{tag}", [D, NT])
        wo_d = P.dram(f"wo_{tag}", [D, D])
        wo_v = wo_d.rearrange("(kc p) d -> p kc d", p=128)
        for h in range(2):
            wi = cnt["w"] % 2
            cnt["w"] += 1
            P.dma("pool", wA[wi][:, :, :], wo_v[:, :, h * 512:(h + 1) * 512], reads=[], writes=[("wA", wi)])
            if h == 0:
                for kc in range(KC):
                    P.dma("pool", hT[:, kc, :], yT_d[kc * 128:(kc + 1) * 128, :], reads=[], writes=[("hT", kc, t) for t in range(NTT)])
            for t in range(NTT):
                ts = slice(t * TT, (t + 1) * TT)
                for dd in range(4):
                    dc = h * 4 + dd
                    di = cnt["psd"] % 4
                    cnt["psd"] += 1
                    for kc in range(KC):
                        P.mm(ps_d[di][:], wA[wi][:, kc, dd * 128:(dd + 1) * 128], hT[:, kc, ts], kc == 0, kc == KC - 1,
                             reads=[("wA", wi), ("hT", kc, t)], writes=[("psd", di)])
                    P.op("dve", lambda e, di=di, dc=dc, ts=ts: e.tensor_tensor(out=xT[:, dc, ts], in0=ps_d[di][:], in1=xT[:, dc, ts], op=ALU.add),
                         reads=[("psd", di), ("xT", dc, t)], writes=[("xT", dc, t)])

    def inproj(tag):
        g = load_gcol(f"g_{tag}", tag)
        win_d = P.dram(f"win_{tag}", [D, FM_COLS + TOK_COLS])
        win_v = win_d.rearrange("(kc p) c -> p kc c", p=128)
        pT_d = P.dram(f"pT_{tag}", [FM_COLS, NT], kind="ExternalOutput")
        ptok_d = P.dram(f"ptok_{tag}", [NT, TOK_COLS], kind="ExternalOutput")
        norm_to_hT(g, tag)
        c0 = 0
        while c0 < FM_COLS:
            n = min(512, FM_COLS - c0)
            wi = cnt["w"] % 2
            cnt["w"] += 1
            P.dma("pool", wA[wi][:, :, :n], win_v[:, :, c0:c0 + n], reads=[], writes=[("wA", wi)])
            for t in range(NTT):
                ts = slice(t * TT, (t + 1) * TT)
                for m0 in range(0, n, 128):
                    m = min(128, n - m0)
                    di = cnt["psd"] % 4
                    cnt["psd"] += 1
                    for kc in range(KC):
                        P.mm(ps_d[di][:m, :], wA[wi][:, kc, m0:m0 + m], hT[:, kc, ts], kc == 0, kc == KC - 1,
                             reads=[("wA", wi), ("hT", kc, t)], writes=[("psd", di)])
                    si = cnt["stg"] % 2
                    cnt["stg"] += 1
                    P.op("act", lambda e, si=si, di=di, m=m: e.activation(out=stg[si][:m, :], in_=ps_d[di][:m, :], func=AF.Copy),
                         reads=[("psd", di)], writes=[("stg", si)])
                    P.dma("sp", pT_d[c0 + m0:c0 + m0 + m, ts], stg[si][:m, :], reads=[("stg", si)], writes=[fkey()])
            c0 += n
        wi = cnt["w"] % 2
        cnt["w"] += 1
        P.dma("pool", wA[wi][:, :, :TOK_COLS], win_v[:, :, FM_COLS:FM_COLS + TOK_COLS], reads=[], writes=[("wA", wi)])
        for b in range(NT // 128):
            t = b // 4
            di = cnt["psd"] % 4
            cnt["psd"] += 1
            for kc in range(KC):
                P.mm(ps_d[di][:, :TOK_COLS], hT[:, kc, b * 128:(b + 1) * 128], wA[wi][:, kc, :TOK_COLS], kc == 0, kc == KC - 1,
                     reads=[("wA", wi), ("hT", kc, t)], writes=[("psd", di)])
            si = cnt["stg"] % 2
            cnt["stg"] += 1
            P.op("act", lambda e, si=si, di=di: e.activation(out=stg[si][:, :TOK_COLS], in_=ps_d[di][:, :TOK_COLS], func=AF.Copy),
                 reads=[("psd", di)], writes=[("stg", si)])
            P.dma("sp", ptok_d[b * 128:(b + 1) * 128, :], stg[si][:, :TOK_COLS], reads=[("stg", si)], writes=[fkey()])

    for kind, tag in stages:
        if kind == "ffn":
            ffn(tag)
        elif kind == "out":
            outproj(tag)
        elif kind == "inproj":
            inproj(tag)
    for kc in range(KC):
        P.dma("sp", xo_d[kc * 128:(kc + 1) * 128, :], xT[:, kc, :], reads=[("xT", kc, t) for t in range(NTT)], writes=[("xo", kc)])
        finals.append(("xo", kc))
    return P.emit(final_wait_keys=finals)


NQB = 16
NIT = 22
NEG = -30000.0


def dsa_consts():
    Rm = np.zeros((128, 128), np.float32)
    for blk in (0, 64):
        for dp in range(32):
            Rm[blk + dp + 32, blk + dp] = -1.0
            Rm[blk + dp, blk + dp + 32] = 1.0
    tri = np.where(np.arange(128)[None, :] > np.arange(128)[:, None], -1e30, 0.0).astype(np.float32)
    I4 = np.tile(np.eye(128, dtype=np.float32), (1, 4))
    sel = np.zeros((65, 64), np.float32)
    sel[64, :] = 1.0
    bo = np.zeros((128, 128), np.float32)
    bo[:64, :64] = 1.0 / 64
    bo[64:, 64:] = 1.0 / 64
    pw = np.tile((2.0 ** -(np.arange(NIT) + 1.0))[None, :], (128, 1)).astype(np.float32)
    inv = 10000.0 ** (-np.arange(32, dtype=np.float32) / 32)
    ang = np.arange(L, dtype=np.float32)[None, :] * inv[:, None]
    cos = np.cos(ang).astype(np.float32)
    sin = np.sin(ang).astype(np.float32)
    cosT = np.tile(cos, (4, 1))
    sinT = np.tile(sin, (4, 1))
    return dict(Rm=Rm, tri=tri, I4=I4, sel=sel, bo=bo, pw=pw, cosT=cosT, sinT=sinT)


def build_dsa():
    P = Prog()
    kT_d = P.dram("kT", [128, L])
    kiT_d = P.dram("kiT", [64, L])
    v_d = P.dram("vtok", [L, 128])
    qT_d = P.dram("qT", [512, NQB * 128])
    qiT_d = P.dram("qiT", [512, NQB * 128])
    wi_d = P.dram("wi", [NQB * 128, 8])
    cos_d = P.dram("cosT", [128, L])
    sin_d = P.dram("sinT", [128, L])
    cosq_d = P.dram("cosq", [128, NQB * 128])
    sinq_d = P.dram("sinq", [128, NQB * 128])
    gq_d = P.dram("gq", [128, 1])
    gk_d = P.dram("gk", [128, 1])
    Rm_d = P.dram("Rm", [128, 128])
    tri_d = P.dram("tri4", [128, 512])
    I4_d = P.dram("I4", [128, 512])
    sel_d = P.dram("sel", [65, 64])
    bo_d = P.dram("bo", [128, 128])
    pw_d = P.dram("pw", [128, NIT])
    ya_d = P.dram("yaT", [512, NQB * 128], kind="ExternalOutput")

    Rm = P.sb([128, 128], BF16, "Rm")
    tri = P.sb([128, 512], F32, "tri")
    I4 = P.sb([128, 512], BF16, "I4")
    sel = P.sb([65, 64], F32, "sel")
    bo = P.sb([128, 128], BF16, "bo")
    pw = P.sb([128, NIT], F32, "pw")
    gq = P.sb([128, 1], F32, "gq")
    gk = P.sb([128, 1], F32, "gk")
    P.dma("pool", Rm[:], Rm_d[:, :], [], ["Rm"])
    P.dma("pool", I4[:], I4_d[:, :], [], ["I4"])
    P.dma("pool", bo[:], bo_d[:, :], [], ["bo"])
    P.dma("sp", tri[:], tri_d[:, :], [], ["tri"])
    P.dma("sp", sel[:], sel_d[:, :], [], ["sel"])
    P.dma("sp", pw[:], pw_d[:, :], [], ["pw"])
    P.dma("sp", gq[:], gq_d[:, :], [], ["gq"])
    P.dma("sp", gk[:], gk_d[:, :], [], ["gk"])

    K_fin = P.sb([128, L], BF16, "K_fin")
    KI_fin = P.sb([128, L], BF16, "KI_fin")
    V1 = P.sb([128, 64, 2, 65], BF16, "V1")
    acc = P.sb([128, L], F32, "acc")
    junk = P.sb([128, L], BF16, "junk")
    mb = P.sb([128, L], BF16, "mb")
    Q_fin = P.sb([128, 4, 128], BF16, "Q_fin")
    QI_fin = P.sb([128, 4, 128], BF16, "QI_fin")
    wi = P.sb([128, 8], F32, "wi")
    raw = [P.sb([128, 512], F32, f"raw{i}") for i in range(2)]
    cs = [P.sb([128, 512], F32, f"cs{i}") for i in range(2)]
    sn = [P.sb([128, 512], F32, f"sn{i}") for i in range(2)]
    sqb = P.sb([128, 512], BF16, "sqb")
    rs = P.sb([128, 512], F32, "rs")
    kn = P.sb([128, 512], F32, "kn")
    knb = P.sb([128, 512], BF16, "knb")
    t1 = P.sb([128, 512], F32, "t1")
    t2 = P.sb([128, 512], F32, "t2")
    rbuf = [P.sb([128, 512], F32, f"rbuf{i}") for i in range(2)]
    pexp = [P.sb([128, 512], BF16, f"pexp{i}") for i in range(2)]
    o_sb = [P.sb([65, 512], F32, f"o_sb{i}") for i in range(2)]
    rden = P.sb([64, 512], F32, "rden")
    ya_t = [P.sb([64, 512], F32, f"ya_t{i}") for i in range(2)]
    col = {n: P.sb([128, 1], F32, "c_" + n) for n in ("mx", "mn", "lo", "rng", "mid", "cnt", "cs_", "thr0")}
    steps = P.sb([128, NIT], F32, "steps")

    ps_prep = P.ps([128, 512], F32, "ps_prep")
    ps_i = [P.ps([128, 512], F32, f"ps_i{i}") for i in range(2)]
    ps_l = [P.ps([128, 512], F32, f"ps_l{i}") for i in range(2)]
    ps_o = [P.ps([128, 512], F32, f"ps_o{i}") for i in range(2)]
    ps_den = P.ps([64, 512], F32, "ps_den")

    cnt = dict(raw=0, r=0, pi=0, pl=0, po=0, pe=0, ya=0)
    finals = []

    P.op("dve", lambda e: e.memset(col["thr0"][:], -1e29), writes=["thr0"])

    def rope_pipeline(src_key, src, cos_ap, sin_ap, cs_keys, out_ap, out_keys, n, norm_g=None, gkey=None):
        if norm_g is not None:
            P.op("act", lambda e: e.activation(out=sqb[:, :n], in_=src, func=AF.Square), [src_key], ["sqb"])
            P.mm(ps_prep[:, :n], bo[:], sqb[:, :n], True, True, ["bo", "sqb"], ["ps_prep"])
            P.op("act", lambda e: e.activation(out=rs[:, :n], in_=ps_prep[:, :n], func=AF.Sqrt, bias=1e-6, scale=1.0), ["ps_prep"], ["rs"])
            P.op("dve", lambda e: e.reciprocal(out=rs[:, :n], in_=rs[:, :n]), ["rs"], ["rs"])
            P.op("dve", lambda e: e.scalar_tensor_tensor(out=kn[:, :n], in0=src, scalar=norm_g[:, 0:1], in1=rs[:, :n], op0=ALU.mult, op1=ALU.mult),
                 [src_key, "rs", gkey], ["kn"])
            cur, cur_key = kn[:, :n], "kn"
        else:
            cur, cur_key = src, src_key
        P.op("act", lambda e: e.activation(out=knb[:, :n], in_=cur, func=AF.Copy), [cur_key], ["knb"])
        P.mm(ps_prep[:, :n], Rm[:], knb[:, :n], True, True, ["Rm", "knb"], ["ps_prep"])
        P.op("dve", lambda e: e.tensor_tensor(out=t1[:, :n], in0=cur, in1=cos_ap, op=ALU.mult), [cur_key] + cs_keys, ["t1"])
        P.op("dve", lambda e: e.tensor_tensor(out=t2[:, :n], in0=ps_prep[:, :n], in1=sin_ap, op=ALU.mult), ["ps_prep"] + cs_keys, ["t2"])
        P.op("dve", lambda e: e.tensor_tensor(out=out_ap, in0=t1[:, :n], in1=t2[:, :n], op=ALU.add), ["t1", "t2"], out_keys)

    v_v = v_d.rearrange("(kt p) (g d) -> p kt g d", p=128, g=2)
    for q4 in range(4):
        P.dma("pool", V1[:, q4 * 16:(q4 + 1) * 16, 0, 0:64], v_v[:, q4 * 16:(q4 + 1) * 16, 0, :], [], [("V1", q4)])
        P.dma("pool", V1[:, q4 * 16:(q4 + 1) * 16, 1, 0:64], v_v[:, q4 * 16:(q4 + 1) * 16, 1, :], [], [("V1b", q4)])
    P.op("dve", lambda e: e.memset(V1[:, :, :, 64:65], 1.0), [], ["V1ones"])
    for kt in range(L // 512):
        ks = slice(kt * 512, (kt + 1) * 512)
        ri = cnt["raw"] % 2
        cnt["raw"] += 1
        P.dma("sp", cs[ri][:], cos_d[:, ks], [], [("cs", ri)])
        P.dma("sp", sn[ri][:], sin_d[:, ks], [], [("sn", ri)])
        P.dma("sp", raw[ri][:], kT_d[:, ks], [], [("raw", ri)])
        rope_pipeline(("raw", ri), raw[ri][:], cs[ri][:], sn[ri][:], [("cs", ri), ("sn", ri)], K_fin[:, ks], [("K", kt)], 512, norm_g=gk, gkey="gk")
        ri2 = cnt["raw"] % 2
        cnt["raw"] += 1
        P.dma("sp", raw[ri2][0:64, :], kiT_d[:, ks], [], [("raw", ri2)])
        P.dma("sp", raw[ri2][64:128, :], kiT_d[:, ks], [], [("raw", ri2, "b")])
        rope_pipeline(("raw", ri2), raw[ri2][:], cs[ri][:], sn[ri][:], [("cs", ri), ("sn", ri), ("raw", ri2, "b")], KI_fin[:, ks], [("KI", kt)], 512)

    qT_v = qT_d.rearrange("(j p) t -> p j t", p=128)
    qiT_v = qiT_d.rearrange("(j p) t -> p j t", p=128)
    ya_v = ya_d.rearrange("(h d) t -> d h t", d=64)

    for lb in range(NQB):
        S = 512 * (lb + 1)
        qs = slice(lb * 128, (lb + 1) * 128)
        nkt = S // 128
        ri = cnt["raw"] % 2
        cnt["raw"] += 1
        P.dma("sp", raw[ri][:].rearrange("p (j t) -> p j t", j=4), qT_v[:, :, qs], [], [("raw", ri)])
        for j in range(4):
            P.dma("sp", cs[ri][:, j * 128:(j + 1) * 128], cosq_d[:, qs], [], [("cs", ri)])
            P.dma("sp", sn[ri][:, j * 128:(j + 1) * 128], sinq_d[:, qs], [], [("sn", ri)])
        cskeys = [("cs", ri), ("sn", ri)]
        rope_pipeline(("raw", ri), raw[ri][:], cs[ri][:], sn[ri][:], cskeys, Q_fin[:].rearrange("p j t -> p (j t)"), ["Q"], 512, norm_g=gq, gkey="gq")
        ri2 = cnt["raw"] % 2
        cnt["raw"] += 1
        P.dma("sp", raw[ri2][:].rearrange("p (j t) -> p j t", j=4), qiT_v[:, :, qs], [], [("raw", ri2)])
        rope_pipeline(("raw", ri2), raw[ri2][:], cs[ri][:], sn[ri][:], cskeys, QI_fin[:].rearrange("p j t -> p (j t)"), ["QI"], 512)
        P.dma("sp", wi[:], wi_d[qs, :], [], ["wi"])

        nst = (S + 511) // 512
        for st in range(nst):
            w = min(512, S - st * 512)
            ss = slice(st * 512, st * 512 + w)
            kts = sorted(set([(st * 512) // 512]))
            for h in range(8):
                m, half = h // 2, h % 2
                pi = cnt["pi"] % 2
                cnt["pi"] += 1
                hp = slice(64 * half, 64 * half + 64)
                P.mm(ps_i[pi][:, :w], QI_fin[hp, m, :], KI_fin[hp, ss], True, True, ["QI", ("KI", st)], [("ps_i", pi)])
                r_i = cnt["r"] % 2
                cnt["r"] += 1
                P.op("act", lambda e, r_i=r_i, pi=pi, w=w: e.activation(out=rbuf[r_i][:, :w], in_=ps_i[pi][:, :w], func=AF.Relu),
                     [("ps_i", pi)], [("rbuf", r_i)])
                if h == 0:
                    P.op("dve", lambda e, r_i=r_i, w=w, ss=ss: e.tensor_scalar(out=acc[:, ss], in0=rbuf[r_i][:, :w], scalar1=wi[:, 0:1], scalar2=None, op0=ALU.mult),
                         [("rbuf", r_i), "wi"], [("acc", st)])
                else:
                    P.op("dve", lambda e, r_i=r_i, w=w, ss=ss, h=h: e.scalar_tensor_tensor(out=acc[:, ss], in0=rbuf[r_i][:, :w], scalar=wi[:, h:h + 1], in1=acc[:, ss],
                                                                                         op0=ALU.mult, op1=ALU.add),
                         [("rbuf", r_i), "wi", ("acc", st)], [("acc", st)])
        acc_keys = [("acc", st) for st in range(nst)]
        if lb == 0:
            P.op("dve", lambda e: e.tensor_tensor(out=t1[:], in0=acc[:, 0:512], in1=tri[:], op=ALU.subtract), [("acc", 0), "tri"], ["t1"])
            P.op("dve", lambda e: e.tensor_reduce(out=col["lo"][:], in_=t1[:], axis=AX.X, op=ALU.min), ["t1"], ["lo"])
        else:
            P.op("dve", lambda e, S=S: e.tensor_reduce(out=col["lo"][:], in_=acc[:, :S - 512], axis=AX.X, op=ALU.min), acc_keys, ["lo"])
        P.op("dve", lambda e, S=S: e.tensor_tensor(out=acc[:, S - 512:S], in0=acc[:, S - 512:S], in1=tri[:], op=ALU.add),
             [("acc", nst - 1), "tri"], [("acc", nst - 1)])
        if True:
            P.op("dve", lambda e, S=S: e.tensor_reduce(out=col["mx"][:], in_=acc[:, :S], axis=AX.X, op=ALU.max), acc_keys, ["mx"])
            P.op("dve", lambda e: e.tensor_tensor(out=col["rng"][:], in0=col["mx"][:], in1=col["lo"][:], op=ALU.subtract), ["mx", "lo"], ["rng"])
            P.op("dve", lambda e: e.tensor_scalar(out=steps[:], in0=pw[:], scalar1=col["rng"][:, 0:1], scalar2=None, op0=ALU.mult), ["pw", "rng"], ["steps"])
            for k in range(NIT):
                P.op("dve", lambda e, k=k: e.tensor_tensor(out=col["mid"][:], in0=col["lo"][:], in1=steps[:, k:k + 1], op=ALU.add), ["lo", "steps"], ["mid"])
                P.op("dve", lambda e, S=S: e.tensor_scalar(out=junk[:, :S], in0=acc[:, :S], scalar1=col["mid"][:, 0:1], scalar2=None, op0=ALU.is_ge,
                                                          op1=ALU.add, accum_out=col["cnt"][:]),
                     acc_keys + ["mid"], ["junk", "cnt"])
                P.op("dve", lambda e: e.memset(col["mn"][:], 0.0), [], ["mn"])
                P.op("dve", lambda e, k=k: e.scalar_tensor_tensor(out=col["cs_"][:], in0=col["cnt"][:], scalar=255.5, in1=steps[:, k:k + 1], op0=ALU.is_ge, op1=ALU.mult),
                     ["cnt", "steps"], ["cs_"])
                P.op("dve", lambda e: e.tensor_tensor(out=col["lo"][:], in0=col["lo"][:], in1=col["cs_"][:], op=ALU.add), ["lo", "cs_"], ["lo"])
            thr, thr_key = col["lo"], "lo"
        P.op("dve", lambda e, S=S, thr=thr: e.tensor_scalar(out=mb[:, :S], in0=acc[:, :S], scalar1=thr[:, 0:1], scalar2=NEG, op0=ALU.is_lt, op1=ALU.mult),
             acc_keys + [thr_key], ["mb"])
        for g in range(2):
            gp = slice(64 * g, 64 * g + 64)
            po = cnt["po"] % 2
            cnt["po"] += 1
            for kt in range(nkt):
                pl = cnt["pl"] % 2
                cnt["pl"] += 1
                P.mm(ps_l[pl][:], K_fin[gp, kt * 128:(kt + 1) * 128], Q_fin[gp, :, :].rearrange("p j t -> p (j t)"), True, False,
                     [("K", kt // 4), "Q"], [("ps_l", pl)])
                P.mm(ps_l[pl][:], mb[:, kt * 128:(kt + 1) * 128], I4[:], False, True, ["mb", "I4"], [("ps_l", pl)])
                pe = cnt["pe"] % 2
                cnt["pe"] += 1
                P.op("act", lambda e, pe=pe, pl=pl: e.activation(out=pexp[pe][:], in_=ps_l[pl][:], func=AF.Exp, scale=0.125),
                     [("ps_l", pl)], [("pexp", pe)])
                P.mm(ps_o[po][:65, :], V1[:, kt, g, :], pexp[pe][:], kt == 0, kt == nkt - 1,
                     [("V1", kt // 16), ("V1b", kt // 16), "V1ones", ("pexp", pe)], [("ps_o", po)])
            P.op("act", lambda e, po=po: e.activation(out=o_sb[po][:], in_=ps_o[po][:65, :], func=AF.Copy), [("ps_o", po)], [("o_sb", po)])
            P.mm(ps_den[:], sel[:], o_sb[po][:], True, True, ["sel", ("o_sb", po)], ["ps_den"])
            P.op("dve", lambda e: e.reciprocal(out=rden[:], in_=ps_den[:]), ["ps_den"], ["rden"])
            yi = cnt["ya"] % 2
            cnt["ya"] += 1
            P.op("dve", lambda e, yi=yi, po=po: e.tensor_tensor(out=ya_t[yi][:], in0=o_sb[po][0:64, :], in1=rden[:], op=ALU.mult),
                 [("o_sb", po), "rden"], [("ya_t", yi)])
            fk = ("fin", len(finals))
            finals.append(fk)
            P.dma("sp", ya_v[:, 4 * g:4 * g + 4, qs], ya_t[yi][:].rearrange("p (j t) -> p j t", j=4), [("ya_t", yi)], [fk])
    return P.emit(finals)


RTT = 256
RG_NBLK = RTT // 128
RG_NTT = L // RTT


def build_rg(layer, nsteps=L):
    P = Prog()
    f32 = F32
    rw_r_d = P.dram("rw_r", [64, L])
    rw_k_d = P.dram("rw_k", [64, L])
    rw_v_d = P.dram("rw_v", [64, L])
    rw_lora_d = P.dram("rw_lora", [128, L])
    rw_vall_d = P.dram("rw_vall", [256, L])
    rwc_d = P.dram("rw_cols", [64, 16])
    rw_mul_d = P.dram("rw_mu_lora", [128, 1])
    rw_muv_d = P.dram("rw_mu_vall", [128, 2])
    rw_wup_d = P.dram("rw_wup", [32, 64])
    rw_aup_d = P.dram("rw_aup", [32, 64])
    rw_gup_d = P.dram("rw_gup", [64, 64])
    rw_vdown_d = P.dram("rw_vdown", [128, 2, 32])
    rw_vup_d = P.dram("rw_vup", [32, 64])
    vfirst_in_d = P.dram("vfirst_in", [64, L])
    vfirst_out_d = P.dram("vfirst_out", [64, L], kind="ExternalOutput")
    gd_q_d = P.dram("gd_q", [64, L])
    gd_k_d = P.dram("gd_k", [64, L])
    gd_v_d = P.dram("gd_v", [64, L])
    gd_z_d = P.dram("gd_z", [64, L])
    gd_b_d = P.dram("gd_b", [1, L])
    gd_a_d = P.dram("gd_a", [1, L])
    gdc_d = P.dram("gd_cols", [64, 16])
    ident_d = P.dram("ident", [128, 128])
    yb_d = P.dram("ybT", [64, L], kind="ExternalOutput")
    yc_d = P.dram("ycT", [64, L], kind="ExternalOutput")

    ident = P.sb([128, 128], f32, "ident")
    ones64 = P.sb([64, 64], f32, "ones64")
    ones1 = P.sb([64, 64], f32, "ones1")
    rwc = P.sb([64, 16], f32, "rwc")
    gdc = P.sb([64, 16], f32, "gdc")
    mul = P.sb([128, 1], f32, "mul")
    muv = P.sb([128, 2], f32, "muv")
    wup = P.sb([32, 64], f32, "wup")
    aup = P.sb([64, 64], f32, "aup")
    gup = P.sb([128, 64], f32, "gup")
    vdown = P.sb([128, 2, 32], f32, "vdown")
    vup = P.sb([32, 64], f32, "vup")
    Acol = P.sb([64, 1], f32, "Acol")
    for sbt, dt_, key in ((ident, ident_d, "ident"), (rwc, rwc_d, "rwc"), (gdc, gdc_d, "gdc"), (mul, rw_mul_d, "mul"), (muv, rw_muv_d, "muv"),
                          (wup, rw_wup_d, "wup"), (vup, rw_vup_d, "vup")):
        P.dma("sp", sbt[:], dt_[:, :], [], [key])
    P.dma("sp", vdown[:], rw_vdown_d[:, :, :], [], ["vdown"])
    P.dma("sp", aup[32:64, :], rw_aup_d[:, :], [], ["aup"])
    P.dma("sp", gup[64:128, :], rw_gup_d[:, :], [], ["gup"])
    P.op("dve", lambda e: e.memset(ones64[:], 1.0 / 64), [], ["ones64"])
    P.op("dve", lambda e: e.memset(ones1[:], 1.0), [], ["ones1"])
    P.op("act", lambda e: e.activation(out=Acol[:], in_=gdc[:, 12:13], func=AF.Exp), ["gdc"], ["Acol"])
    nAcol = P.sb([64, 1], f32, "nAcol")
    P.op("dve", lambda e: e.tensor_scalar(out=nAcol[:], in0=Acol[:], scalar1=-1.0, scalar2=None, op0=ALU.mult), ["Acol"], ["nAcol"])

    def T2(shape, name, dt=f32):
        return [P.sb(shape, dt, f"{name}{i}") for i in range(2)]

    NB = 2
    raw_r = T2([64, RTT + 1], "raw_r"); raw_k = T2([64, RTT + 1], "raw_k"); raw_v = T2([64, RTT + 1], "raw_v")
    raw_l = T2([128, RTT + 1], "raw_l"); raw_va = [T2([128, RTT + 1], "raw_va0"), T2([128, RTT + 1], "raw_va1")]
    NTMP = 56
    tmp = [P.sb([128, RTT], f32, f"tmp{i}") for i in range(NTMP)]
    r_t = T2([64, RTT], "r_t"); w_t = T2([64, RTT], "w_t"); a_t = T2([64, RTT], "a_t"); b_t = T2([64, RTT], "b_t")
    g_t = T2([64, RTT], "g_t"); v_t = T2([64, RTT], "v_t"); km_t = T2([64, RTT], "km_t"); bon_t = T2([64, RTT], "bon_t")
    ktok_r = T2([128, RG_NBLK, 64], "ktok_r"); vtok_r = T2([128, RG_NBLK, 64], "vtok_r")
    graw_q = T2([64, RTT + 3], "graw_q"); graw_k = T2([64, RTT + 3], "graw_k"); graw_v = T2([64, RTT + 3], "graw_v")
    gz = T2([64, RTT], "gz"); gb = T2([64, RTT], "gb"); ga = T2([64, RTT], "ga")
    gq_t = T2([64, RTT], "gq_t"); gk_t = T2([64, RTT], "gk_t"); gal_t = T2([64, RTT], "gal_t"); gnb_t = T2([64, RTT], "gnb_t")
    gv_t = T2([64, RTT], "gv_t"); gkb_t = T2([64, RTT], "gkb_t")
    ktok_g = T2([128, RG_NBLK, 64], "ktok_g"); vtok_g = T2([128, RG_NBLK, 64], "vtok_g")
    ytile = T2([64, RTT], "ytile"); otile = T2([64, RTT], "otile")
    yout = T2([64, RTT], "yout"); oout = T2([64, RTT], "oout")
    vmask = {"r": [P.sb([128, 64], f32, f"vmask_r{i}") for i in range(3)], "g": [P.sb([128, 64], f32, f"vmask_g{i}") for i in range(3)]}
    arep = {"r": [P.sb([64, 64], f32, f"arep_r{i}") for i in range(3)], "g": [P.sb([64, 64], f32, f"arep_g{i}") for i in range(3)]}
    St = {"r": [P.sb([64, 64], f32, f"St_r{i}") for i in range(3)], "g": [P.sb([64, 64], f32, f"St_g{i}") for i in range(3)]}
    Stmp = {"r": [P.sb([64, 64], f32, f"Stmp_r{i}") for i in range(2)], "g": [P.sb([64, 64], f32, f"Stmp_g{i}") for i in range(2)]}

    ps_kv = {"r": P.ps([64, 512], f32, "ps_kv_r"), "g": P.ps([64, 512], f32, "ps_kv_g")}
    ps_sa = {"r": P.ps([64, 512], f32, "ps_sa_r"), "g": P.ps([64, 512], f32, "ps_sa_g")}
    ps_y1 = {"r": P.ps([64, 512], f32, "ps_yr"), "g": P.ps([64, 512], f32, "ps_yg")}
    ps_y = {"r": [ps_y1["r"], ps_y1["r"]], "g": [ps_y1["g"], ps_y1["g"]]}
    ps_p = [P.ps([128, 512], f32, f"ps_p{i}") for i in range(2)]
    cnt = dict(pp=0, tmp=0, kv=0, sa=0)
    finals = []

    def fin():
        k = ("fin", len(finals))
        finals.append(k)
        return k

    for kind in ("r", "g"):
        P.op("dve", lambda e, kind=kind: e.memset(St[kind][2][:], 0.0), [], [("St", kind, 2)])

    def newtmp():
        i = cnt["tmp"]
        cnt["tmp"] += 1
        assert i < NTMP
        return tmp[i], ("tmp", i)

    def newps():
        i = cnt["pp"] % 2
        cnt["pp"] += 1
        return ps_p[i][:, 0:RTT], ("ps_p", i)

    def bcast_sum(dst, dkey, src, skey, lhs, lkey, rows=64):
        P.mm(dst, lhs, src, True, True, [skey, lkey], [dkey])

    def rsqrt_from_ps(out_ap, okey, ps_ap, pkey, eps):
        P.op("act", lambda e: e.activation(out=out_ap, in_=ps_ap, func=AF.Sqrt, bias=eps, scale=1.0), [pkey], [okey])
        P.op("dve", lambda e: e.reciprocal(out=out_ap, in_=out_ap), [okey], [okey])

    def transpose_to_tok(dst, dkey, srcT, skey):
        ps_t, ptk = newps()
        for bq in range(RG_NBLK):
            P.mm(ps_t[:, bq * 64:(bq + 1) * 64], srcT[:, bq * 128:(bq + 1) * 128], ident[0:64, 0:64], True, True, [skey, "ident"], [ptk])
        P.op("act", lambda e: e.activation(out=dst[:].rearrange("p a b -> p (a b)"), in_=ps_t[:, 0:RG_NBLK * 64], func=AF.Copy), [ptk], [dkey])

    def shift_mix(raw, rkey, mu_ap, mukey, out_ap, okey, rows):
        t, tk = newtmp()
        P.op("dve", lambda e: e.tensor_tensor(out=t[:rows, :], in0=raw[:rows, 0:RTT], in1=raw[:rows, 1:RTT + 1], op=ALU.subtract), [rkey], [tk])
        P.op("dve", lambda e: e.scalar_tensor_tensor(out=out_ap, in0=t[:rows, :], scalar=mu_ap, in1=raw[:rows, 1:RTT + 1], op0=ALU.mult, op1=ALU.add),
             [tk, rkey, mukey], [okey])

    def load_shift(raw, key, src_d, t0, rows, npre):
        if t0 == 0:
            P.op("dve", lambda e: e.memset(raw[:rows, 0:npre], 0.0), [], [key + ("z",)])
            P.dma("sp", raw[:rows, npre:npre + RTT], src_d[:, 0:RTT], [key + ("z",)], [key])
        else:
            P.dma("sp", raw[:rows, :], src_d[:, t0 - npre:t0 + RTT], [], [key])

    def prep_rwkv(n):
        bi = n % 2
        t0 = n * RTT
        ts = slice(t0, t0 + RTT)
        K = lambda nm: ("rw", nm, bi)
        load_shift(raw_r[bi], K("raw_r"), rw_r_d, t0, 64, 1)
        load_shift(raw_k[bi], K("raw_k"), rw_k_d, t0, 64, 1)
        load_shift(raw_v[bi], K("raw_v"), rw_v_d, t0, 64, 1)
        load_shift(raw_l[bi], K("raw_l"), rw_lora_d, t0, 128, 1)
        shift_mix(raw_r[bi], K("raw_r"), rwc[:, 0:1], "rwc", r_t[bi][:], K("r"), 64)
        kx, kxk = newtmp()
        shift_mix(raw_k[bi], K("raw_k"), rwc[:, 1:2], "rwc", kx[:64, :], kxk, 64)
        shift_mix(raw_v[bi], K("raw_v"), rwc[:, 2:3], "rwc", v_t[bi][:], K("v"), 64)
        lo, lok = newtmp()
        shift_mix(raw_l[bi], K("raw_l"), mul[:, 0:1], "mul", lo[:, :], lok, 128)
        th, thk = newtmp()
        P.op("act", lambda e: e.activation(out=th[0:32, :], in_=lo[0:32, :], func=AF.Tanh), [lok], [thk])
        ps, psk = newps()
        P.mm(ps[0:64, :], wup[:], th[0:32, :], True, True, ["wup", thk], [psk])
        e1, e1k = newtmp()
        nw0, nw0k = newtmp()
        P.op("dve", lambda e: e.tensor_scalar(out=nw0[0:64, 0:1], in0=rwc[:, 3:4], scalar1=-1.0, scalar2=None, op0=ALU.mult), ["rwc"], [nw0k])
        P.op("act", lambda e: e.activation(out=e1[0:64, :], in_=ps[0:64, :], func=AF.Exp, bias=nw0[0:64, 0:1], scale=-1.0), [psk, nw0k], [e1k])
        P.op("act", lambda e: e.activation(out=e1[0:64, :], in_=e1[0:64, :], func=AF.Ln, bias=1.0, scale=1.0), [e1k], [e1k])
        P.op("act", lambda e: e.activation(out=e1[0:64, :], in_=e1[0:64, :], func=AF.Exp, bias=-0.5, scale=-1.0), [e1k], [e1k])
        P.op("act", lambda e: e.activation(out=w_t[bi][:], in_=e1[0:64, :], func=AF.Exp, scale=-1.0), [e1k], [K("w")])
        ps2, ps2k = newps()
        P.mm(ps2[0:64, :], aup[32:64, :], lo[32:64, :], True, True, ["aup", lok], [ps2k])
        asg, asgk = newtmp()
        P.op("act", lambda e: e.activation(out=asg[0:64, :], in_=ps2[0:64, :], func=AF.Sigmoid, bias=rwc[:, 4:5], scale=1.0), [ps2k, "rwc"], [asgk])
        sg_, sgk = newtmp()
        P.op("act", lambda e: e.activation(out=sg_[64:128, :], in_=lo[64:128, :], func=AF.Sigmoid), [lok], [sgk])
        ps3, ps3k = newps()
        P.mm(ps3[0:64, :], gup[64:128, :], sg_[64:128, :], True, True, ["gup", sgk], [ps3k])
        P.op("act", lambda e: e.activation(out=g_t[bi][:], in_=ps3[0:64, :], func=AF.Copy), [ps3k], [K("g")])
        return dict(bi=bi, ts=ts, kx=kx, kxk=kxk, asg=asg, asgk=asgk, lo=lo, lok=lok)

    def prep_rwkv2(n, c):
        bi = c["bi"]
        t0 = n * RTT
        ts = c["ts"]
        K = lambda nm: ("rw", nm, bi)
        kx, kxk, asg, asgk = c["kx"], c["kxk"], c["asg"], c["asgk"]
        kk, kkk = newtmp()
        P.op("dve", lambda e: e.tensor_scalar(out=kk[0:64, :], in0=kx[0:64, :], scalar1=rwc[:, 5:6], scalar2=None, op0=ALU.mult), [kxk, "rwc"], [kkk])
        sq, sqk = newtmp()
        P.op("act", lambda e: e.activation(out=sq[0:64, :], in_=kk[0:64, :], func=AF.Square), [kkk], [sqk])
        ps, psk = newps()
        P.mm(ps[0:64, :], ones1[:], sq[0:64, :], True, True, ["ones1", sqk], [psk])
        rsq, rsqk = sq, sqk
        rsqrt_from_ps(rsq[0:64, :], rsqk, ps[0:64, :], psk, 1e-12)
        P.op("dve", lambda e: e.tensor_tensor(out=kk[0:64, :], in0=kk[0:64, :], in1=rsq[0:64, :], op=ALU.mult), [kkk, rsqk], [kkk])
        P.op("dve", lambda e: e.tensor_scalar(out=a_t[bi][:], in0=kk[0:64, :], scalar1=-1.0, scalar2=None, op0=ALU.mult), [kkk], [K("a")])
        P.op("dve", lambda e: e.tensor_tensor(out=b_t[bi][:], in0=kk[0:64, :], in1=asg[0:64, :], op=ALU.mult), [kkk, asgk], [K("b")])
        f, fk = newtmp()
        P.op("dve", lambda e: e.tensor_scalar(out=f[0:64, :], in0=asg[0:64, :], scalar1=-1.0, scalar2=rwc[:, 6:7], op0=ALU.add, op1=ALU.mult), [asgk, "rwc"], [fk])
        P.op("dve", lambda e: e.scalar_tensor_tensor(out=km_t[bi][:], in0=f[0:64, :], scalar=1.0, in1=kx[0:64, :], op0=ALU.add, op1=ALU.mult), [fk, kxk], [K("km")])
        if layer == 0:
            fk2 = fin()
            P.dma("sp", vfirst_out_d[:, ts], v_t[bi][:], [K("v")], [fk2])
        else:
            vas = []
            for h2 in range(2):
                key = K(f"raw_va{h2}")
                src = rw_vall_d[h2 * 128:(h2 + 1) * 128, :]
                if t0 == 0:
                    P.op("dve", lambda e, h2=h2: e.memset(raw_va[h2][bi][:, 0:1], 0.0), [], [key + ("z",)])
                    P.dma("sp", raw_va[h2][bi][:, 1:1 + RTT], src[:, 0:RTT], [key + ("z",)], [key])
                else:
                    P.dma("sp", raw_va[h2][bi][:, :], src[:, t0 - 1:t0 + RTT], [], [key])
                va, vak = newtmp()
                shift_mix(raw_va[h2][bi], key, muv[:, h2:h2 + 1], "muv", va[:, :], vak, 128)
                vas.append((va, vak))
            ps4, ps4k = newps()
            for h2 in range(2):
                P.mm(ps4[0:32, :], vdown[:, h2, :], vas[h2][0][:, :], h2 == 0, h2 == 1, ["vdown", vas[h2][1]], [ps4k])
            t32, t32k = newtmp()
            P.op("act", lambda e: e.activation(out=t32[0:32, :], in_=ps4[0:32, :], func=AF.Copy), [ps4k], [t32k])
            ps5, ps5k = newps()
            P.mm(ps5[0:64, :], vup[:], t32[0:32, :], True, True, ["vup", t32k], [ps5k])
            sgv, sgvk = newtmp()
            P.op("act", lambda e: e.activation(out=sgv[0:64, :], in_=ps5[0:64, :], func=AF.Sigmoid, bias=rwc[:, 10:11], scale=1.0), [ps5k, "rwc"], [sgvk])
            vf, vfk = newtmp()
            P.dma("sp", vf[0:64, :], vfirst_in_d[:, ts], [], [vfk])
            P.op("dve", lambda e: e.tensor_tensor(out=vf[0:64, :], in0=vf[0:64, :], in1=v_t[bi][:], op=ALU.subtract), [vfk, K("v")], [vfk])
            P.op("dve", lambda e: e.tensor_tensor(out=vf[0:64, :], in0=vf[0:64, :], in1=sgv[0:64, :], op=ALU.mult), [vfk, sgvk], [vfk])
            P.op("dve", lambda e: e.tensor_tensor(out=v_t[bi][:], in0=v_t[bi][:], in1=vf[0:64, :], op=ALU.add), [vfk, K("v")], [K("v")])
            fk2 = fin()
            P.dma("sp", vfirst_out_d[:, ts], v_t[bi][:], [K("v")], [fk2])
        rk, rkk = newtmp()
        P.op("dve", lambda e: e.scalar_tensor_tensor(out=rk[0:64, :], in0=r_t[bi][:], scalar=rwc[:, 7:8], in1=km_t[bi][:], op0=ALU.mult, op1=ALU.mult),
             [K("r"), K("km"), "rwc"], [rkk])
        ps6, ps6k = newps()
        P.mm(ps6[0:64, :], ones1[:], rk[0:64, :], True, True, ["ones1", rkk], [ps6k])
        P.op("dve", lambda e: e.tensor_tensor(out=bon_t[bi][:], in0=ps6[0:64, :], in1=v_t[bi][:], op=ALU.mult), [ps6k, K("v")], [K("bon")])
        transpose_to_tok(ktok_r[bi], K("ktok"), km_t[bi], K("km"))
        transpose_to_tok(vtok_r[bi], K("vtok"), v_t[bi], K("v"))

    def prep_gdn(n):
        bi = n % 2
        t0 = n * RTT
        ts = slice(t0, t0 + RTT)
        K = lambda nm: ("gd", nm, bi)
        load_shift(graw_q[bi], K("raw_q"), gd_q_d, t0, 64, 3)
        load_shift(graw_k[bi], K("raw_k"), gd_k_d, t0, 64, 3)
        load_shift(graw_v[bi], K("raw_v"), gd_v_d, t0, 64, 3)
        P.dma("sp", gz[bi][:], gd_z_d[:, ts], [], [K("z")])
        P.dma("sp", gb[bi][:], gd_b_d[:, ts].partition_broadcast(64), [], [K("b")])
        P.dma("sp", ga[bi][:], gd_a_d[:, ts].partition_broadcast(64), [], [K("a")])
        outs = {}
        for gi_, (raw, rk_) in enumerate(((graw_q[bi], K("raw_q")), (graw_k[bi], K("raw_k")), (graw_v[bi], K("raw_v")))):
            cv, cvk = newtmp()
            P.op("dve", lambda e, raw=raw, cv=cv, gi_=gi_: e.tensor_scalar(out=cv[0:64, :], in0=raw[:, 0:RTT], scalar1=gdc[:, 4 * gi_:4 * gi_ + 1], scalar2=None, op0=ALU.mult),
                 [rk_, "gdc"], [cvk])
            for i in range(1, 4):
                P.op("dve", lambda e, raw=raw, cv=cv, gi_=gi_, i=i: e.scalar_tensor_tensor(out=cv[0:64, :], in0=raw[:, i:i + RTT], scalar=gdc[:, 4 * gi_ + i:4 * gi_ + i + 1],
                                                                                      in1=cv[0:64, :], op0=ALU.mult, op1=ALU.add),
                     [rk_, "gdc", cvk], [cvk])
            P.op("act", lambda e, cv=cv: e.activation(out=cv[0:64, :], in_=cv[0:64, :], func=AF.Silu), [cvk], [cvk])
            outs[gi_] = (cv, cvk)
        for gi_, scale_, dst, dk in ((0, 0.125, gq_t[bi], K("q")), (1, 1.0, gk_t[bi], K("k"))):
            cv, cvk = outs[gi_]
            sq, sqk = newtmp()
            P.op("act", lambda e, cv=cv, sq=sq: e.activation(out=sq[0:64, :], in_=cv[0:64, :], func=AF.Square), [cvk], [sqk])
            ps, psk = newps()
            P.mm(ps[0:64, :], ones1[:], sq[0:64, :], True, True, ["ones1", sqk], [psk])
            rsqrt_from_ps(sq[0:64, :], sqk, ps[0:64, :], psk, 1e-6)
            P.op("dve", lambda e, cv=cv, sq=sq, dst=dst, scale_=scale_: e.scalar_tensor_tensor(out=dst[:], in0=cv[0:64, :], scalar=scale_, in1=sq[0:64, :], op0=ALU.mult, op1=ALU.mult),
                 [cvk, sqk], [dk])
        cvv, cvvk = outs[2]
        P.op("act", lambda e: e.activation(out=gv_t[bi][:], in_=cvv[0:64, :], func=AF.Copy), [cvvk], [K("v")])
        P.op("act", lambda e: e.activation(out=gb[bi][:], in_=gb[bi][:], func=AF.Sigmoid), [K("b")], [K("b")])
        P.op("act", lambda e: e.activation(out=ga[bi][:], in_=ga[bi][:], func=AF.Exp, bias=gdc[:, 13:14], scale=1.0), [K("a"), "gdc"], [K("a")])
        P.op("act", lambda e: e.activation(out=ga[bi][:], in_=ga[bi][:], func=AF.Ln, bias=1.0, scale=1.0), [K("a")], [K("a")])
        P.op("act", lambda e: e.activation(out=gal_t[bi][:], in_=ga[bi][:], func=AF.Exp, scale=nAcol[:, 0:1]), [K("a"), "nAcol"], [K("al")])
        P.op("dve", lambda e: e.tensor_tensor(out=gkb_t[bi][:], in0=gk_t[bi][:], in1=gb[bi][:], op=ALU.mult), [K("k"), K("b")], [K("kb")])
        P.op("dve", lambda e: e.scalar_tensor_tensor(out=gnb_t[bi][:], in0=gkb_t[bi][:], scalar=-1.0, in1=gal_t[bi][:], op0=ALU.mult, op1=ALU.mult),
             [K("kb"), K("al")], [K("nb")])
        transpose_to_tok(ktok_g[bi], K("ktok"), gkb_t[bi], K("kb"))
        transpose_to_tok(vtok_g[bi], K("vtok"), gv_t[bi], K("v"))
        P.op("act", lambda e: e.activation(out=gz[bi][:], in_=gz[bi][:], func=AF.Silu), [K("z")], [K("z")])

    def scan_parts(kind, t, aT, akey, wT, wkey, bT, bkey, ktok, kkey, vtok, vkey, rT, rkey, psy, pykey):
        tl = t % RTT
        blk, tb = tl // 128, tl % 128
        prev = (t - 1) % 3
        cur = t % 3
        vm = vmask[kind][t % 3]
        vmk = ("vmask", kind, t % 3)
        kvs = "kv" + kind
        sas = "sa" + kind
        kv_ps = ps_kv[kind][:, 0:64]
        sa_ps = ps_sa[kind][:, 0:64]
        stm = Stmp[kind][t % 2]
        stk = ("Stmp", kind, t % 2)

        def f_vmask():
            P.op("act", lambda e: e.activation(out=vm[:], in_=vtok[:, blk, :], func=AF.Copy, scale=ident[:, tb:tb + 1]), [vkey, "ident"], [vmk])

        def f_kv():
            P.mm(kv_ps, ktok[:, blk, :], vm[:], True, True, [kkey, vmk], [("ps_s", kvs)])

        def f_sa():
            P.mm(sa_ps, aT[:, tl:tl + 1].to_broadcast([64, 64]), St[kind][prev][:], True, True, [akey, ("St", kind, prev)], [("ps_s", sas)])

        def f_op1():
            P.op("dve", lambda e: e.scalar_tensor_tensor(out=stm[:], in0=St[kind][prev][:], scalar=wT[:, tl:tl + 1], in1=kv_ps, op0=ALU.mult, op1=ALU.add),
                 [("St", kind, prev), wkey, ("ps_s", kvs)], [stk])

        def f_op2():
            P.op("dve", lambda e: e.scalar_tensor_tensor(out=St[kind][cur][:], in0=sa_ps, scalar=bT[:, tl:tl + 1], in1=stm[:], op0=ALU.mult, op1=ALU.add),
                 [("ps_s", sas), bkey, stk], [("St", kind, cur)])

        def f_y():
            P.mm(psy[:, tl:tl + 1], St[kind][cur][:], rT[:, tl:tl + 1], True, True, [("St", kind, cur), rkey], [pykey])

        return dict(vmask=f_vmask, kv=f_kv, sa=f_sa, op1=f_op1, op2=f_op2, y=f_y)

    def post_rwkv(n):
        bi = n % 2
        ts = slice(n * RTT, (n + 1) * RTT)
        K = lambda nm: ("rw", nm, bi)
        y = ytile[bi]
        P.op("act", lambda e: e.activation(out=y[:], in_=ps_y["r"][bi][:, 0:RTT], func=AF.Copy), [("ps_y", "r", 0)], [K("y")])
        ps, psk = newps()
        P.mm(ps[0:64, :], ones64[:], y[:], True, True, ["ones64", K("y")], [psk])
        P.op("dve", lambda e: e.tensor_tensor(out=y[:], in0=y[:], in1=ps[0:64, :], op=ALU.subtract), [K("y"), psk], [K("y")])
        sq, sqk = newtmp()
        P.op("act", lambda e: e.activation(out=sq[0:64, :], in_=y[:], func=AF.Square), [K("y")], [sqk])
        ps2, ps2k = newps()
        P.mm(ps2[0:64, :], ones64[:], sq[0:64, :], True, True, ["ones64", sqk], [ps2k])
        rsqrt_from_ps(sq[0:64, :], sqk, ps2[0:64, :], ps2k, 64e-5)
        P.op("dve", lambda e: e.tensor_tensor(out=y[:], in0=y[:], in1=sq[0:64, :], op=ALU.mult), [K("y"), sqk], [K("y")])
        P.op("dve", lambda e: e.tensor_scalar(out=y[:], in0=y[:], scalar1=rwc[:, 8:9], scalar2=rwc[:, 9:10], op0=ALU.mult, op1=ALU.add), [K("y"), "rwc"], [K("y")])
        P.op("dve", lambda e: e.tensor_tensor(out=y[:], in0=y[:], in1=bon_t[bi][:], op=ALU.add), [K("y"), K("bon")], [K("y")])
        P.op("dve", lambda e: e.tensor_tensor(out=yout[bi][:], in0=y[:], in1=g_t[bi][:], op=ALU.mult), [K("y"), K("g")], [K("yout")])
        P.dma("sp", yb_d[:, ts], yout[bi][:], [K("yout")], [fin()])

    def post_gdn(n):
        bi = n % 2
        ts = slice(n * RTT, (n + 1) * RTT)
        K = lambda nm: ("gd", nm, bi)
        o = otile[bi]
        P.op("act", lambda e: e.activation(out=o[:], in_=ps_y["g"][bi][:, 0:RTT], func=AF.Copy), [("ps_y", "g", 0)], [K("o")])
        sq, sqk = newtmp()
        P.op("act", lambda e: e.activation(out=sq[0:64, :], in_=o[:], func=AF.Square), [K("o")], [sqk])
        ps, psk = newps()
        P.mm(ps[0:64, :], ones64[:], sq[0:64, :], True, True, ["ones64", sqk], [psk])
        rsqrt_from_ps(sq[0:64, :], sqk, ps[0:64, :], psk, 1e-6)
        P.op("dve", lambda e: e.scalar_tensor_tensor(out=o[:], in0=o[:], scalar=gdc[:, 14:15], in1=sq[0:64, :], op0=ALU.mult, op1=ALU.mult), [K("o"), sqk, "gdc"], [K("o")])
        P.op("dve", lambda e: e.tensor_tensor(out=oout[bi][:], in0=o[:], in1=gz[bi][:], op=ALU.mult), [K("o"), K("z")], [K("oout")])
        P.dma("sp", yc_d[:, ts], oout[bi][:], [K("oout")], [fin()])

    ntiles = (nsteps + RTT - 1) // RTT

    def prep_all(n):
        cnt["tmp"] = 0
        c = prep_rwkv(n)
        prep_rwkv2(n, c)
        prep_gdn(n)

    prep_all(0)
    for n in range(ntiles):
        stepper = Stepper(P, lambda n=n: prep_all(n + 1)) if n + 1 < ntiles else None
        bi = n % 2
        Kr = lambda nm: ("rw", nm, bi)
        Kg = lambda nm: ("gd", nm, bi)
        nst = min(RTT, nsteps - n * RTT)

        def parts(tl):
            t = n * RTT + tl
            return (scan_parts("r", t, a_t[bi], Kr("a"), w_t[bi], Kr("w"), b_t[bi], Kr("b"), ktok_r[bi], Kr("ktok"), vtok_r[bi], Kr("vtok"), r_t[bi], Kr("r"),
                               ps_y["r"][bi], ("ps_y", "r", 0)),
                    scan_parts("g", t, gk_t[bi], Kg("k"), gal_t[bi], Kg("al"), gnb_t[bi], Kg("nb"), ktok_g[bi], Kg("ktok"), vtok_g[bi], Kg("vtok"), gq_t[bi], Kg("q"),
                               ps_y["g"][bi], ("ps_y", "g", 0)))

        cur_p = parts(0)
        for pp_ in cur_p:
            pp_["vmask"]()
        for pp_ in cur_p:
            pp_["kv"]()
        prev_p = None
        for tl in range(nst):
            nxt_p = parts(tl + 1) if tl + 1 < nst else None
            if nxt_p is not None:
                for pp_ in nxt_p:
                    pp_["vmask"]()
            for pp_ in cur_p:
                pp_["sa"]()
            if prev_p is not None:
                for pp_ in prev_p:
                    pp_["y"]()
            for pp_ in cur_p:
                pp_["op1"]()
            if nxt_p is not None:
                for pp_ in nxt_p:
                    pp_["kv"]()
            for pp_ in cur_p:
                pp_["op2"]()
            prev_p, cur_p = cur_p, nxt_p
            if stepper is not None and tl >= 8:
                stepper.step(1)
        for pp_ in prev_p:
            pp_["y"]()
        if stepper is not None:
            stepper.finish()
        post_rwkv(n)
        post_gdn(n)
    return P.emit(finals)


NB = 64

def core_blocks(c):
    j = c % 4
    out = []
    for k in range(8):
        out += [8 * k + j, 8 * k + 7 - j]
    return out

def core_tokens(c):
    b = c // 4
    pos = np.concatenate([np.arange(g * 128, (g + 1) * 128) for g in core_blocks(c)])
    return b, pos

def fm_cols():
    cols = []
    for j in range(4):
        cols += list(range(64 * j, 64 * j + 64)) + list(range(64 * (j + 4), 64 * (j + 4) + 64))
    cols += list(range(512, 640))
    cols += list(range(768, 1280))
    cols += list(range(1280, 1344))
    cols += list(range(1352, 2248))
    g0 = 2248
    for h in range(4):
        cols += list(range(g0 + 64 * h, g0 + 64 * h + 64)) + list(range(g0 + 256 + 64 * h, g0 + 256 + 64 * h + 64))
    cols += list(range(g0 + 512, g0 + 768))
    assert len(cols) == 2880
    return cols

def tok_cols():
    cols = list(range(640, 768)) + list(range(1344, 1352)) + list(range(3272, 3276)) + list(range(3276, 3280)) + list(range(3016, 3272))
    assert len(cols) == 400
    return cols

def gcol(v):
    return np.ascontiguousarray(v.reshape(8, 128).T)

def core_blocks(c):
    j = c % 4
    return [4 * m + j for m in range(16)]

def tri4_for(c):
    j = c % 4
    q = np.arange(128)[:, None]
    col = np.arange(512)[None, :]
    blk, w = col // 128, col % 128
    valid = (blk < j) | ((blk == j) & (w <= q))
    return np.where(valid, 0.0, -1e30).astype(np.float32)

def q_pair_rows():
    rows = []
    for j in range(4):
        rows += list(range(64 * j, 64 * j + 64)) + list(range(64 * (j + 4), 64 * (j + 4) + 64))
    return rows

def rg_inputs(d, p_b, c, l, vfirst=None):
    j = c % 4
    hs = slice(64 * j, 64 * j + 64)
    pb = p_b[:, 1352:2248]
    pc = p_b[:, 2248:3280]
    T = lambda a: np.ascontiguousarray(a.T.astype(np.float32))
    mu = d["rwkv_mu"][l]
    rwc = np.zeros((64, 16), np.float32)
    rwc[:, 0] = mu[0:256][hs]; rwc[:, 1] = mu[256:512][hs]; rwc[:, 2] = mu[512:768][hs]
    rwc[:, 3] = d["rwkv_w0"][l][hs]; rwc[:, 4] = d["rwkv_a0"][l][hs]; rwc[:, 5] = d["rwkv_k_k"][l][hs]; rwc[:, 6] = d["rwkv_k_a"][l][hs]
    rwc[:, 7] = d["rwkv_r_k"][l][j]; rwc[:, 8] = d["rwkv_gn_w"][l][hs]; rwc[:, 9] = d["rwkv_gn_b"][l][hs]
    if l >= 1:
        rwc[:, 10] = d["rwkv_v0"][l - 1][hs]
        vdown = np.ascontiguousarray(d["rwkv_v_down"][l - 1].reshape(2, 128, 32).transpose(1, 0, 2))
        vup = np.ascontiguousarray(d["rwkv_v_up"][l - 1][:, hs])
    else:
        vdown = np.zeros((128, 2, 32), np.float32)
        vup = np.zeros((32, 64), np.float32)
    gdc = np.zeros((64, 16), np.float32)
    conv = d["gdn_conv"][l]
    for gi in range(3):
        for i in range(4):
            gdc[:, 4 * gi + i] = conv[i, gi * 256 + 64 * j: gi * 256 + 64 * j + 64]
    gdc[:, 12] = d["gdn_a_log"][l][j]; gdc[:, 13] = d["gdn_dt_bias"][l][j]; gdc[:, 14] = d["gdn_o_norm"][l]
    L = p_b.shape[0]
    return dict(
        rw_r=T(pb[:, 0:256][:, hs]), rw_k=T(pb[:, 256:512][:, hs]), rw_v=T(pb[:, 512:768][:, hs]), rw_lora=T(pb[:, 768:896]), rw_vall=T(pb[:, 512:768]),
        rw_cols=rwc, rw_mu_lora=mu[768:896][:, None].copy(), rw_mu_vall=np.ascontiguousarray(mu[512:768].reshape(2, 128).T),
        rw_wup=np.ascontiguousarray(d["rwkv_w_up"][l][:, hs]), rw_aup=np.ascontiguousarray(d["rwkv_a_up"][l][:, hs]), rw_gup=np.ascontiguousarray(d["rwkv_g_up"][l][:, hs]),
        rw_vdown=vdown, rw_vup=vup, vfirst_in=(vfirst if vfirst is not None else np.zeros((64, L), np.float32)),
        gd_q=T(pc[:, 0:256][:, hs]), gd_k=T(pc[:, 256:512][:, hs]), gd_v=T(pc[:, 512:768][:, hs]), gd_z=T(pc[:, 768:1024][:, hs]),
        gd_b=np.ascontiguousarray(pc[:, 1024 + j][None, :]), gd_a=np.ascontiguousarray(pc[:, 1028 + j][None, :]), gd_cols=gdc,
        ident=np.eye(128, dtype=np.float32))


_PROGS = {}


def _prog(name, builder):
    if name not in _PROGS:
        _PROGS[name] = builder()
    return _PROGS[name]


def _mixers(d, l, pT_list, ptok_list, vfirst_list):
    fm, tk = fm_cols(), tok_cols()
    p_full = [np.zeros((L, 3280), np.float32) for _ in range(2)]
    for c in range(8):
        b, pos = core_tokens(c)
        tmp = np.zeros((2048, 3280), np.float32)
        tmp[:, fm] = pT_list[c].T
        tmp[:, tk] = ptok_list[c]
        p_full[b][pos] = tmp
    C = dsa_consts()
    maps = []
    for c in range(8):
        b, pos = core_tokens(c)
        pa = p_full[b]
        maps.append(dict(kT=np.ascontiguousarray(pa[:, 512:640].T), kiT=np.ascontiguousarray(pa[:, 1280:1344].T), vtok=np.ascontiguousarray(pa[:, 640:768]),
                         qT=np.ascontiguousarray(pa[pos][:, q_pair_rows()].T), qiT=np.ascontiguousarray(pa[pos][:, 768:1280].T),
                         wi=np.ascontiguousarray(pa[pos][:, 1344:1352]),
                         cosT=C["cosT"], sinT=C["sinT"], cosq=np.ascontiguousarray(C["cosT"][:, pos]), sinq=np.ascontiguousarray(C["sinT"][:, pos]),
                         gq=np.tile(d["att_q_norm"][l], 2)[:, None].copy(), gk=np.tile(d["att_k_norm"][l], 2)[:, None].copy(),
                         Rm=C["Rm"], tri4=tri4_for(c), I4=C["I4"], sel=C["sel"], bo=C["bo"], pw=C["pw"]))
    res_a = run(_prog("dsa", build_dsa), maps)
    maps = [rg_inputs(d, p_full[c // 4], c, l, vfirst_list[c] if vfirst_list is not None else None) for c in range(8)]
    res_r = run(_prog(f"rg{min(l, 1)}", lambda: build_rg(min(l, 1))), maps)
    yT = []
    for c in range(8):
        b, pos = core_tokens(c)
        y = np.zeros((1024, 2048), np.float32)
        y[0:512] = res_a[c]["yaT"]
        for jj in range(4):
            y[512 + 64 * jj:512 + 64 * jj + 64] = res_r[4 * b + jj]["ybT"][:, pos]
            y[768 + 64 * jj:768 + 64 * jj + 64] = res_r[4 * b + jj]["ycT"][:, pos]
        yT.append(y)
    return yT, [res_r[c]["vfirst_out"] for c in range(8)]


def kernel(**inputs):
    d = {k: np.asarray(v) for k, v in inputs.items()}
    perm = fm_cols() + tok_cols()
    x = d["x"]

    def ffn_w(l, i, tag):
        return {f"g_{tag}": gcol(d["ffn_norm"][l, i]), f"wg_{tag}": np.ascontiguousarray(d["ffn_w_gate"][l, i]),
                f"wu_{tag}": np.ascontiguousarray(d["ffn_w_up"][l, i]), f"wd_{tag}": np.ascontiguousarray(d["ffn_w_down"][l, i])}

    def inp_w(l, tag):
        return {f"g_{tag}": gcol(d["mix_norm"][l]), f"win_{tag}": np.ascontiguousarray(d["w_in"][l][:, perm])}

    w0 = {**ffn_w(0, 0, "f00"), **inp_w(0, "i0")}
    maps = []
    for c in range(8):
        b, pos = core_tokens(c)
        maps.append({"xT": np.ascontiguousarray(x[b, pos, :].T), **w0})
    r0 = run(_prog("P0", lambda: build_P([("ffn", "f00"), ("inproj", "i0")])), maps)
    yT, vfirst = _mixers(d, 0, [r0[c]["pT_i0"] for c in range(8)], [r0[c]["ptok_i0"] for c in range(8)], None)
    w1 = {"wo_o0": np.ascontiguousarray(d["w_out"][0]), **ffn_w(0, 1, "f01"), **ffn_w(1, 0, "f10"), **inp_w(1, "i1")}
    maps = [{"xT": r0[c]["xo"], "yT_o0": yT[c], **w1} for c in range(8)]
    r1 = run(_prog("P1", lambda: build_P([("out", "o0"), ("ffn", "f01"), ("ffn", "f10"), ("inproj", "i1")])), maps)
    yT, _ = _mixers(d, 1, [r1[c]["pT_i1"] for c in range(8)], [r1[c]["ptok_i1"] for c in range(8)], vfirst)
    w2 = {"wo_o1": np.ascontiguousarray(d["w_out"][1]), **ffn_w(1, 1, "f11")}
    maps = [{"xT": r1[c]["xo"], "yT_o1": yT[c], **w2} for c in range(8)]
    r2 = run(_prog("P2", lambda: build_P([("out", "o1"), ("ffn", "f11")])), maps)
    out = np.zeros((2, L, 1024), np.float32)
    for c in range(8):
        b, pos = core_tokens(c)
        out[b, pos, :] = r2[c]["xo"].T
    return out
```

```python
import numpy as np
L = 8192
import concourse.bass as bass
import concourse.mybir as mybir
from concourse.bass_utils import run_bass_kernel_spmd

F32 = mybir.dt.float32
BF16 = mybir.dt.bfloat16
ALU = mybir.AluOpType
AF = mybir.ActivationFunctionType
AX = mybir.AxisListType

ENGS = ("pe", "act", "dve", "pool", "sp")
NDMASEM = 8


class Prog:
    def __init__(self, name="k"):
        self.nc = bass.Bass("TRN2", target_bir_lowering=False)
        self.ops = []
        self.last_write = {}
        self.readers = {}
        self.n_sb = 0
        self.n_ps = 0

    def dram(self, name, shape, dtype=F32, kind="ExternalInput"):
        return self.nc.dram_tensor(name, list(shape), dtype, kind=kind).ap()

    def sb(self, shape, dtype=F32, name=None):
        self.n_sb += 1
        return self.nc.alloc_sbuf_tensor("s_" + (name or f"sb{self.n_sb}"), list(shape), dtype)

    def ps(self, shape, dtype=F32, name=None):
        self.n_ps += 1
        return self.nc.alloc_psum_tensor("p_" + (name or f"ps{self.n_ps}"), list(shape), dtype)

    def op(self, eng, fn, reads=(), writes=(), dma=False):
        import sys, threading
        st = getattr(self, "_stepper", None)
        if st is not None and threading.current_thread() is st.thread:
            st.go.acquire()
            try:
                return self._op(eng, fn, reads, writes, dma, sys._getframe(1))
            finally:
                st.done.release()
        r = self._op(eng, fn, reads, writes, dma, sys._getframe(1))
        hk = getattr(self, "_hook", None)
        if hk is not None:
            hk()
        return r

    def _op(self, eng, fn, reads, writes, dma, fr):
        import sys
        if fr.f_code.co_name in ("mm", "dma"):
            fr = fr.f_back
        self._line = fr.f_lineno
        oid = len(self.ops)
        deps = set()
        for k in reads:
            if k in self.last_write:
                deps.add(self.last_write[k])
        for k in writes:
            if k in self.last_write:
                deps.add(self.last_write[k])
            for r in self.readers.get(k, ()):
                deps.add(r)
        deps.discard(oid)
        self.ops.append(dict(eng=eng, fn=fn, deps=deps, dma=dma, users=0, line=self._line, id=oid))
        for k in reads:
            self.readers.setdefault(k, []).append(oid)
        for k in writes:
            self.last_write[k] = oid
            self.readers[k] = []
        return oid

    def mm(self, out, lhsT, rhs, start, stop, reads, writes):
        self.op("pe", lambda e: e.matmul(out, lhsT, rhs, start=start, stop=stop), reads, writes)

    def dma(self, eng, out, in_, reads, writes, **kw):
        self.op(eng, lambda e: e.dma_start(out=out, in_=in_, **kw), reads, writes, dma=True)

    def emit(self, final_wait_keys=()):
        nc = self.nc
        ops = self.ops
        fdeps = set()
        for k in final_wait_keys:
            if k in self.last_write:
                fdeps.add(self.last_write[k])
        for o in ops:
            if o["eng"] == "pe" and not o["dma"]:
                o["deps"] = {d for d in o["deps"] if not (ops[d]["eng"] == "pe" and not ops[d]["dma"])}
        for o in ops:
            for d in o["deps"]:
                ops[d]["users"] += 1
        for d in fdeps:
            ops[d]["users"] += 1
        from contextlib import ExitStack
        with ExitStack() as st:
            esem = {e: st.enter_context(nc.semaphore(f"s_{e}")) for e in ENGS}
            dsem = {e: [st.enter_context(nc.semaphore(f"d_{e}{i}")) for i in range(NDMASEM)] for e in ("sp", "act", "pool")}
            ecount = {e: 0 for e in ENGS}
            dcount = {e: [0] * NDMASEM for e in dsem}
            drr = {e: 0 for e in dsem}
            for o in ops:
                e = o["eng"]
                if o["dma"]:
                    i = drr[e]
                    drr[e] = (i + 1) % NDMASEM
                    o["prev_ticket"] = (dsem[e][i], dcount[e][i])
                    dcount[e][i] += 16
                    o["ticket"] = (dsem[e][i], dcount[e][i])
                elif o["users"] > 0:
                    ecount[e] += 1
                    o["ticket"] = (esem[e], ecount[e])
                else:
                    o["ticket"] = None
            per_eng = {e: [o for o in ops if o["eng"] == e] for e in ENGS}
            block = st.enter_context(nc.Block())

            self.trace = {e: [] for e in ENGS}
            semname = {id(v): k for k, v in esem.items()}
            for k, lst in dsem.items():
                for i, v in enumerate(lst):
                    semname[id(v)] = f"d_{k}{i}"

            def run_engine(e, eng):
                waited = {}
                tr = self.trace[e]

                def wait(sem, val):
                    key = id(sem)
                    if waited.get(key, 0) >= val:
                        return
                    waited[key] = val
                    tr.append(f"   wait {semname[key]} >= {val}")
                    eng.wait_ge(sem, val)

                for o in per_eng[e]:
                    for d in sorted(o["deps"]):
                        sem, val = ops[d]["ticket"]
                        wait(sem, val)
                    if o["dma"]:
                        psem, pval = o["prev_ticket"]
                        if pval > 0:
                            wait(psem, pval)
                    ins = o["fn"](eng)
                    tr.append(f"op{o['id']} L{o['line']} {'dma' if o['dma'] else ''} -> {semname[id(o['ticket'][0])] + '=' + str(o['ticket'][1]) if o['ticket'] else '-'}")
                    if o["ticket"] is not None:
                        sem, val = o["ticket"]
                        ins.then_inc(sem, 16 if o["dma"] else 1)
                if e == "sp":
                    for d in sorted(fdeps):
                        sem, val = ops[d]["ticket"]
                        wait(sem, val)

            @block.sync
            def _(eng):
                run_engine("sp", eng)

            @block.tensor
            def _(eng):
                run_engine("pe", eng)

            @block.scalar
            def _(eng):
                run_engine("act", eng)

            @block.vector
            def _(eng):
                run_engine("dve", eng)

            @block.gpsimd
            def _(eng):
                run_engine("pool", eng)
        return nc


def run(prog_nc, in_maps, n=8):
    res = run_bass_kernel_spmd(prog_nc, in_maps, core_ids=list(range(n)))
    return res.results


class Stepper:
    def __init__(self, P, fn):
        import threading
        self.P = P
        self.go = threading.Semaphore(0)
        self.done = threading.Semaphore(0)
        self.finished = False
        self.exc = None

        def body():
            try:
                fn()
            except BaseException as e:
                self.exc = e
            self.finished = True
            self.done.release()

        self.thread = threading.Thread(target=body, daemon=True)
        P._stepper = self
        self.thread.start()

    def step(self, k=1):
        for _ in range(k):
            if self.finished:
                return
            self.go.release()
            self.done.acquire()
            if self.finished:
                return

    def finish(self):
        while not self.finished:
            self.go.release()
            self.done.acquire()
        self.P._stepper = None
        if self.exc is not None:
            raise self.exc


D = 1024
DFF = 2816
NT = 2048
TT = 512
NTT = NT // TT
KC = D // 128
FC = DFF // 128
FM_COLS = 2880
TOK_COLS = 400
EPS = 1e-6


def build_P(stages):
    P = Prog()
    nc = P.nc
    xT_d = P.dram("xT", [D, NT])
    xo_d = P.dram("xo", [D, NT], kind="ExternalOutput")
    xT = P.sb([128, KC, NT], F32, "xT")
    hT = P.sb([128, KC, NT], BF16, "hT")
    ones = P.sb([128, 128], BF16, "ones")
    rstd = [P.sb([128, TT], F32, f"rstd{i}") for i in range(2)]
    sq = [P.sb([128, TT], BF16, f"sq{i}") for i in range(2)]
    wA = [P.sb([128, KC, 512], BF16, f"wA{i}") for i in range(2)]
    wB = [P.sb([128, KC, 512], BF16, f"wB{i}") for i in range(2)]
    wD = [P.sb([128, 4, D], BF16, f"wD{i}") for i in range(2)]
    actT = [P.sb([128, 4, TT], BF16, f"actT{i}") for i in range(2)]
    sg = [P.sb([128, TT], F32, f"sg{i}") for i in range(2)]
    stg = [P.sb([128, TT], F32, f"stg{i}") for i in range(2)]
    ps_a = [P.ps([128, TT], F32, f"psa{i}") for i in range(2)]
    ps_b = [P.ps([128, TT], F32, f"psb{i}") for i in range(2)]
    ps_d = [P.ps([128, TT], F32, f"psd{i}") for i in range(4)]

    P.op("dve", lambda e: e.memset(ones[:], 1.0 / D), writes=["ones"])
    for kc in range(KC):
        P.dma("sp", xT[:, kc, :], xT_d[kc * 128:(kc + 1) * 128, :], reads=[], writes=[("xT", kc, t) for t in range(NTT)])

    finals = []

    def fkey():
        k = ("fin", len(finals))
        finals.append(k)
        return k

    cnt = dict(sq=0, rstd=0, w=0, wd=0, act=0, sg=0, stg=0, psa=0, psd=0)

    def norm_to_hT(gcol, tag):
        for t in range(NTT):
            ts = slice(t * TT, (t + 1) * TT)
            pi = cnt["psa"] % 2
            cnt["psa"] += 1
            for kc in range(KC):
                si = cnt["sq"] % 2
                cnt["sq"] += 1
                P.op("act", lambda e, kc=kc, si=si, ts=ts: e.activation(out=sq[si][:], in_=xT[:, kc, ts], func=AF.Square),
                     reads=[("xT", kc, t)], writes=[("sq", si)])
                P.mm(ps_a[pi][:], ones[:], sq[si][:], kc == 0, kc == KC - 1, reads=["ones", ("sq", si)], writes=[("psa", pi)])
            ri = cnt["rstd"] % 2
            cnt["rstd"] += 1
            P.op("act", lambda e, ri=ri, pi=pi: e.activation(out=rstd[ri][:], in_=ps_a[pi][:], func=AF.Sqrt, bias=EPS, scale=1.0),
                 reads=[("psa", pi)], writes=[("rstd", ri)])
            P.op("dve", lambda e, ri=ri: e.reciprocal(out=rstd[ri][:], in_=rstd[ri][:]),
                 reads=[("rstd", ri)], writes=[("rstd", ri)])
            for kc in range(KC):
                P.op("dve", lambda e, kc=kc, ri=ri, ts=ts: e.scalar_tensor_tensor(out=hT[:, kc, ts], in0=xT[:, kc, ts], scalar=gcol[:, kc:kc + 1],
                                                                              in1=rstd[ri][:], op0=ALU.mult, op1=ALU.mult),
                     reads=[("xT", kc, t), ("rstd", ri), ("g", tag)], writes=[("hT", kc, t)])

    def load_gcol(name, tag):
        g_d = P.dram(name, [128, KC])
        g = P.sb([128, KC], F32, "g_" + name)
        P.dma("sp", g[:], g_d[:, :], reads=[], writes=[("g", tag)])
        return g

    def ffn(tag):
        g = load_gcol(f"g_{tag}", tag)
        wg_d = P.dram(f"wg_{tag}", [D, DFF])
        wu_d = P.dram(f"wu_{tag}", [D, DFF])
        wd_d = P.dram(f"wd_{tag}", [DFF, D])
        wg_v = wg_d.rearrange("(kc p) f -> p kc f", p=128)
        wu_v = wu_d.rearrange("(kc p) f -> p kc f", p=128)
        wd_v = wd_d.rearrange("(fc p) d -> p fc d", p=128)
        norm_to_hT(g, tag)
        groups = []
        f0 = 0
        while f0 < FC:
            n = min(4, FC - f0)
            groups.append((f0, n))
            f0 += n

        def load_group(gi):
            f0, n = groups[gi]
            wi = cnt["w"] % 2
            cnt["w"] += 1
            P.dma("pool", wA[wi][:, :, :n * 128], wg_v[:, :, f0 * 128:(f0 + n) * 128], reads=[], writes=[("wA", wi)])
            P.dma("pool", wB[wi][:, :, :n * 128], wu_v[:, :, f0 * 128:(f0 + n) * 128], reads=[], writes=[("wB", wi)])
            P.dma("pool", wD[wi][:, :n, :], wd_v[:, f0:f0 + n, :], reads=[], writes=[("wD", wi)])
            return wi

        nxt = load_group(0)
        for gi, (f0, n) in enumerate(groups):
            wi = nxt
            if gi + 1 < len(groups):
                nxt = load_group(gi + 1)
            for t in range(NTT):
                ts = slice(t * TT, (t + 1) * TT)
                ai = cnt["act"] % 2
                cnt["act"] += 1
                for f in range(n):
                    pi = cnt["psa"] % 2
                    cnt["psa"] += 1
                    for kc in range(KC):
                        P.mm(ps_a[pi][:], wA[wi][:, kc, f * 128:(f + 1) * 128], hT[:, kc, ts], kc == 0, kc == KC - 1,
                             reads=[("wA", wi), ("hT", kc, t)], writes=[("psa", pi)])
                    for kc in range(KC):
                        P.mm(ps_b[pi][:], wB[wi][:, kc, f * 128:(f + 1) * 128], hT[:, kc, ts], kc == 0, kc == KC - 1,
                             reads=[("wB", wi), ("hT", kc, t)], writes=[("psb", pi)])
                    si = cnt["sg"] % 2
                    cnt["sg"] += 1
                    P.op("act", lambda e, si=si, pi=pi: e.activation(out=sg[si][:], in_=ps_a[pi][:], func=AF.Silu),
                         reads=[("psa", pi)], writes=[("sg", si)])
                    P.op("dve", lambda e, si=si, pi=pi, ai=ai, f=f: e.tensor_tensor(out=actT[ai][:, f, :], in0=sg[si][:], in1=ps_b[pi][:], op=ALU.mult),
                         reads=[("sg", si), ("psb", pi)], writes=[("actT", ai, f)])
                for dc in range(KC):
                    di = cnt["psd"] % 4
                    cnt["psd"] += 1
                    for f in range(n):
                        P.mm(ps_d[di][:], wD[wi][:, f, dc * 128:(dc + 1) * 128], actT[ai][:, f, :], f == 0, f == n - 1,
                             reads=[("wD", wi), ("actT", ai, f)], writes=[("psd", di)])
                    P.op("dve", lambda e, di=di, dc=dc, ts=ts: e.scalar_tensor_tensor(out=xT[:, dc, ts], in0=ps_d[di][:], scalar=0.5, in1=xT[:, dc, ts],
                                                                                  op0=ALU.mult, op1=ALU.add),
                         reads=[("psd", di), ("xT", dc, t)], writes=[("xT", dc, t)])

    def outproj(tag):
        yT_d = P.dram(f"yT_{tag}", [D, NT])
        wo_d = P.dram(f"wo_{tag}", [D, D])
        wo_v = wo_d.rearrange("(kc p) d -> p kc d", p=128)
        for h in range(2):
            wi = cnt["w"] % 2
            cnt["w"] += 1
            P.dma("pool", wA[wi][:, :, :], wo_v[:, :, h * 512:(h + 1) * 512], reads=[], writes=[("wA", wi)])
            if h == 0:
                for kc in range(KC):
                    P.dma("pool", hT[:, kc, :], yT_d[kc * 128:(kc + 1) * 128, :], reads=[], writes=[("hT", kc, t) for t in range(NTT)])
            for t in range(NTT):
                ts = slice(t * TT, (t + 1) * TT)
                for dd in range(4):
                    dc = h * 4 + dd
                    di = cnt["psd"] % 4
                    cnt["psd"] += 1
                    for kc in range(KC):
                        P.mm(ps_d[di][:], wA[wi][:, kc, dd * 128:(dd + 1) * 128], hT[:, kc, ts], kc == 0, kc == KC - 1,
                             reads=[("wA", wi), ("hT", kc, t)], writes=[("psd", di)])
                    P.op("dve", lambda e, di=di, dc=dc, ts=ts: e.tensor_tensor(out=xT[:, dc, ts], in0=ps_d[di][:], in1=xT[:, dc, ts], op=ALU.add),
                         reads=[("psd", di), ("xT", dc, t)], writes=[("xT", dc, t)])

    def inproj(tag):
        g = load_gcol(f"g_{tag}", tag)
        win_d = P.dram(f"win_{tag}", [D, FM_COLS + TOK_COLS])
        win_v = win_d.rearrange("(kc p) c -> p kc c", p=128)
        pT_d = P.dram(f"pT_{tag}", [FM_COLS, NT], kind="ExternalOutput")
        ptok_d = P.dram(f"ptok_{tag}", [NT, TOK_COLS], kind="ExternalOutput")
        norm_to_hT(g, tag)
        c0 = 0
        while c0 < FM_COLS:
            n = min(512, FM_COLS - c0)
            wi = cnt["w"] % 2
            cnt["w"] += 1
            P.dma("pool", wA[wi][:, :, :n], win_v[:, :, c0:c0 + n], reads=[], writes=[("wA", wi)])
            for t in range(NTT):
                ts = slice(t * TT, (t + 1) * TT)
                for m0 in range(0, n, 128):
                    m = min(128, n - m0)
                    di = cnt["psd"] % 4
                    cnt["psd"] += 1
                    for kc in range(KC):
                        P.mm(ps_d[di][:m, :], wA[wi][:, kc, m0:m0 + m], hT[:, kc, ts], kc == 0, kc == KC - 1,
                             reads=[("wA", wi), ("hT", kc, t)], writes=[("psd", di)])
                    si = cnt["stg"] % 2
                    cnt["stg"] += 1
                    P.op("act", lambda e, si=si, di=di, m=m: e.activation(out=stg[si][:m, :], in_=ps_d[di][:m, :], func=AF.Copy),
                         reads=[("psd", di)], writes=[("stg", si)])
                    P.dma("sp", pT_d[c0 + m0:c0 + m0 + m, ts], stg[si][:m, :], reads=[("stg", si)], writes=[fkey()])
            c0 += n
        wi = cnt["w"] % 2
        cnt["w"] += 1
        P.dma("pool", wA[wi][:, :, :TOK_COLS], win_v[:, :, FM_COLS:FM_COLS + TOK_COLS], reads=[], writes=[("wA", wi)])
        for b in range(NT // 128):
            t = b // 4
            di = cnt["psd"] % 4
            cnt["psd"] += 1
            for kc in range(KC):
                P.mm(ps_d[di][:, :TOK_COLS], hT[:, kc, b * 128:(b + 1) * 128], wA[wi][:, kc, :TOK_COLS], kc == 0, kc == KC - 1,
                     reads=[("wA", wi), ("hT", kc, t)], writes=[("psd", di)])
            si = cnt["stg"] % 2
            cnt["stg"] += 1
            P.op("act", lambda e, si=si, di=di: e.activation(out=stg[si][:, :TOK_COLS], in_=ps_d[di][:, :TOK_COLS], func=AF.Copy),
                 reads=[("psd", di)], writes=[("stg", si)])
            P.dma("sp", ptok_d[b * 128:(b + 1) * 128, :], stg[si][:, :TOK_COLS], reads=[("stg", si)], writes=[fkey()])

    for kind, tag in stages:
        if kind == "ffn":
            ffn(tag)
        elif kind == "out":
            outproj(tag)
        elif kind == "inproj":
            inproj(tag)
    for kc in range(KC):
        P.dma("sp", xo_d[kc * 128:(kc + 1) * 128, :], xT[:, kc, :], reads=[("xT", kc, t) for t in range(NTT)], writes=[("xo", kc)])
        finals.append(("xo", kc))
    return P.emit(final_wait_keys=finals)


NQB = 16
NIT = 22
NEG = -30000.0


def dsa_consts():
    Rm = np.zeros((128, 128), np.float32)
    for blk in (0, 64):
        for dp in range(32):
            Rm[blk + dp + 32, blk + dp] = -1.0
            Rm[blk + dp, blk + dp + 32] = 1.0
    tri = np.where(np.arange(128)[None, :] > np.arange(128)[:, None], -1e30, 0.0).astype(np.float32)
    I4 = np.tile(np.eye(128, dtype=np.float32), (1, 4))
    sel = np.zeros((65, 64), np.float32)
    sel[64, :] = 1.0
    bo = np.zeros((128, 128), np.float32)
    bo[:64, :64] = 1.0 / 64
    bo[64:, 64:] = 1.0 / 64
    pw = np.tile((2.0 ** -(np.arange(NIT) + 1.0))[None, :], (128, 1)).astype(np.float32)
    inv = 10000.0 ** (-np.arange(32, dtype=np.float32) / 32)
    ang = np.arange(L, dtype=np.float32)[None, :] * inv[:, None]
    cos = np.cos(ang).astype(np.float32)
    sin = np.sin(ang).astype(np.float32)
    cosT = np.tile(cos, (4, 1))
    sinT = np.tile(sin, (4, 1))
    return dict(Rm=Rm, tri=tri, I4=I4, sel=sel, bo=bo, pw=pw, cosT=cosT, sinT=sinT)


def build_dsa():
    P = Prog()
    kT_d = P.dram("kT", [128, L])
    kiT_d = P.dram("kiT", [64, L])
    v_d = P.dram("vtok", [L, 128])
    qT_d = P.dram("qT", [512, NQB * 128])
    qiT_d = P.dram("qiT", [512, NQB * 128])
    wi_d = P.dram("wi", [NQB * 128, 8])
    cos_d = P.dram("cosT", [128, L])
    sin_d = P.dram("sinT", [128, L])
    cosq_d = P.dram("cosq", [128, NQB * 128])
    sinq_d = P.dram("sinq", [128, NQB * 128])
    gq_d = P.dram("gq", [128, 1])
    gk_d = P.dram("gk", [128, 1])
    Rm_d = P.dram("Rm", [128, 128])
    tri_d = P.dram("tri4", [128, 512])
    I4_d = P.dram("I4", [128, 512])
    sel_d = P.dram("sel", [65, 64])
    bo_d = P.dram("bo", [128, 128])
    pw_d = P.dram("pw", [128, NIT])
    ya_d = P.dram("yaT", [512, NQB * 128], kind="ExternalOutput")

    Rm = P.sb([128, 128], BF16, "Rm")
    tri = P.sb([128, 512], F32, "tri")
    I4 = P.sb([128, 512], BF16, "I4")
    sel = P.sb([65, 64], F32, "sel")
    bo = P.sb([128, 128], BF16, "bo")
    pw = P.sb([128, NIT], F32, "pw")
    gq = P.sb([128, 1], F32, "gq")
    gk = P.sb([128, 1], F32, "gk")
    P.dma("pool", Rm[:], Rm_d[:, :], [], ["Rm"])
    P.dma("pool", I4[:], I4_d[:, :], [], ["I4"])
    P.dma("pool", bo[:], bo_d[:, :], [], ["bo"])
    P.dma("sp", tri[:], tri_d[:, :], [], ["tri"])
    P.dma("sp", sel[:], sel_d[:, :], [], ["sel"])
    P.dma("sp", pw[:], pw_d[:, :], [], ["pw"])
    P.dma("sp", gq[:], gq_d[:, :], [], ["gq"])
    P.dma("sp", gk[:], gk_d[:, :], [], ["gk"])

    K_fin = P.sb([128, L], BF16, "K_fin")
    KI_fin = P.sb([128, L], BF16, "KI_fin")
    V1 = P.sb([128, 64, 2, 65], BF16, "V1")
    acc = P.sb([128, L], F32, "acc")
    mbuf = [P.sb([128, L], BF16, f"mb{i}") for i in range(2)]
    Q_fin = [P.sb([128, 4, 128], BF16, f"Q_fin{i}") for i in range(2)]
    QI_fin = P.sb([128, 4, 128], BF16, "QI_fin")
    wi = P.sb([128, 8], F32, "wi")
    raw = [P.sb([128, 512], F32, f"raw{i}") for i in range(2)]
    cs = [P.sb([128, 512], F32, f"cs{i}") for i in range(2)]
    sn = [P.sb([128, 512], F32, f"sn{i}") for i in range(2)]
    sqb = P.sb([128, 512], BF16, "sqb")
    rs = P.sb([128, 512], F32, "rs")
    kn = P.sb([128, 512], F32, "kn")
    knb = P.sb([128, 512], BF16, "knb")
    t1 = P.sb([128, 512], F32, "t1")
    t2 = P.sb([128, 512], F32, "t2")
    rbuf = [P.sb([128, 512], F32, f"rbuf{i}") for i in range(2)]
    pexp = [P.sb([128, 512], BF16, f"pexp{i}") for i in range(2)]
    o_sb = [P.sb([65, 512], F32, f"o_sb{i}") for i in range(2)]
    rden = P.sb([64, 512], F32, "rden")
    ya_t = [P.sb([64, 512], F32, f"ya_t{i}") for i in range(2)]
    col = {n: P.sb([128, 1], F32, "c_" + n) for n in ("mx", "mn", "lo", "rng", "mid", "cnt", "cs_", "thr0")}
    steps = P.sb([128, NIT], F32, "steps")

    ps_prep = P.ps([128, 512], F32, "ps_prep")
    ps_i = [P.ps([128, 512], F32, f"ps_i{i}") for i in range(2)]
    ps_l = [P.ps([128, 512], F32, f"ps_l{i}") for i in range(2)]
    ps_o = [P.ps([128, 512], F32, f"ps_o{i}") for i in range(2)]
    ps_den = P.ps([64, 512], F32, "ps_den")

    cnt = dict(raw=0, r=0, pi=0, pl=0, po=0, pe=0, ya=0)
    finals = []

    P.op("dve", lambda e: e.memset(col["thr0"][:], -1e29), writes=["thr0"])

    def rope_pipeline(src_key, src, cos_ap, sin_ap, cs_keys, out_ap, out_keys, n, norm_g=None, gkey=None):
        if norm_g is not None:
            P.op("act", lambda e: e.activation(out=sqb[:, :n], in_=src, func=AF.Square), [src_key], ["sqb"])
            P.mm(ps_prep[:, :n], bo[:], sqb[:, :n], True, True, ["bo", "sqb"], ["ps_prep"])
            P.op("act", lambda e: e.activation(out=rs[:, :n], in_=ps_prep[:, :n], func=AF.Sqrt, bias=1e-6, scale=1.0), ["ps_prep"], ["rs"])
            P.op("dve", lambda e: e.reciprocal(out=rs[:, :n], in_=rs[:, :n]), ["rs"], ["rs"])
            P.op("dve", lambda e: e.scalar_tensor_tensor(out=kn[:, :n], in0=src, scalar=norm_g[:, 0:1], in1=rs[:, :n], op0=ALU.mult, op1=ALU.mult),
                 [src_key, "rs", gkey], ["kn"])
            cur, cur_key = kn[:, :n], "kn"
        else:
            cur, cur_key = src, src_key
        P.op("act", lambda e: e.activation(out=knb[:, :n], in_=cur, func=AF.Copy), [cur_key], ["knb"])
        P.mm(ps_prep[:, :n], Rm[:], knb[:, :n], True, True, ["Rm", "knb"], ["ps_prep"])
        P.op("dve", lambda e: e.tensor_tensor(out=t1[:, :n], in0=cur, in1=cos_ap, op=ALU.mult), [cur_key] + cs_keys, ["t1"])
        P.op("dve", lambda e: e.tensor_tensor(out=t2[:, :n], in0=ps_prep[:, :n], in1=sin_ap, op=ALU.mult), ["ps_prep"] + cs_keys, ["t2"])
        P.op("dve", lambda e: e.tensor_tensor(out=out_ap, in0=t1[:, :n], in1=t2[:, :n], op=ALU.add), ["t1", "t2"], out_keys)

    v_v = v_d.rearrange("(kt p) (g d) -> p kt g d", p=128, g=2)
    for q4 in range(4):
        P.dma("pool", V1[:, q4 * 16:(q4 + 1) * 16, 0, 0:64], v_v[:, q4 * 16:(q4 + 1) * 16, 0, :], [], [("V1", q4)])
        P.dma("pool", V1[:, q4 * 16:(q4 + 1) * 16, 1, 0:64], v_v[:, q4 * 16:(q4 + 1) * 16, 1, :], [], [("V1b", q4)])
    P.op("dve", lambda e: e.memset(V1[:, :, :, 64:65], 1.0), [], ["V1ones"])
    for kt in range(L // 512):
        ks = slice(kt * 512, (kt + 1) * 512)
        ri = cnt["raw"] % 2
        cnt["raw"] += 1
        P.dma("sp", cs[ri][:], cos_d[:, ks], [], [("cs", ri)])
        P.dma("sp", sn[ri][:], sin_d[:, ks], [], [("sn", ri)])
        P.dma("sp", raw[ri][:], kT_d[:, ks], [], [("raw", ri)])
        rope_pipeline(("raw", ri), raw[ri][:], cs[ri][:], sn[ri][:], [("cs", ri), ("sn", ri)], K_fin[:, ks], [("K", kt)], 512, norm_g=gk, gkey="gk")
        ri2 = cnt["raw"] % 2
        cnt["raw"] += 1
        P.dma("sp", raw[ri2][0:64, :], kiT_d[:, ks], [], [("raw", ri2)])
        P.dma("sp", raw[ri2][64:128, :], kiT_d[:, ks], [], [("raw", ri2, "b")])
        rope_pipeline(("raw", ri2), raw[ri2][:], cs[ri][:], sn[ri][:], [("cs", ri), ("sn", ri), ("raw", ri2, "b")], KI_fin[:, ks], [("KI", kt)], 512)

    qT_v = qT_d.rearrange("(j p) t -> p j t", p=128)
    qiT_v = qiT_d.rearrange("(j p) t -> p j t", p=128)
    ya_v = ya_d.rearrange("(h d) t -> d h t", d=64)

    def front(lb):
        S = 512 * (lb + 1)
        qs = slice(lb * 128, (lb + 1) * 128)
        nkt = S // 128
        ri = cnt["raw"] % 2
        cnt["raw"] += 1
        P.dma("sp", raw[ri][:].rearrange("p (j t) -> p j t", j=4), qT_v[:, :, qs], [], [("raw", ri)])
        for j in range(4):
            P.dma("sp", cs[ri][:, j * 128:(j + 1) * 128], cosq_d[:, qs], [], [("cs", ri)])
            P.dma("sp", sn[ri][:, j * 128:(j + 1) * 128], sinq_d[:, qs], [], [("sn", ri)])
        cskeys = [("cs", ri), ("sn", ri)]
        rope_pipeline(("raw", ri), raw[ri][:], cs[ri][:], sn[ri][:], cskeys, Q_fin[lb % 2][:].rearrange("p j t -> p (j t)"), [("Q", lb % 2)], 512, norm_g=gq, gkey="gq")
        ri2 = cnt["raw"] % 2
        cnt["raw"] += 1
        P.dma("sp", raw[ri2][:].rearrange("p (j t) -> p j t", j=4), qiT_v[:, :, qs], [], [("raw", ri2)])
        rope_pipeline(("raw", ri2), raw[ri2][:], cs[ri][:], sn[ri][:], cskeys, QI_fin[:].rearrange("p j t -> p (j t)"), ["QI"], 512)
        P.dma("sp", wi[:], wi_d[qs, :], [], ["wi"])

        nst = (S + 511) // 512
        for st in range(nst):
            w = min(512, S - st * 512)
            ss = slice(st * 512, st * 512 + w)
            kts = sorted(set([(st * 512) // 512]))
            for h in range(8):
                m, half = h // 2, h % 2
                pi = cnt["pi"] % 2
                cnt["pi"] += 1
                hp = slice(64 * half, 64 * half + 64)
                P.mm(ps_i[pi][:, :w], QI_fin[hp, m, :], KI_fin[hp, ss], True, True, ["QI", ("KI", st)], [("ps_i", pi)])
                r_i = cnt["r"] % 2
                cnt["r"] += 1
                P.op("act", lambda e, r_i=r_i, pi=pi, w=w: e.activation(out=rbuf[r_i][:, :w], in_=ps_i[pi][:, :w], func=AF.Relu),
                     [("ps_i", pi)], [("rbuf", r_i)])
                if h == 0:
                    P.op("dve", lambda e, r_i=r_i, w=w, ss=ss: e.tensor_scalar(out=acc[:, ss], in0=rbuf[r_i][:, :w], scalar1=wi[:, 0:1], scalar2=None, op0=ALU.mult),
                         [("rbuf", r_i), "wi"], [("acc", st)])
                else:
                    P.op("dve", lambda e, r_i=r_i, w=w, ss=ss, h=h: e.scalar_tensor_tensor(out=acc[:, ss], in0=rbuf[r_i][:, :w], scalar=wi[:, h:h + 1], in1=acc[:, ss],
                                                                                         op0=ALU.mult, op1=ALU.add),
                         [("rbuf", r_i), "wi", ("acc", st)], [("acc", st)])
        acc_keys = [("acc", st) for st in range(nst)]
        if lb == 0:
            P.op("dve", lambda e: e.tensor_tensor(out=t1[:], in0=acc[:, 0:512], in1=tri[:], op=ALU.subtract), [("acc", 0), "tri"], ["t1"])
            P.op("dve", lambda e: e.tensor_reduce(out=col["lo"][:], in_=t1[:], axis=AX.X, op=ALU.min), ["t1"], ["lo"])
        else:
            P.op("dve", lambda e, S=S: e.tensor_reduce(out=col["lo"][:], in_=acc[:, :S - 512], axis=AX.X, op=ALU.min), acc_keys, ["lo"])
        P.op("dve", lambda e, S=S: e.tensor_tensor(out=acc[:, S - 512:S], in0=acc[:, S - 512:S], in1=tri[:], op=ALU.add),
             [("acc", nst - 1), "tri"], [("acc", nst - 1)])
        if True:
            P.op("dve", lambda e, S=S: e.tensor_reduce(out=col["mx"][:], in_=acc[:, :S], axis=AX.X, op=ALU.max), acc_keys, ["mx"])
            P.op("dve", lambda e: e.tensor_tensor(out=col["rng"][:], in0=col["mx"][:], in1=col["lo"][:], op=ALU.subtract), ["mx", "lo"], ["rng"])
            P.op("dve", lambda e: e.tensor_scalar(out=steps[:], in0=pw[:], scalar1=col["rng"][:, 0:1], scalar2=None, op0=ALU.mult), ["pw", "rng"], ["steps"])
            for k in range(NIT):
                P.op("dve", lambda e, k=k: e.tensor_tensor(out=col["mid"][:], in0=col["lo"][:], in1=steps[:, k:k + 1], op=ALU.add), ["lo", "steps"], ["mid"])
                P.op("dve", lambda e, S=S: e.tensor_scalar(out=mbuf[lb % 2][:, :S], in0=acc[:, :S], scalar1=col["mid"][:, 0:1], scalar2=None, op0=ALU.is_ge,
                                                          op1=ALU.add, accum_out=col["cnt"][:]),
                     acc_keys + ["mid"], [("mb", lb % 2), "cnt"])
                P.op("dve", lambda e: e.memset(col["mn"][:], 0.0), [], ["mn"])
                P.op("dve", lambda e, k=k: e.scalar_tensor_tensor(out=col["cs_"][:], in0=col["cnt"][:], scalar=255.5, in1=steps[:, k:k + 1], op0=ALU.is_ge, op1=ALU.mult),
                     ["cnt", "steps"], ["cs_"])
                P.op("dve", lambda e: e.tensor_tensor(out=col["lo"][:], in0=col["lo"][:], in1=col["cs_"][:], op=ALU.add), ["lo", "cs_"], ["lo"])
            thr, thr_key = col["lo"], "lo"
        P.op("dve", lambda e, S=S, thr=thr: e.tensor_scalar(out=mbuf[lb % 2][:, :S], in0=acc[:, :S], scalar1=thr[:, 0:1], scalar2=NEG, op0=ALU.is_lt, op1=ALU.mult),
             acc_keys + [thr_key], [("mb", lb % 2)])

    def attention(lb):
        S = 512 * (lb + 1)
        qs = slice(lb * 128, (lb + 1) * 128)
        nkt = S // 128
        for g in range(2):
            gp = slice(64 * g, 64 * g + 64)
            po = cnt["po"] % 2
            cnt["po"] += 1
            for kt in range(nkt):
                pl = cnt["pl"] % 2
                cnt["pl"] += 1
                P.mm(ps_l[pl][:], K_fin[gp, kt * 128:(kt + 1) * 128], Q_fin[lb % 2][gp, :, :].rearrange("p j t -> p (j t)"), True, False,
                     [("K", kt // 4), ("Q", lb % 2)], [("ps_l", pl)])
                P.mm(ps_l[pl][:], mbuf[lb % 2][:, kt * 128:(kt + 1) * 128], I4[:], False, True, [("mb", lb % 2), "I4"], [("ps_l", pl)])
                pe = cnt["pe"] % 2
                cnt["pe"] += 1
                P.op("act", lambda e, pe=pe, pl=pl: e.activation(out=pexp[pe][:], in_=ps_l[pl][:], func=AF.Exp, scale=0.125),
                     [("ps_l", pl)], [("pexp", pe)])
                P.mm(ps_o[po][:65, :], V1[:, kt, g, :], pexp[pe][:], kt == 0, kt == nkt - 1,
                     [("V1", kt // 16), ("V1b", kt // 16), "V1ones", ("pexp", pe)], [("ps_o", po)])
            P.op("act", lambda e, po=po: e.activation(out=o_sb[po][:], in_=ps_o[po][:65, :], func=AF.Copy), [("ps_o", po)], [("o_sb", po)])
            P.mm(ps_den[:], sel[:], o_sb[po][:], True, True, ["sel", ("o_sb", po)], ["ps_den"])
            P.op("dve", lambda e: e.reciprocal(out=rden[:], in_=ps_den[:]), ["ps_den"], ["rden"])
            yi = cnt["ya"] % 2
            cnt["ya"] += 1
            P.op("dve", lambda e, yi=yi, po=po: e.tensor_tensor(out=ya_t[yi][:], in0=o_sb[po][0:64, :], in1=rden[:], op=ALU.mult),
                 [("o_sb", po), "rden"], [("ya_t", yi)])
            fk = ("fin", len(finals))
            finals.append(fk)
            P.dma("sp", ya_v[:, 4 * g:4 * g + 4, qs], ya_t[yi][:].rearrange("p (j t) -> p j t", j=4), [("ya_t", yi)], [fk])

    front(0)
    for lb in range(NQB):
        stp = Stepper(P, lambda lb=lb: attention(lb))
        if lb + 1 < NQB:
            n_att = 2 * (4 * (lb + 1)) * 4 + 16
            n_front = 60 + 24 * (lb + 2) + 5 * NIT + 8
            P._hook_k = max(1, -(-n_att // n_front))
            P._hook = lambda: stp.step(P._hook_k)
            front(lb + 1)
            P._hook = None
        stp.finish()
    return P.emit(finals)


RTT = 256
RG_NBLK = RTT // 128
RG_NTT = L // RTT


def build_rg(layer, nsteps=L):
    P = Prog()
    f32 = F32
    rw_r_d = P.dram("rw_r", [64, L])
    rw_k_d = P.dram("rw_k", [64, L])
    rw_v_d = P.dram("rw_v", [64, L])
    rw_lora_d = P.dram("rw_lora", [128, L])
    rw_vall_d = P.dram("rw_vall", [256, L])
    rwc_d = P.dram("rw_cols", [64, 16])
    rw_mul_d = P.dram("rw_mu_lora", [128, 1])
    rw_muv_d = P.dram("rw_mu_vall", [128, 2])
    rw_wup_d = P.dram("rw_wup", [32, 64])
    rw_aup_d = P.dram("rw_aup", [32, 64])
    rw_gup_d = P.dram("rw_gup", [64, 64])
    rw_vdown_d = P.dram("rw_vdown", [128, 2, 32])
    rw_vup_d = P.dram("rw_vup", [32, 64])
    vfirst_in_d = P.dram("vfirst_in", [64, L])
    vfirst_out_d = P.dram("vfirst_out", [64, L], kind="ExternalOutput")
    gd_q_d = P.dram("gd_q", [64, L])
    gd_k_d = P.dram("gd_k", [64, L])
    gd_v_d = P.dram("gd_v", [64, L])
    gd_z_d = P.dram("gd_z", [64, L])
    gd_b_d = P.dram("gd_b", [1, L])
    gd_a_d = P.dram("gd_a", [1, L])
    gdc_d = P.dram("gd_cols", [64, 16])
    ident_d = P.dram("ident", [128, 128])
    yb_d = P.dram("ybT", [64, L], kind="ExternalOutput")
    yc_d = P.dram("ycT", [64, L], kind="ExternalOutput")

    ident = P.sb([128, 128], f32, "ident")
    ones64 = P.sb([64, 64], f32, "ones64")
    ones1 = P.sb([64, 64], f32, "ones1")
    rwc = P.sb([64, 16], f32, "rwc")
    gdc = P.sb([64, 16], f32, "gdc")
    mul = P.sb([128, 1], f32, "mul")
    muv = P.sb([128, 2], f32, "muv")
    wup = P.sb([32, 64], f32, "wup")
    aup = P.sb([64, 64], f32, "aup")
    gup = P.sb([128, 64], f32, "gup")
    vdown = P.sb([128, 2, 32], f32, "vdown")
    vup = P.sb([32, 64], f32, "vup")
    Acol = P.sb([64, 1], f32, "Acol")
    for sbt, dt_, key in ((ident, ident_d, "ident"), (rwc, rwc_d, "rwc"), (gdc, gdc_d, "gdc"), (mul, rw_mul_d, "mul"), (muv, rw_muv_d, "muv"),
                          (wup, rw_wup_d, "wup"), (vup, rw_vup_d, "vup")):
        P.dma("sp", sbt[:], dt_[:, :], [], [key])
    P.dma("sp", vdown[:], rw_vdown_d[:, :, :], [], ["vdown"])
    P.dma("sp", aup[32:64, :], rw_aup_d[:, :], [], ["aup"])
    P.dma("sp", gup[64:128, :], rw_gup_d[:, :], [], ["gup"])
    P.op("dve", lambda e: e.memset(ones64[:], 1.0 / 64), [], ["ones64"])
    P.op("dve", lambda e: e.memset(ones1[:], 1.0), [], ["ones1"])
    P.op("act", lambda e: e.activation(out=Acol[:], in_=gdc[:, 12:13], func=AF.Exp), ["gdc"], ["Acol"])
    nAcol = P.sb([64, 1], f32, "nAcol")
    P.op("dve", lambda e: e.tensor_scalar(out=nAcol[:], in0=Acol[:], scalar1=-1.0, scalar2=None, op0=ALU.mult), ["Acol"], ["nAcol"])

    def T2(shape, name, dt=f32):
        return [P.sb(shape, dt, f"{name}{i}") for i in range(2)]

    NB = 2
    raw_r = T2([64, RTT + 1], "raw_r"); raw_k = T2([64, RTT + 1], "raw_k"); raw_v = T2([64, RTT + 1], "raw_v")
    raw_l = T2([128, RTT + 1], "raw_l"); raw_va = [T2([128, RTT + 1], "raw_va0"), T2([128, RTT + 1], "raw_va1")]
    NTMP = 56
    tmp = [P.sb([128, RTT], f32, f"tmp{i}") for i in range(NTMP)]
    r_t = T2([64, RTT], "r_t"); w_t = T2([64, RTT], "w_t"); a_t = T2([64, RTT], "a_t"); b_t = T2([64, RTT], "b_t")
    g_t = T2([64, RTT], "g_t"); v_t = T2([64, RTT], "v_t"); km_t = T2([64, RTT], "km_t"); bon_t = T2([64, RTT], "bon_t")
    ktok_r = T2([128, RG_NBLK, 64], "ktok_r"); vtok_r = T2([128, RG_NBLK, 64], "vtok_r")
    graw_q = T2([64, RTT + 3], "graw_q"); graw_k = T2([64, RTT + 3], "graw_k"); graw_v = T2([64, RTT + 3], "graw_v")
    gz = T2([64, RTT], "gz"); gb = T2([64, RTT], "gb"); ga = T2([64, RTT], "ga")
    gq_t = T2([64, RTT], "gq_t"); gk_t = T2([64, RTT], "gk_t"); gal_t = T2([64, RTT], "gal_t"); gnb_t = T2([64, RTT], "gnb_t")
    gv_t = T2([64, RTT], "gv_t"); gkb_t = T2([64, RTT], "gkb_t")
    ktok_g = T2([128, RG_NBLK, 64], "ktok_g"); vtok_g = T2([128, RG_NBLK, 64], "vtok_g")
    ytile = T2([64, RTT], "ytile"); otile = T2([64, RTT], "otile")
    yout = T2([64, RTT], "yout"); oout = T2([64, RTT], "oout")
    vmask = {"r": [P.sb([128, 64], f32, f"vmask_r{i}") for i in range(3)], "g": [P.sb([128, 64], f32, f"vmask_g{i}") for i in range(3)]}
    arep = {"r": [P.sb([64, 64], f32, f"arep_r{i}") for i in range(3)], "g": [P.sb([64, 64], f32, f"arep_g{i}") for i in range(3)]}
    St = {"r": [P.sb([64, 64], f32, f"St_r{i}") for i in range(3)], "g": [P.sb([64, 64], f32, f"St_g{i}") for i in range(3)]}
    Stmp = {"r": [P.sb([64, 64], f32, f"Stmp_r{i}") for i in range(2)], "g": [P.sb([64, 64], f32, f"Stmp_g{i}") for i in range(2)]}

    ps_kv = {"r": P.ps([64, 512], f32, "ps_kv_r"), "g": P.ps([64, 512], f32, "ps_kv_g")}
    ps_sa = {"r": P.ps([64, 512], f32, "ps_sa_r"), "g": P.ps([64, 512], f32, "ps_sa_g")}
    ps_y1 = {"r": P.ps([64, 512], f32, "ps_yr"), "g": P.ps([64, 512], f32, "ps_yg")}
    ps_y = {"r": [ps_y1["r"], ps_y1["r"]], "g": [ps_y1["g"], ps_y1["g"]]}
    ps_p = [P.ps([128, 512], f32, f"ps_p{i}") for i in range(2)]
    cnt = dict(pp=0, tmp=0, kv=0, sa=0)
    finals = []

    def fin():
        k = ("fin", len(finals))
        finals.append(k)
        return k

    for kind in ("r", "g"):
        P.op("dve", lambda e, kind=kind: e.memset(St[kind][2][:], 0.0), [], [("St", kind, 2)])

    def newtmp():
        i = cnt["tmp"]
        cnt["tmp"] += 1
        assert i < NTMP
        return tmp[i], ("tmp", i)

    def newps():
        i = cnt["pp"] % 2
        cnt["pp"] += 1
        return ps_p[i][:, 0:RTT], ("ps_p", i)

    def bcast_sum(dst, dkey, src, skey, lhs, lkey, rows=64):
        P.mm(dst, lhs, src, True, True, [skey, lkey], [dkey])

    def rsqrt_from_ps(out_ap, okey, ps_ap, pkey, eps):
        P.op("act", lambda e: e.activation(out=out_ap, in_=ps_ap, func=AF.Sqrt, bias=eps, scale=1.0), [pkey], [okey])
        P.op("dve", lambda e: e.reciprocal(out=out_ap, in_=out_ap), [okey], [okey])

    def transpose_to_tok(dst, dkey, srcT, skey):
        ps_t, ptk = newps()
        for bq in range(RG_NBLK):
            P.mm(ps_t[:, bq * 64:(bq + 1) * 64], srcT[:, bq * 128:(bq + 1) * 128], ident[0:64, 0:64], True, True, [skey, "ident"], [ptk])
        P.op("act", lambda e: e.activation(out=dst[:].rearrange("p a b -> p (a b)"), in_=ps_t[:, 0:RG_NBLK * 64], func=AF.Copy), [ptk], [dkey])

    def shift_mix(raw, rkey, mu_ap, mukey, out_ap, okey, rows):
        t, tk = newtmp()
        P.op("dve", lambda e: e.tensor_tensor(out=t[:rows, :], in0=raw[:rows, 0:RTT], in1=raw[:rows, 1:RTT + 1], op=ALU.subtract), [rkey], [tk])
        P.op("dve", lambda e: e.scalar_tensor_tensor(out=out_ap, in0=t[:rows, :], scalar=mu_ap, in1=raw[:rows, 1:RTT + 1], op0=ALU.mult, op1=ALU.add),
             [tk, rkey, mukey], [okey])

    def load_shift(raw, key, src_d, t0, rows, npre):
        if t0 == 0:
            P.op("dve", lambda e: e.memset(raw[:rows, 0:npre], 0.0), [], [key + ("z",)])
            P.dma("sp", raw[:rows, npre:npre + RTT], src_d[:, 0:RTT], [key + ("z",)], [key])
        else:
            P.dma("sp", raw[:rows, :], src_d[:, t0 - npre:t0 + RTT], [], [key])

    def prep_rwkv(n):
        bi = n % 2
        t0 = n * RTT
        ts = slice(t0, t0 + RTT)
        K = lambda nm: ("rw", nm, bi)
        load_shift(raw_r[bi], K("raw_r"), rw_r_d, t0, 64, 1)
        load_shift(raw_k[bi], K("raw_k"), rw_k_d, t0, 64, 1)
        load_shift(raw_v[bi], K("raw_v"), rw_v_d, t0, 64, 1)
        load_shift(raw_l[bi], K("raw_l"), rw_lora_d, t0, 128, 1)
        shift_mix(raw_r[bi], K("raw_r"), rwc[:, 0:1], "rwc", r_t[bi][:], K("r"), 64)
        kx, kxk = newtmp()
        shift_mix(raw_k[bi], K("raw_k"), rwc[:, 1:2], "rwc", kx[:64, :], kxk, 64)
        shift_mix(raw_v[bi], K("raw_v"), rwc[:, 2:3], "rwc", v_t[bi][:], K("v"), 64)
        lo, lok = newtmp()
        shift_mix(raw_l[bi], K("raw_l"), mul[:, 0:1], "mul", lo[:, :], lok, 128)
        th, thk = newtmp()
        P.op("act", lambda e: e.activation(out=th[0:32, :], in_=lo[0:32, :], func=AF.Tanh), [lok], [thk])
        ps, psk = newps()
        P.mm(ps[0:64, :], wup[:], th[0:32, :], True, True, ["wup", thk], [psk])
        e1, e1k = newtmp()
        nw0, nw0k = newtmp()
        P.op("dve", lambda e: e.tensor_scalar(out=nw0[0:64, 0:1], in0=rwc[:, 3:4], scalar1=-1.0, scalar2=None, op0=ALU.mult), ["rwc"], [nw0k])
        P.op("act", lambda e: e.activation(out=e1[0:64, :], in_=ps[0:64, :], func=AF.Exp, bias=nw0[0:64, 0:1], scale=-1.0), [psk, nw0k], [e1k])
        P.op("act", lambda e: e.activation(out=e1[0:64, :], in_=e1[0:64, :], func=AF.Ln, bias=1.0, scale=1.0), [e1k], [e1k])
        P.op("act", lambda e: e.activation(out=e1[0:64, :], in_=e1[0:64, :], func=AF.Exp, bias=-0.5, scale=-1.0), [e1k], [e1k])
        P.op("act", lambda e: e.activation(out=w_t[bi][:], in_=e1[0:64, :], func=AF.Exp, scale=-1.0), [e1k], [K("w")])
        ps2, ps2k = newps()
        P.mm(ps2[0:64, :], aup[32:64, :], lo[32:64, :], True, True, ["aup", lok], [ps2k])
        asg, asgk = newtmp()
        P.op("act", lambda e: e.activation(out=asg[0:64, :], in_=ps2[0:64, :], func=AF.Sigmoid, bias=rwc[:, 4:5], scale=1.0), [ps2k, "rwc"], [asgk])
        sg_, sgk = newtmp()
        P.op("act", lambda e: e.activation(out=sg_[64:128, :], in_=lo[64:128, :], func=AF.Sigmoid), [lok], [sgk])
        ps3, ps3k = newps()
        P.mm(ps3[0:64, :], gup[64:128, :], sg_[64:128, :], True, True, ["gup", sgk], [ps3k])
        P.op("act", lambda e: e.activation(out=g_t[bi][:], in_=ps3[0:64, :], func=AF.Copy), [ps3k], [K("g")])
        return dict(bi=bi, ts=ts, kx=kx, kxk=kxk, asg=asg, asgk=asgk, lo=lo, lok=lok)

    def prep_rwkv2(n, c):
        bi = c["bi"]
        t0 = n * RTT
        ts = c["ts"]
        K = lambda nm: ("rw", nm, bi)
        kx, kxk, asg, asgk = c["kx"], c["kxk"], c["asg"], c["asgk"]
        kk, kkk = newtmp()
        P.op("dve", lambda e: e.tensor_scalar(out=kk[0:64, :], in0=kx[0:64, :], scalar1=rwc[:, 5:6], scalar2=None, op0=ALU.mult), [kxk, "rwc"], [kkk])
        sq, sqk = newtmp()
        P.op("act", lambda e: e.activation(out=sq[0:64, :], in_=kk[0:64, :], func=AF.Square), [kkk], [sqk])
        ps, psk = newps()
        P.mm(ps[0:64, :], ones1[:], sq[0:64, :], True, True, ["ones1", sqk], [psk])
        rsq, rsqk = sq, sqk
        rsqrt_from_ps(rsq[0:64, :], rsqk, ps[0:64, :], psk, 1e-12)
        P.op("dve", lambda e: e.tensor_tensor(out=kk[0:64, :], in0=kk[0:64, :], in1=rsq[0:64, :], op=ALU.mult), [kkk, rsqk], [kkk])
        P.op("dve", lambda e: e.tensor_scalar(out=a_t[bi][:], in0=kk[0:64, :], scalar1=-1.0, scalar2=None, op0=ALU.mult), [kkk], [K("a")])
        P.op("dve", lambda e: e.tensor_tensor(out=b_t[bi][:], in0=kk[0:64, :], in1=asg[0:64, :], op=ALU.mult), [kkk, asgk], [K("b")])
        f, fk = newtmp()
        P.op("dve", lambda e: e.tensor_scalar(out=f[0:64, :], in0=asg[0:64, :], scalar1=-1.0, scalar2=rwc[:, 6:7], op0=ALU.add, op1=ALU.mult), [asgk, "rwc"], [fk])
        P.op("dve", lambda e: e.scalar_tensor_tensor(out=km_t[bi][:], in0=f[0:64, :], scalar=1.0, in1=kx[0:64, :], op0=ALU.add, op1=ALU.mult), [fk, kxk], [K("km")])
        if layer == 0:
            fk2 = fin()
            P.dma("sp", vfirst_out_d[:, ts], v_t[bi][:], [K("v")], [fk2])
        else:
            vas = []
            for h2 in range(2):
                key = K(f"raw_va{h2}")
                src = rw_vall_d[h2 * 128:(h2 + 1) * 128, :]
                if t0 == 0:
                    P.op("dve", lambda e, h2=h2: e.memset(raw_va[h2][bi][:, 0:1], 0.0), [], [key + ("z",)])
                    P.dma("sp", raw_va[h2][bi][:, 1:1 + RTT], src[:, 0:RTT], [key + ("z",)], [key])
                else:
                    P.dma("sp", raw_va[h2][bi][:, :], src[:, t0 - 1:t0 + RTT], [], [key])
                va, vak = newtmp()
                shift_mix(raw_va[h2][bi], key, muv[:, h2:h2 + 1], "muv", va[:, :], vak, 128)
                vas.append((va, vak))
            ps4, ps4k = newps()
            for h2 in range(2):
                P.mm(ps4[0:32, :], vdown[:, h2, :], vas[h2][0][:, :], h2 == 0, h2 == 1, ["vdown", vas[h2][1]], [ps4k])
            t32, t32k = newtmp()
            P.op("act", lambda e: e.activation(out=t32[0:32, :], in_=ps4[0:32, :], func=AF.Copy), [ps4k], [t32k])
            ps5, ps5k = newps()
            P.mm(ps5[0:64, :], vup[:], t32[0:32, :], True, True, ["vup", t32k], [ps5k])
            sgv, sgvk = newtmp()
            P.op("act", lambda e: e.activation(out=sgv[0:64, :], in_=ps5[0:64, :], func=AF.Sigmoid, bias=rwc[:, 10:11], scale=1.0), [ps5k, "rwc"], [sgvk])
            vf, vfk = newtmp()
            P.dma("sp", vf[0:64, :], vfirst_in_d[:, ts], [], [vfk])
            P.op("dve", lambda e: e.tensor_tensor(out=vf[0:64, :], in0=vf[0:64, :], in1=v_t[bi][:], op=ALU.subtract), [vfk, K("v")], [vfk])
            P.op("dve", lambda e: e.tensor_tensor(out=vf[0:64, :], in0=vf[0:64, :], in1=sgv[0:64, :], op=ALU.mult), [vfk, sgvk], [vfk])
            P.op("dve", lambda e: e.tensor_tensor(out=v_t[bi][:], in0=v_t[bi][:], in1=vf[0:64, :], op=ALU.add), [vfk, K("v")], [K("v")])
            fk2 = fin()
            P.dma("sp", vfirst_out_d[:, ts], v_t[bi][:], [K("v")], [fk2])
        rk, rkk = newtmp()
        P.op("dve", lambda e: e.scalar_tensor_tensor(out=rk[0:64, :], in0=r_t[bi][:], scalar=rwc[:, 7:8], in1=km_t[bi][:], op0=ALU.mult, op1=ALU.mult),
             [K("r"), K("km"), "rwc"], [rkk])
        ps6, ps6k = newps()
        P.mm(ps6[0:64, :], ones1[:], rk[0:64, :], True, True, ["ones1", rkk], [ps6k])
        P.op("dve", lambda e: e.tensor_tensor(out=bon_t[bi][:], in0=ps6[0:64, :], in1=v_t[bi][:], op=ALU.mult), [ps6k, K("v")], [K("bon")])
        transpose_to_tok(ktok_r[bi], K("ktok"), km_t[bi], K("km"))
        transpose_to_tok(vtok_r[bi], K("vtok"), v_t[bi], K("v"))

    def prep_gdn(n):
        bi = n % 2
        t0 = n * RTT
        ts = slice(t0, t0 + RTT)
        K = lambda nm: ("gd", nm, bi)
        load_shift(graw_q[bi], K("raw_q"), gd_q_d, t0, 64, 3)
        load_shift(graw_k[bi], K("raw_k"), gd_k_d, t0, 64, 3)
        load_shift(graw_v[bi], K("raw_v"), gd_v_d, t0, 64, 3)
        P.dma("sp", gz[bi][:], gd_z_d[:, ts], [], [K("z")])
        P.dma("sp", gb[bi][:], gd_b_d[:, ts].partition_broadcast(64), [], [K("b")])
        P.dma("sp", ga[bi][:], gd_a_d[:, ts].partition_broadcast(64), [], [K("a")])
        outs = {}
        for gi_, (raw, rk_) in enumerate(((graw_q[bi], K("raw_q")), (graw_k[bi], K("raw_k")), (graw_v[bi], K("raw_v")))):
            cv, cvk = newtmp()
            P.op("dve", lambda e, raw=raw, cv=cv, gi_=gi_: e.tensor_scalar(out=cv[0:64, :], in0=raw[:, 0:RTT], scalar1=gdc[:, 4 * gi_:4 * gi_ + 1], scalar2=None, op0=ALU.mult),
                 [rk_, "gdc"], [cvk])
            for i in range(1, 4):
                P.op("dve", lambda e, raw=raw, cv=cv, gi_=gi_, i=i: e.scalar_tensor_tensor(out=cv[0:64, :], in0=raw[:, i:i + RTT], scalar=gdc[:, 4 * gi_ + i:4 * gi_ + i + 1],
                                                                                      in1=cv[0:64, :], op0=ALU.mult, op1=ALU.add),
                     [rk_, "gdc", cvk], [cvk])
            P.op("act", lambda e, cv=cv: e.activation(out=cv[0:64, :], in_=cv[0:64, :], func=AF.Silu), [cvk], [cvk])
            outs[gi_] = (cv, cvk)
        for gi_, scale_, dst, dk in ((0, 0.125, gq_t[bi], K("q")), (1, 1.0, gk_t[bi], K("k"))):
            cv, cvk = outs[gi_]
            sq, sqk = newtmp()
            P.op("act", lambda e, cv=cv, sq=sq: e.activation(out=sq[0:64, :], in_=cv[0:64, :], func=AF.Square), [cvk], [sqk])
            ps, psk = newps()
            P.mm(ps[0:64, :], ones1[:], sq[0:64, :], True, True, ["ones1", sqk], [psk])
            rsqrt_from_ps(sq[0:64, :], sqk, ps[0:64, :], psk, 1e-6)
            P.op("dve", lambda e, cv=cv, sq=sq, dst=dst, scale_=scale_: e.scalar_tensor_tensor(out=dst[:], in0=cv[0:64, :], scalar=scale_, in1=sq[0:64, :], op0=ALU.mult, op1=ALU.mult),
                 [cvk, sqk], [dk])
        cvv, cvvk = outs[2]
        P.op("act", lambda e: e.activation(out=gv_t[bi][:], in_=cvv[0:64, :], func=AF.Copy), [cvvk], [K("v")])
        P.op("act", lambda e: e.activation(out=gb[bi][:], in_=gb[bi][:], func=AF.Sigmoid), [K("b")], [K("b")])
        P.op("act", lambda e: e.activation(out=ga[bi][:], in_=ga[bi][:], func=AF.Exp, bias=gdc[:, 13:14], scale=1.0), [K("a"), "gdc"], [K("a")])
        P.op("act", lambda e: e.activation(out=ga[bi][:], in_=ga[bi][:], func=AF.Ln, bias=1.0, scale=1.0), [K("a")], [K("a")])
        P.op("act", lambda e: e.activation(out=gal_t[bi][:], in_=ga[bi][:], func=AF.Exp, scale=nAcol[:, 0:1]), [K("a"), "nAcol"], [K("al")])
        P.op("dve", lambda e: e.tensor_tensor(out=gkb_t[bi][:], in0=gk_t[bi][:], in1=gb[bi][:], op=ALU.mult), [K("k"), K("b")], [K("kb")])
        P.op("dve", lambda e: e.scalar_tensor_tensor(out=gnb_t[bi][:], in0=gkb_t[bi][:], scalar=-1.0, in1=gal_t[bi][:], op0=ALU.mult, op1=ALU.mult),
             [K("kb"), K("al")], [K("nb")])
        transpose_to_tok(ktok_g[bi], K("ktok"), gkb_t[bi], K("kb"))
        transpose_to_tok(vtok_g[bi], K("vtok"), gv_t[bi], K("v"))
        P.op("act", lambda e: e.activation(out=gz[bi][:], in_=gz[bi][:], func=AF.Silu), [K("z")], [K("z")])

    def scan_parts(kind, t, aT, akey, wT, wkey, bT, bkey, ktok, kkey, vtok, vkey, rT, rkey, psy, pykey):
        tl = t % RTT
        blk, tb = tl // 128, tl % 128
        prev = (t - 1) % 3
        cur = t % 3
        vm = vmask[kind][t % 3]
        vmk = ("vmask", kind, t % 3)
        kvs = "kv" + kind
        sas = "sa" + kind
        kv_ps = ps_kv[kind][:, 0:64]
        sa_ps = ps_sa[kind][:, 0:64]
        stm = Stmp[kind][t % 2]
        stk = ("Stmp", kind, t % 2)

        def f_vmask():
            P.op("act", lambda e: e.activation(out=vm[:], in_=vtok[:, blk, :], func=AF.Copy, scale=ident[:, tb:tb + 1]), [vkey, "ident"], [vmk])

        def f_kv():
            P.mm(kv_ps, ktok[:, blk, :], vm[:], True, True, [kkey, vmk], [("ps_s", kvs)])

        def f_sa():
            P.mm(sa_ps, aT[:, tl:tl + 1].to_broadcast([64, 64]), St[kind][prev][:], True, True, [akey, ("St", kind, prev)], [("ps_s", sas)])

        def f_op1():
            P.op("dve", lambda e: e.scalar_tensor_tensor(out=stm[:], in0=St[kind][prev][:], scalar=wT[:, tl:tl + 1], in1=kv_ps, op0=ALU.mult, op1=ALU.add),
                 [("St", kind, prev), wkey, ("ps_s", kvs)], [stk])

        def f_op2():
            P.op("dve", lambda e: e.scalar_tensor_tensor(out=St[kind][cur][:], in0=sa_ps, scalar=bT[:, tl:tl + 1], in1=stm[:], op0=ALU.mult, op1=ALU.add),
                 [("ps_s", sas), bkey, stk], [("St", kind, cur)])

        def f_y():
            P.mm(psy[:, tl:tl + 1], St[kind][cur][:], rT[:, tl:tl + 1], True, True, [("St", kind, cur), rkey], [pykey])

        return dict(vmask=f_vmask, kv=f_kv, sa=f_sa, op1=f_op1, op2=f_op2, y=f_y)

    def post_rwkv(n):
        bi = n % 2
        ts = slice(n * RTT, (n + 1) * RTT)
        K = lambda nm: ("rw", nm, bi)
        y = ytile[bi]
        P.op("act", lambda e: e.activation(out=y[:], in_=ps_y["r"][bi][:, 0:RTT], func=AF.Copy), [("ps_y", "r", 0)], [K("y")])
        ps, psk = newps()
        P.mm(ps[0:64, :], ones64[:], y[:], True, True, ["ones64", K("y")], [psk])
        P.op("dve", lambda e: e.tensor_tensor(out=y[:], in0=y[:], in1=ps[0:64, :], op=ALU.subtract), [K("y"), psk], [K("y")])
        sq, sqk = newtmp()
        P.op("act", lambda e: e.activation(out=sq[0:64, :], in_=y[:], func=AF.Square), [K("y")], [sqk])
        ps2, ps2k = newps()
        P.mm(ps2[0:64, :], ones64[:], sq[0:64, :], True, True, ["ones64", sqk], [ps2k])
        rsqrt_from_ps(sq[0:64, :], sqk, ps2[0:64, :], ps2k, 64e-5)
        P.op("dve", lambda e: e.tensor_tensor(out=y[:], in0=y[:], in1=sq[0:64, :], op=ALU.mult), [K("y"), sqk], [K("y")])
        P.op("dve", lambda e: e.tensor_scalar(out=y[:], in0=y[:], scalar1=rwc[:, 8:9], scalar2=rwc[:, 9:10], op0=ALU.mult, op1=ALU.add), [K("y"), "rwc"], [K("y")])
        P.op("dve", lambda e: e.tensor_tensor(out=y[:], in0=y[:], in1=bon_t[bi][:], op=ALU.add), [K("y"), K("bon")], [K("y")])
        P.op("dve", lambda e: e.tensor_tensor(out=yout[bi][:], in0=y[:], in1=g_t[bi][:], op=ALU.mult), [K("y"), K("g")], [K("yout")])
        P.dma("sp", yb_d[:, ts], yout[bi][:], [K("yout")], [fin()])

    def post_gdn(n):
        bi = n % 2
        ts = slice(n * RTT, (n + 1) * RTT)
        K = lambda nm: ("gd", nm, bi)
        o = otile[bi]
        P.op("act", lambda e: e.activation(out=o[:], in_=ps_y["g"][bi][:, 0:RTT], func=AF.Copy), [("ps_y", "g", 0)], [K("o")])
        sq, sqk = newtmp()
        P.op("act", lambda e: e.activation(out=sq[0:64, :], in_=o[:], func=AF.Square), [K("o")], [sqk])
        ps, psk = newps()
        P.mm(ps[0:64, :], ones64[:], sq[0:64, :], True, True, ["ones64", sqk], [psk])
        rsqrt_from_ps(sq[0:64, :], sqk, ps[0:64, :], psk, 1e-6)
        P.op("dve", lambda e: e.scalar_tensor_tensor(out=o[:], in0=o[:], scalar=gdc[:, 14:15], in1=sq[0:64, :], op0=ALU.mult, op1=ALU.mult), [K("o"), sqk, "gdc"], [K("o")])
        P.op("dve", lambda e: e.tensor_tensor(out=oout[bi][:], in0=o[:], in1=gz[bi][:], op=ALU.mult), [K("o"), K("z")], [K("oout")])
        P.dma("sp", yc_d[:, ts], oout[bi][:], [K("oout")], [fin()])

    ntiles = (nsteps + RTT - 1) // RTT

    def prep_all(n):
        cnt["tmp"] = 0
        c = prep_rwkv(n)
        prep_rwkv2(n, c)
        prep_gdn(n)

    prep_all(0)
    for n in range(ntiles):
        stepper = Stepper(P, lambda n=n: prep_all(n + 1)) if n + 1 < ntiles else None
        bi = n % 2
        Kr = lambda nm: ("rw", nm, bi)
        Kg = lambda nm: ("gd", nm, bi)
        nst = min(RTT, nsteps - n * RTT)

        def parts(tl):
            t = n * RTT + tl
            return (scan_parts("r", t, a_t[bi], Kr("a"), w_t[bi], Kr("w"), b_t[bi], Kr("b"), ktok_r[bi], Kr("ktok"), vtok_r[bi], Kr("vtok"), r_t[bi], Kr("r"),
                               ps_y["r"][bi], ("ps_y", "r", 0)),
                    scan_parts("g", t, gk_t[bi], Kg("k"), gal_t[bi], Kg("al"), gnb_t[bi], Kg("nb"), ktok_g[bi], Kg("ktok"), vtok_g[bi], Kg("vtok"), gq_t[bi], Kg("q"),
                               ps_y["g"][bi], ("ps_y", "g", 0)))

        cur_p = parts(0)
        for pp_ in cur_p:
            pp_["vmask"]()
        for pp_ in cur_p:
            pp_["kv"]()
        prev_p = None
        for tl in range(nst):
            nxt_p = parts(tl + 1) if tl + 1 < nst else None
            if nxt_p is not None:
                for pp_ in nxt_p:
                    pp_["vmask"]()
            for pp_ in cur_p:
                pp_["sa"]()
            if prev_p is not None:
                for pp_ in prev_p:
                    pp_["y"]()
            for pp_ in cur_p:
                pp_["op1"]()
            if nxt_p is not None:
                for pp_ in nxt_p:
                    pp_["kv"]()
            for pp_ in cur_p:
                pp_["op2"]()
            prev_p, cur_p = cur_p, nxt_p
            if stepper is not None and tl >= 8:
                stepper.step(1)
        for pp_ in prev_p:
            pp_["y"]()
        if stepper is not None:
            stepper.finish()
        post_rwkv(n)
        post_gdn(n)
    return P.emit(finals)


NB = 64

def core_blocks(c):
    j = c % 4
    out = []
    for k in range(8):
        out += [8 * k + j, 8 * k + 7 - j]
    return out

def core_tokens(c):
    b = c // 4
    pos = np.concatenate([np.arange(g * 128, (g + 1) * 128) for g in core_blocks(c)])
    return b, pos

def fm_cols():
    cols = []
    for j in range(4):
        cols += list(range(64 * j, 64 * j + 64)) + list(range(64 * (j + 4), 64 * (j + 4) + 64))
    cols += list(range(512, 640))
    cols += list(range(768, 1280))
    cols += list(range(1280, 1344))
    cols += list(range(1352, 2248))
    g0 = 2248
    for h in range(4):
        cols += list(range(g0 + 64 * h, g0 + 64 * h + 64)) + list(range(g0 + 256 + 64 * h, g0 + 256 + 64 * h + 64))
    cols += list(range(g0 + 512, g0 + 768))
    assert len(cols) == 2880
    return cols

def tok_cols():
    cols = list(range(640, 768)) + list(range(1344, 1352)) + list(range(3272, 3276)) + list(range(3276, 3280)) + list(range(3016, 3272))
    assert len(cols) == 400
    return cols

def gcol(v):
    return np.ascontiguousarray(v.reshape(8, 128).T)

def core_blocks(c):
    j = c % 4
    return [4 * m + j for m in range(16)]

def tri4_for(c):
    j = c % 4
    q = np.arange(128)[:, None]
    col = np.arange(512)[None, :]
    blk, w = col // 128, col % 128
    valid = (blk < j) | ((blk == j) & (w <= q))
    return np.where(valid, 0.0, -1e30).astype(np.float32)

def q_pair_rows():
    rows = []
    for j in range(4):
        rows += list(range(64 * j, 64 * j + 64)) + list(range(64 * (j + 4), 64 * (j + 4) + 64))
    return rows

def rg_inputs(d, p_b, c, l, vfirst=None):
    j = c % 4
    hs = slice(64 * j, 64 * j + 64)
    pb = p_b[:, 1352:2248]
    pc = p_b[:, 2248:3280]
    T = lambda a: np.ascontiguousarray(a.T.astype(np.float32))
    mu = d["rwkv_mu"][l]
    rwc = np.zeros((64, 16), np.float32)
    rwc[:, 0] = mu[0:256][hs]; rwc[:, 1] = mu[256:512][hs]; rwc[:, 2] = mu[512:768][hs]
    rwc[:, 3] = d["rwkv_w0"][l][hs]; rwc[:, 4] = d["rwkv_a0"][l][hs]; rwc[:, 5] = d["rwkv_k_k"][l][hs]; rwc[:, 6] = d["rwkv_k_a"][l][hs]
    rwc[:, 7] = d["rwkv_r_k"][l][j]; rwc[:, 8] = d["rwkv_gn_w"][l][hs]; rwc[:, 9] = d["rwkv_gn_b"][l][hs]
    if l >= 1:
        rwc[:, 10] = d["rwkv_v0"][l - 1][hs]
        vdown = np.ascontiguousarray(d["rwkv_v_down"][l - 1].reshape(2, 128, 32).transpose(1, 0, 2))
        vup = np.ascontiguousarray(d["rwkv_v_up"][l - 1][:, hs])
    else:
        vdown = np.zeros((128, 2, 32), np.float32)
        vup = np.zeros((32, 64), np.float32)
    gdc = np.zeros((64, 16), np.float32)
    conv = d["gdn_conv"][l]
    for gi in range(3):
        for i in range(4):
            gdc[:, 4 * gi + i] = conv[i, gi * 256 + 64 * j: gi * 256 + 64 * j + 64]
    gdc[:, 12] = d["gdn_a_log"][l][j]; gdc[:, 13] = d["gdn_dt_bias"][l][j]; gdc[:, 14] = d["gdn_o_norm"][l]
    L = p_b.shape[0]
    return dict(
        rw_r=T(pb[:, 0:256][:, hs]), rw_k=T(pb[:, 256:512][:, hs]), rw_v=T(pb[:, 512:768][:, hs]), rw_lora=T(pb[:, 768:896]), rw_vall=T(pb[:, 512:768]),
        rw_cols=rwc, rw_mu_lora=mu[768:896][:, None].copy(), rw_mu_vall=np.ascontiguousarray(mu[512:768].reshape(2, 128).T),
        rw_wup=np.ascontiguousarray(d["rwkv_w_up"][l][:, hs]), rw_aup=np.ascontiguousarray(d["rwkv_a_up"][l][:, hs]), rw_gup=np.ascontiguousarray(d["rwkv_g_up"][l][:, hs]),
        rw_vdown=vdown, rw_vup=vup, vfirst_in=(vfirst if vfirst is not None else np.zeros((64, L), np.float32)),
        gd_q=T(pc[:, 0:256][:, hs]), gd_k=T(pc[:, 256:512][:, hs]), gd_v=T(pc[:, 512:768][:, hs]), gd_z=T(pc[:, 768:1024][:, hs]),
        gd_b=np.ascontiguousarray(pc[:, 1024 + j][None, :]), gd_a=np.ascontiguousarray(pc[:, 1028 + j][None, :]), gd_cols=gdc,
        ident=np.eye(128, dtype=np.float32))


_PROGS = {}


def _prog(name, builder):
    if name not in _PROGS:
        _PROGS[name] = builder()
    return _PROGS[name]


def _mixers(d, l, pT_list, ptok_list, vfirst_list):
    fm, tk = fm_cols(), tok_cols()
    p_full = [np.zeros((L, 3280), np.float32) for _ in range(2)]
    for c in range(8):
        b, pos = core_tokens(c)
        tmp = np.zeros((2048, 3280), np.float32)
        tmp[:, fm] = pT_list[c].T
        tmp[:, tk] = ptok_list[c]
        p_full[b][pos] = tmp
    C = dsa_consts()
    maps = []
    for c in range(8):
        b, pos = core_tokens(c)
        pa = p_full[b]
        maps.append(dict(kT=np.ascontiguousarray(pa[:, 512:640].T), kiT=np.ascontiguousarray(pa[:, 1280:1344].T), vtok=np.ascontiguousarray(pa[:, 640:768]),
                         qT=np.ascontiguousarray(pa[pos][:, q_pair_rows()].T), qiT=np.ascontiguousarray(pa[pos][:, 768:1280].T),
                         wi=np.ascontiguousarray(pa[pos][:, 1344:1352]),
                         cosT=C["cosT"], sinT=C["sinT"], cosq=np.ascontiguousarray(C["cosT"][:, pos]), sinq=np.ascontiguousarray(C["sinT"][:, pos]),
                         gq=np.tile(d["att_q_norm"][l], 2)[:, None].copy(), gk=np.tile(d["att_k_norm"][l], 2)[:, None].copy(),
                         Rm=C["Rm"], tri4=tri4_for(c), I4=C["I4"], sel=C["sel"], bo=C["bo"], pw=C["pw"]))
    res_a = run(_prog("dsa", build_dsa), maps)
    maps = [rg_inputs(d, p_full[c // 4], c, l, vfirst_list[c] if vfirst_list is not None else None) for c in range(8)]
    res_r = run(_prog(f"rg{min(l, 1)}", lambda: build_rg(min(l, 1))), maps)
    yT = []
    for c in range(8):
        b, pos = core_tokens(c)
        y = np.zeros((1024, 2048), np.float32)
        y[0:512] = res_a[c]["yaT"]
        for jj in range(4):
            y[512 + 64 * jj:512 + 64 * jj + 64] = res_r[4 * b + jj]["ybT"][:, pos]
            y[768 + 64 * jj:768 + 64 * jj + 64] = res_r[4 * b + jj]["ycT"][:, pos]
        yT.append(y)
    return yT, [res_r[c]["vfirst_out"] for c in range(8)]


def kernel(**inputs):
    d = {k: np.asarray(v) for k, v in inputs.items()}
    perm = fm_cols() + tok_cols()
    x = d["x"]

    def ffn_w(l, i, tag):
        return {f"g_{tag}": gcol(d["ffn_norm"][l, i]), f"wg_{tag}": np.ascontiguousarray(d["ffn_w_gate"][l, i]),
                f"wu_{tag}": np.ascontiguousarray(d["ffn_w_up"][l, i]), f"wd_{tag}": np.ascontiguousarray(d["ffn_w_down"][l, i])}

    def inp_w(l, tag):
        return {f"g_{tag}": gcol(d["mix_norm"][l]), f"win_{tag}": np.ascontiguousarray(d["w_in"][l][:, perm])}

    w0 = {**ffn_w(0, 0, "f00"), **inp_w(0, "i0")}
    maps = []
    for c in range(8):
        b, pos = core_tokens(c)
        maps.append({"xT": np.ascontiguousarray(x[b, pos, :].T), **w0})
    r0 = run(_prog("P0", lambda: build_P([("ffn", "f00"), ("inproj", "i0")])), maps)
    yT, vfirst = _mixers(d, 0, [r0[c]["pT_i0"] for c in range(8)], [r0[c]["ptok_i0"] for c in range(8)], None)
    w1 = {"wo_o0": np.ascontiguousarray(d["w_out"][0]), **ffn_w(0, 1, "f01"), **ffn_w(1, 0, "f10"), **inp_w(1, "i1")}
    maps = [{"xT": r0[c]["xo"], "yT_o0": yT[c], **w1} for c in range(8)]
    r1 = run(_prog("P1", lambda: build_P([("out", "o0"), ("ffn", "f01"), ("ffn", "f10"), ("inproj", "i1")])), maps)
    yT, _ = _mixers(d, 1, [r1[c]["pT_i1"] for c in range(8)], [r1[c]["ptok_i1"] for c in range(8)], vfirst)
    w2 = {"wo_o1": np.ascontiguousarray(d["w_out"][1]), **ffn_w(1, 1, "f11")}
    maps = [{"xT": r1[c]["xo"], "yT_o1": yT[c], **w2} for c in range(8)]
    r2 = run(_prog("P2", lambda: build_P([("out", "o1"), ("ffn", "f11")])), maps)
    out = np.zeros((2, L, 1024), np.float32)
    for c in range(8):
        b, pos = core_tokens(c)
        out[b, pos, :] = r2[c]["xo"].T
    return out
```
